# Optimizing a Trainium2 kernel written in Bass

```python
import jax
import jax.numpy as jnp
from jax import lax
import numpy as np

D_MODEL = 1024
BATCH = 8
SEQ = 4096
DEPTH = 4

RET_HEADS = 4
RET_HEAD_DIM = D_MODEL // 2 // RET_HEADS
RET_VALUE_DIM = D_MODEL // 2 // RET_HEADS
RET_CHUNK = 128
RET_WIDTH = RET_HEADS * RET_VALUE_DIM
WIN_Q_HEADS = 8
WIN_KV_HEADS = 2
WIN_HEAD_DIM = D_MODEL // 2 // WIN_Q_HEADS
WINDOW = 128
WIN_BLOCK = WINDOW
WIN_WIDTH = WIN_Q_HEADS * WIN_HEAD_DIM
FOURIER_GROUPS = 4
ROPE_THETA = 10000.0
N_GROUPS = 4
EXPERTS_PER_GROUP = 8
N_EXPERTS = N_GROUPS * EXPERTS_PER_GROUP
TOP_K = 2
EXPERT_HIDDEN = D_MODEL // 2
LN_EPS = 1e-5
GN_EPS = 1e-6
DEEPNORM_ALPHA = (2.0 * DEPTH) ** 0.25
DEEPNORM_BETA = (8.0 * DEPTH) ** -0.25
N_EVEN = (DEPTH + 1) // 2
N_ODD = DEPTH // 2
RET_QK = RET_HEADS * RET_HEAD_DIM
WIN_KV = WIN_KV_HEADS * WIN_HEAD_DIM
EVEN_SPLITS = tuple(np.cumsum([RET_QK, RET_QK, RET_WIDTH, RET_WIDTH, WIN_WIDTH, WIN_KV])[:].tolist())
IN_EVEN = 2 * RET_QK + 2 * RET_WIDTH + WIN_WIDTH + 2 * WIN_KV

kernel_name = "hybrid_retention_swa_fnet_hmoe_encoder"


def layer_norm(x, gain, bias):
    xf = x.astype(jnp.float32)
    mu = jnp.mean(xf, axis=-1, keepdims=True)
    var = jnp.mean(jnp.square(xf - mu), axis=-1, keepdims=True)
    y = (xf - mu) * lax.rsqrt(var + LN_EPS) * gain.astype(jnp.float32) + bias.astype(jnp.float32)
    return y.astype(x.dtype)


def rope(x, pos):
    d = x.shape[-1]
    half = d // 2
    inv = ROPE_THETA ** (-jnp.arange(half, dtype=jnp.float32) / half)
    ang = pos.astype(jnp.float32)[:, None] * inv[None, :]
    cos = jnp.cos(ang)[None, :, None, :]
    sin = jnp.sin(ang)[None, :, None, :]
    xf = x.astype(jnp.float32)
    x1, x2 = xf[..., :half], xf[..., half:]
    return jnp.concatenate([x1 * cos - x2 * sin, x1 * sin + x2 * cos], axis=-1).astype(x.dtype)


def retention_one_direction(q, k, v, log_gamma, include_diag):
    B, H, S, dk = q.shape
    dv = v.shape[-1]
    C = RET_CHUNK
    NC = S // C
    qc = q.reshape(B, H, NC, C, dk)
    kc = k.reshape(B, H, NC, C, dk)
    vc = v.reshape(B, H, NC, C, dv)
    pos = jnp.arange(C, dtype=jnp.float32)
    lg = log_gamma[:, None, None]
    diff = pos[:, None] - pos[None, :]
    mask = diff >= 0 if include_diag else diff > 0
    dmat = jnp.where(mask[None], jnp.exp(lg * jnp.maximum(diff, 0.0)[None]), 0.0)
    scores = jnp.einsum('bhncd,bhnmd->bhncm', qc, kc) * dmat[None, :, None]
    y_inner = jnp.einsum('bhncm,bhnmv->bhncv', scores, vc)
    zeta = jnp.exp(log_gamma[:, None] * (C - 1.0 - pos)[None])
    kv = jnp.einsum('bhnmd,bhnmv->nbhdv', kc * zeta[None, :, None, :, None], vc)
    chunk_decay = jnp.exp(log_gamma * C)[None, :, None, None]

    def step(state, kv_n):
        return state * chunk_decay + kv_n, state

    _, r_prev = lax.scan(step, jnp.zeros((B, H, dk, dv), jnp.float32), kv)
    xi = jnp.exp(log_gamma[:, None] * (pos + 1.0)[None])
    y_cross = jnp.einsum('bhncd,nbhdv->bhncv', qc * xi[None, :, None, :, None], r_prev)
    return (y_inner + y_cross).reshape(B, H, S, dv)


def bidirectional_retention(q, k, v, decay_logit):
    qf = jnp.transpose(q.astype(jnp.float32), (0, 2, 1, 3))
    kf = jnp.transpose(k.astype(jnp.float32), (0, 2, 1, 3))
    vf = jnp.transpose(v.astype(jnp.float32), (0, 2, 1, 3))
    log_gamma = jax.nn.log_sigmoid(decay_logit.astype(jnp.float32))
    fwd = retention_one_direction(qf, kf, vf, log_gamma[0], True)
    bwd = jnp.flip(retention_one_direction(jnp.flip(qf, 2), jnp.flip(kf, 2), jnp.flip(vf, 2),
                                           log_gamma[1], False), 2)
    return jnp.transpose(fwd + bwd, (0, 2, 1, 3))


def banded_keys(t):
    B, S, Hkv, d = t.shape
    NB = S // WIN_BLOCK
    tp = jnp.pad(t, ((0, 0), (WINDOW, WINDOW), (0, 0), (0, 0))).reshape(B, NB + 2, WIN_BLOCK, Hkv, d)
    return jnp.concatenate([tp[:, :-2], tp[:, 1:-1], tp[:, 2:]], axis=2)


def window_attention_with_sink(q, k, v, sink_logit):
    B, S, Hq, d = q.shape
    Hkv = k.shape[2]
    G = Hq // Hkv
    NB = S // WIN_BLOCK
    qb = q.reshape(B, NB, WIN_BLOCK, Hkv, G, d)
    kb = banded_keys(k)
    vb = banded_keys(v)
    s = jnp.einsum('bnqhgd,bnkhd->bnhgqk', qb, kb).astype(jnp.float32)
    i = jnp.arange(WIN_BLOCK)[:, None]
    j = jnp.arange(3 * WIN_BLOCK)[None, :]
    rel_ok = jnp.abs(i - j + WINDOW) <= WINDOW
    kpos = jnp.arange(NB)[:, None] * WIN_BLOCK + jnp.arange(3 * WIN_BLOCK)[None, :] - WINDOW
    in_ok = (kpos >= 0) & (kpos < S)
    mask = rel_ok[None] & in_ok[:, None, :]
    s = jnp.where(mask[None, :, None, None], s, -1e30)
    sink = jnp.broadcast_to(sink_logit.astype(jnp.float32).reshape(1, 1, Hkv, G, 1, 1), s.shape[:-1] + (1,))
    p = jax.nn.softmax(jnp.concatenate([s, sink], axis=-1), axis=-1)[..., :-1]
    o = jnp.einsum('bnhgqk,bnkhd->bnqhgd', p.astype(v.dtype), vb)
    return o.reshape(B, S, Hq * d)


def even_mixer(x, w_in, decay_logit, gn_gain, sink_logit, w_out, pos):
    B, S, _ = x.shape
    h = x @ w_in
    qa, ka, va, ga, qb, kb, vb = jnp.split(h, EVEN_SPLITS, axis=-1)
    qa = rope(qa.reshape(B, S, RET_HEADS, RET_HEAD_DIM), pos)
    ka = rope(ka.reshape(B, S, RET_HEADS, RET_HEAD_DIM), pos) * (RET_HEAD_DIM ** -0.5)
    va = va.reshape(B, S, RET_HEADS, RET_VALUE_DIM)
    ya = bidirectional_retention(qa, ka, va, decay_logit)
    mu = jnp.mean(ya, axis=-1, keepdims=True)
    var = jnp.mean(jnp.square(ya - mu), axis=-1, keepdims=True)
    ya = ((ya - mu) * lax.rsqrt(var + GN_EPS)).reshape(B, S, RET_WIDTH) * gn_gain.astype(jnp.float32)
    ya = jax.nn.silu(ga) * ya.astype(x.dtype)
    qb = rope(qb.reshape(B, S, WIN_Q_HEADS, WIN_HEAD_DIM), pos) * (WIN_HEAD_DIM ** -0.5)
    kb = rope(kb.reshape(B, S, WIN_KV_HEADS, WIN_HEAD_DIM), pos)
    vb = vb.reshape(B, S, WIN_KV_HEADS, WIN_HEAD_DIM)
    yb = window_attention_with_sink(qb, kb, vb, sink_logit)
    return jnp.concatenate([ya, yb], axis=-1) @ w_out


def fourier_mixer(x, w_out):
    B, S, D = x.shape
    xg = x.astype(jnp.float32).reshape(B, S, FOURIER_GROUPS, D // FOURIER_GROUPS)
    y = jnp.fft.fft2(xg, axes=(1, 3), norm='ortho').real.reshape(B, S, D)
    return y.astype(x.dtype) @ w_out


def hierarchical_moe(x, w_coarse, b_coarse, w_fine, b_fine, w_gate, w_up, w_down):
    B, S, D = x.shape
    xt = x.reshape(B * S, D)
    T = B * S
    coarse = (xt @ w_coarse + b_coarse).astype(jnp.float32)
    p_group, g_idx = lax.top_k(jax.nn.softmax(coarse, axis=-1), 1)
    fine = (xt @ w_fine + b_fine).astype(jnp.float32).reshape(T, N_GROUPS, EXPERTS_PER_GROUP)
    fine_g = jnp.take_along_axis(fine, g_idx[:, :, None], axis=1)[:, 0]
    top_val, top_idx = lax.top_k(fine_g, TOP_K)
    gate = jax.nn.softmax(top_val, axis=-1) * p_group
    expert_id = (g_idx * EXPERTS_PER_GROUP + top_idx).reshape(-1)
    order = jnp.argsort(expert_id)
    tok = order // TOP_K
    sizes = jnp.bincount(expert_id, length=N_EXPERTS).astype(jnp.int32)
    xs = xt[tok]
    hid = jax.nn.silu(lax.ragged_dot(xs, w_gate, sizes)) * lax.ragged_dot(xs, w_up, sizes)
    ys = lax.ragged_dot(hid, w_down, sizes) * gate.reshape(-1)[order][:, None].astype(x.dtype)
    out = jax.ops.segment_sum(ys, tok, num_segments=T)
    return out.reshape(B, S, D)


def setup_inputs(seed: int = 0) -> dict:
    key = jax.random.key(seed)
    ks = jax.random.split(key, 20)
    f32 = jnp.float32
    nrm = lambda k, shape: jax.random.normal(k, shape, f32)
    base_logit = jnp.log(2.0 ** (5.0 + jnp.arange(RET_HEADS, dtype=f32)) - 1.0)
    return {
        "x": nrm(ks[0], (BATCH, SEQ, D_MODEL)),
        "w_in_even": nrm(ks[1], (N_EVEN, D_MODEL, IN_EVEN)) * D_MODEL ** -0.5,
        "ret_decay_logit": base_logit[None, None, :] + 0.1 * nrm(ks[2], (N_EVEN, 2, RET_HEADS)),
        "ret_gn_gain": 1.0 + 0.02 * nrm(ks[3], (N_EVEN, RET_WIDTH)),
        "sink_logit": 0.5 * nrm(ks[4], (N_EVEN, WIN_Q_HEADS)),
        "w_out_even": nrm(ks[5], (N_EVEN, D_MODEL, D_MODEL)) * D_MODEL ** -0.5 * DEEPNORM_BETA,
        "w_out_fourier": nrm(ks[6], (N_ODD, D_MODEL, D_MODEL)) * D_MODEL ** -0.5 * DEEPNORM_BETA,
        "ln1_gain": 1.0 + 0.02 * nrm(ks[7], (DEPTH, D_MODEL)),
        "ln1_bias": 0.02 * nrm(ks[8], (DEPTH, D_MODEL)),
        "ln2_gain": 1.0 + 0.02 * nrm(ks[9], (DEPTH, D_MODEL)),
        "ln2_bias": 0.02 * nrm(ks[10], (DEPTH, D_MODEL)),
        "router_coarse_w": nrm(ks[11], (DEPTH, D_MODEL, N_GROUPS)) * D_MODEL ** -0.5,
        "router_coarse_b": 0.01 * nrm(ks[12], (DEPTH, N_GROUPS)),
        "router_fine_w": nrm(ks[13], (DEPTH, D_MODEL, N_EXPERTS)) * D_MODEL ** -0.5,
        "router_fine_b": 0.01 * nrm(ks[14], (DEPTH, N_EXPERTS)),
        "expert_w_gate": nrm(ks[15], (DEPTH, N_EXPERTS, D_MODEL, EXPERT_HIDDEN)) * D_MODEL ** -0.5,
        "expert_w_up": nrm(ks[16], (DEPTH, N_EXPERTS, D_MODEL, EXPERT_HIDDEN)) * D_MODEL ** -0.5,
        "expert_w_down": nrm(ks[17], (DEPTH, N_EXPERTS, EXPERT_HIDDEN, D_MODEL)) * EXPERT_HIDDEN ** -0.5 * DEEPNORM_BETA,
    }


def reference(x, w_in_even, ret_decay_logit, ret_gn_gain, sink_logit, w_out_even, w_out_fourier,
              ln1_gain, ln1_bias, ln2_gain, ln2_bias, router_coarse_w, router_coarse_b,
              router_fine_w, router_fine_b, expert_w_gate, expert_w_up, expert_w_down):
    pos = jnp.arange(x.shape[1], dtype=jnp.int32)
    for layer in range(DEPTH):
        if layer % 2 == 0:
            e = layer // 2
            mix = even_mixer(x, w_in_even[e], ret_decay_logit[e], ret_gn_gain[e], sink_logit[e],
                             w_out_even[e], pos)
        else:
            mix = fourier_mixer(x, w_out_fourier[layer // 2])
        x = layer_norm(DEEPNORM_ALPHA * x + mix, ln1_gain[layer], ln1_bias[layer])
        ffn = hierarchical_moe(x, router_coarse_w[layer], router_coarse_b[layer], router_fine_w[layer],
                               router_fine_b[layer], expert_w_gate[layer], expert_w_up[layer],
                               expert_w_down[layer])
        x = layer_norm(DEEPNORM_ALPHA * x + ffn, ln2_gain[layer], ln2_bias[layer])
    return x
```

```python
from contextlib import ExitStack
import numpy as np
import ml_dtypes
import concourse.bass as bass
import concourse.mybir as mybir
from concourse.bass_utils import run_bass_kernel_spmd

F32 = mybir.dt.float32; BF16 = mybir.dt.bfloat16; I32 = mybir.dt.int32
AF = mybir.ActivationFunctionType; ALU = mybir.AluOpType; AX = mybir.AxisListType

S = 4096; D = 1024; NT = S // 128; DEPTH = 4
NE = 32; HID = 512; CAP = 384
ALPHA = (2.0 * DEPTH) ** 0.25
LN_EPS = 1e-5; GN_EPS = 1e-6
IN_EVEN = 2816


class KB:
    def __init__(self, nc, n_dma_sems=40, same_engine_sync=True):
        self.nc = nc
        self.eng = {'pe': nc.tensor, 'act': nc.scalar, 'dve': nc.vector, 'pool': nc.gpsimd, 'sp': nc.sync}
        self.sem = {e: nc.alloc_semaphore(f"s_{e}") for e in ['pe', 'act', 'dve', 'pool']}
        self.seq = {e: 0 for e in self.sem}
        self.waited = {e: {} for e in self.eng}
        self.qpool = {}
        self.dma_sems = []
        for q, n in (('sp', 44), ('pool', 44), ('act', 8)):
            self.qpool[q] = [len(self.dma_sems), n, 0]
            self.dma_sems += [nc.alloc_semaphore(f"d{q}{i}") for i in range(n)]
        self.dma_cnt = [0] * len(self.dma_sems)
        self.lastw = {}
        self.readers = {}
        self.same = same_engine_sync
        self.nwaits = 0
        self.nops = 0

    def _wait(self, e, tok):
        semkey, sem, val = tok
        w = self.waited[e]
        if w.get(semkey, 0) >= val:
            return
        self.eng[e].wait_ge(sem, val)
        w[semkey] = val
        self.nwaits += 1

    def _deps(self, e, R, W):
        best = {}

        def add(t):
            if t[0] == e and (e == 'pe' or not self.same):
                return
            if t[0] not in best or best[t[0]][2] < t[2]:
                best[t[0]] = t
        for r in R:
            for t in self.lastw.get(r, {}).values():
                add(t)
        for w_ in W:
            for t in self.lastw.get(w_, {}).values():
                add(t)
            for t in self.readers.get(w_, {}).values():
                add(t)
        for t in best.values():
            self._wait(e, t)

    def _commit(self, tok, R, W):
        for r in R:
            d = self.readers.setdefault(r, {})
            if tok[0] not in d or d[tok[0]][2] < tok[2]:
                d[tok[0]] = tok
        for w_ in W:
            self.lastw.setdefault(w_, {})[tok[0]] = tok
            self.readers[w_] = {}

    def op(self, e, fn, R=(), W=()):
        self._deps(e, R, W)
        ins = fn(self.eng[e])
        self.seq[e] += 1
        ins.then_inc(self.sem[e], 1)
        self._commit((e, self.sem[e], self.seq[e]), R, W)
        self.nops += 1
        return ins

    def dma(self, q, out, in_, R=(), W=(), indirect=None, **kw):
        qp = self.qpool[q]
        i = qp[0] + qp[2]
        qp[2] = (qp[2] + 1) % qp[1]
        sem = self.dma_sems[i]
        if self.dma_cnt[i] > 0:
            self._wait(q, (('d', i), sem, 16 * self.dma_cnt[i]))
        self._deps(q, R, W)
        if indirect is None:
            ins = self.eng[q].dma_start(out=out, in_=in_, **kw)
        else:
            ins = self.eng[q].indirect_dma_start(out=out, in_=in_, **indirect, **kw)
        self.dma_cnt[i] += 1
        ins.then_inc(sem, 16)
        self._commit((('d', i), sem, 16 * self.dma_cnt[i]), R, W)
        self.nops += 1
        return ins

    def finish_all(self):
        toks = [(e, self.sem[e], self.seq[e]) for e in self.sem if self.seq[e] > 0]
        toks += [(('d', i), s, 16 * c) for i, (s, c) in enumerate(zip(self.dma_sems, self.dma_cnt)) if c > 0]
        for t in toks:
            self._wait('sp', t)

    def barrier(self):
        toks = [(e, self.sem[e], self.seq[e]) for e in self.sem if self.seq[e] > 0]
        toks += [(('d', i), s, 16 * c) for i, (s, c) in enumerate(zip(self.dma_sems, self.dma_cnt)) if c > 0]
        for e in self.eng:
            for t in toks:
                if t[0] != e or e != 'pe':
                    self._wait(e, t)
        self.lastw = {}
        self.readers = {}


CST_COLS = {}


def _cst_layout():
    off = 0
    for name, n in [('ident', 128), ('ltri', 128), ('ones', 128), ('ecap', 1024), ('rp', 128), ('rn', 128),
                    ('cp1', 128), ('cmc', 128), ('pcol', 4), ('mk', 384)]:
        CST_COLS[name] = (off, n)
        off += n
    return off


CST_N = _cst_layout()


def host_consts():
    c = np.zeros((128, CST_N), np.float32)
    p = np.arange(128)

    def put(name, arr):
        o, n = CST_COLS[name]
        c[:, o:o + n] = arr
    put('ident', np.eye(128))
    put('ltri', (p[:, None] < p[None, :]).astype(np.float32))
    put('ones', np.ones((128, 128)))
    put('ecap', np.tile((np.arange(NE) * CAP)[None, :], (128, NT)))
    dif = (p[None, :] - p[:, None]).astype(np.float32)
    put('rp', np.maximum(dif, 0.0))
    put('rn', np.maximum(-dif, 0.0))
    jj = np.arange(384)
    put('mk', np.where((jj[None, :] >= p[:, None]) & (jj[None, :] <= p[:, None] + 256), 0.0, -30000.0))
    put('cp1', np.tile((p + 1.0)[None, :], (128, 1)))
    put('cmc', np.tile((128.0 - p)[None, :], (128, 1)))
    pc = np.stack([127.0 - p, p.astype(np.float64), np.full(128, 128.0), np.zeros(128)], 1)
    put('pcol', pc)
    return c


class Ctx:
    uid = 0

    def sbt(self, name, shape, dt, **kw):
        Ctx.uid += 1
        return self.nc.sbuf_tensor(f"{name}_u{Ctx.uid}", shape, dt, **kw)

    def pst(self, name, shape, dt, **kw):
        Ctx.uid += 1
        return self.nc.psum_tensor(f"{name}_u{Ctx.uid}", shape, dt, **kw)


def cslice(cx, name):
    o, n = CST_COLS[name]
    return cx.cst[:, o:o + n]


def load_common(cx):
    nc, k = cx.nc, cx.k
    cx.cst = nc.alloc_sbuf_tensor("cst_sb", [128, CST_N], F32)
    k.dma('sp', cx.cst[:], cx.d_cst, W=['cst'])
    cx.ident_b = nc.alloc_sbuf_tensor("ident_b", [128, 128], BF16)
    k.op('dve', lambda e: e.tensor_copy(out=cx.ident_b[:], in_=cslice(cx, 'ident')), R=['cst'], W=['ident_b'])
    cx.bound_reg = nc.gpsimd.to_reg(NE * CAP - 1)
    cx.bound_reg_s = nc.gpsimd.to_reg(S - 1)
    cx.eps_ln = nc.alloc_sbuf_tensor("eps_ln", [128, 1], F32)
    k.op('dve', lambda e: e.memset(cx.eps_ln[:], LN_EPS), W=['eps_ln'])
    cx.eps_gn = nc.alloc_sbuf_tensor("eps_gn", [128, 1], F32)
    k.op('dve', lambda e: e.memset(cx.eps_gn[:], GN_EPS), W=['eps_gn'])


def layernorm_tile(cx, h, hkey, out, okey, gain, bias, gkeys, wk, sfx='', mid=None, defer=False, hout=None, houtkey=None):
    k = cx.k
    j = (int(sfx) % 4) if sfx else 0
    st, mv, sc = wk['st'][:, j, :], wk['mv'][:, j, :], wk['sc'][:, j, :]
    K_ = lambda n: n + str(j)
    if hout is None:
        hout, houtkey = h, hkey
    k.op('dve', lambda e: e.bn_stats(out=st[:, 0:6], in_=h[:, 0:512]), R=[hkey], W=[K_('ln_st')])
    k.op('dve', lambda e: e.bn_stats(out=st[:, 6:12], in_=h[:, 512:1024]), R=[hkey], W=[K_('ln_st')])
    k.op('dve', lambda e: e.bn_aggr(out=mv, in_=st), R=[K_('ln_st')], W=[K_('ln_mv')])
    k.op('act', lambda e: e.activation(out=sc[:, 0:1], in_=mv[:, 1:2], func=AF.Sqrt, bias=cx.eps_ln[:], scale=1.0),
         R=[K_('ln_mv'), 'eps_ln'], W=[K_('ln_sd')])
    if mid is not None:
        mid()
    k.op('dve', lambda e: e.reciprocal(out=sc[:, 1:2], in_=sc[:, 0:1]), R=[K_('ln_sd')], W=[K_('ln_rstd')])
    k.op('dve', lambda e: e.scalar_tensor_tensor(out=sc[:, 2:3], in0=mv[:, 0:1], scalar=-1.0, in1=sc[:, 1:2],
                                                 op0=ALU.mult, op1=ALU.mult), R=[K_('ln_mv'), K_('ln_rstd')], W=[K_('ln_nmr')])
    k.op('act', lambda e: e.activation(out=hout[:], in_=h[:], func=AF.Identity, bias=sc[:, 2:3], scale=sc[:, 1:2]),
         R=[hkey, K_('ln_rstd'), K_('ln_nmr')], W=[houtkey])

    def tail():
        k.op('dve', lambda e: e.tensor_tensor(out=hout[:], in0=hout[:], in1=gain, op=ALU.mult), R=[houtkey] + gkeys, W=[houtkey])
        k.op('pool', lambda e: e.tensor_tensor(out=out, in0=hout[:], in1=bias, op=ALU.add), R=[houtkey] + gkeys, W=[okey])
    if defer:
        return tail
    tail()
    return None


def layernorm_gen(cx, h, hkey, out, okey, gain, bias, gkeys, wk, slot):
    k = cx.k
    j = slot % 4
    st, mv, sc = wk['st'][:, j, :], wk['mv'][:, j, :], wk['sc'][:, j, :]
    K_ = lambda n: n + str(j)
    k.op('dve', lambda e: e.bn_stats(out=st[:, 0:6], in_=h[:, 0:512]), R=[hkey], W=[K_('ln_st')])
    yield
    k.op('dve', lambda e: e.bn_stats(out=st[:, 6:12], in_=h[:, 512:1024]), R=[hkey], W=[K_('ln_st')])
    yield
    k.op('dve', lambda e: e.bn_aggr(out=mv, in_=st), R=[K_('ln_st')], W=[K_('ln_mv')])
    yield
    k.op('act', lambda e: e.activation(out=sc[:, 0:1], in_=mv[:, 1:2], func=AF.Sqrt, bias=cx.eps_ln[:], scale=1.0),
         R=[K_('ln_mv'), 'eps_ln'], W=[K_('ln_sd')])
    yield
    k.op('dve', lambda e: e.reciprocal(out=sc[:, 1:2], in_=sc[:, 0:1]), R=[K_('ln_sd')], W=[K_('ln_rstd')])
    yield
    k.op('dve', lambda e: e.scalar_tensor_tensor(out=sc[:, 2:3], in0=mv[:, 0:1], scalar=-1.0, in1=sc[:, 1:2],
                                                 op0=ALU.mult, op1=ALU.mult), R=[K_('ln_mv'), K_('ln_rstd')], W=[K_('ln_nmr')])
    yield
    k.op('act', lambda e: e.activation(out=h[:], in_=h[:], func=AF.Identity, bias=sc[:, 2:3], scale=sc[:, 1:2]),
         R=[hkey, K_('ln_rstd'), K_('ln_nmr')], W=[hkey])
    yield
    k.op('dve', lambda e: e.tensor_tensor(out=h[:], in0=h[:], in1=gain, op=ALU.mult), R=[hkey] + gkeys, W=[hkey])
    yield
    k.op('pool', lambda e: e.tensor_tensor(out=out, in0=h[:], in1=bias, op=ALU.add), R=[hkey] + gkeys, W=[okey])
    yield


def interleave(gens):
    gens = list(gens)
    while gens:
        for g in list(gens):
            try:
                next(g)
            except StopIteration:
                gens.remove(g)


def ln_params(cx, es, which, L):
    nc, k = cx.nc, cx.k
    g = es.enter_context(cx.sbt(f"{which}_g_sb", [128, D], F32))
    b = es.enter_context(cx.sbt(f"{which}_b_sb", [128, D], F32))
    k.dma('sp', g[:], cx.din[which + '_gain'][L].partition_broadcast(128), W=[which + '_g'])
    k.dma('sp', b[:], cx.din[which + '_bias'][L].partition_broadcast(128), W=[which + '_b'])
    return g[:], b[:], [which + '_g', which + '_b']


def ln_work(cx, es, pfx):
    nc = cx.nc
    return {'st': es.enter_context(cx.sbt(pfx + "_st", [128, 4, 12], F32)),
            'mv': es.enter_context(cx.sbt(pfx + "_mv", [128, 4, 2], F32)),
            'sc': es.enter_context(cx.sbt(pfx + "_sc", [128, 4, 4], F32))}


def moe_phase(cx, L, X1, X2):
    nc, k = cx.nc, cx.k
    din = cx.din
    XE, YE = cx.XE, cx.YE
    NJ = CAP // 128
    NW = 4
    with ExitStack() as es:
        sb = lambda n, s, d: es.enter_context(cx.sbt(n, s, d))
        g_all = [sb("g1_all", [128, NT], F32), sb("g2_all", [128, NT], F32)]
        dsti = [sb("dsti0", [128, NT], I32), sb("dsti1", [128, NT], I32)]
        wg = [sb(f"wg{j}", [128, 8, HID], BF16) for j in range(NW - 1)]
        wu = [sb(f"wu{j}", [128, 8, HID], BF16) for j in range(NW - 1)]
        wd = [sb(f"wd{j}", [128, 4, D], BF16) for j in range(NW - 1)]

        def LW(ex):
            wb = ex % NW
            k.dma('pool', wg[wb][:], din['expert_w_gate'][L, ex].rearrange("(kc p) n -> p kc n", p=128), W=[f'wg{wb}'])
            k.dma('pool', wu[wb][:], din['expert_w_up'][L, ex].rearrange("(kc p) n -> p kc n", p=128), W=[f'wu{wb}'])
            k.dma('pool', wd[wb][:], din['expert_w_down'][L, ex].rearrange("(kc p) n -> p kc n", p=128), W=[f'wd{wb}'])
        LW(0)
        LW(1)
        LW(2)
        ident = cslice(cx, 'ident')

        with ExitStack() as esx:
            sb2 = lambda n, s, d: esx.enter_context(cx.sbt(n, s, d))
            xb_all = sb2("xb_all", [128, NT, D], BF16)
            lgc = sb2("lgc", [128, NT, 4], F32)
            lgf = sb2("lgf", [128, NT * NE], F32)
            wr = sb2("wr", [128, 8, 36], F32)
            rb = sb2("rb", [128, 36], F32)
            k.dma('sp', wr[:, :, 0:4], din['router_coarse_w'][L].rearrange("(kc p) n -> p kc n", p=128), W=['wr'])
            k.dma('sp', wr[:, :, 4:36], din['router_fine_w'][L].rearrange("(kc p) n -> p kc n", p=128), W=['wr'])
            k.dma('sp', rb[:, 0:4], din['router_coarse_b'][L].partition_broadcast(128), W=['rb'])
            k.dma('sp', rb[:, 4:36], din['router_fine_b'][L].partition_broadcast(128), W=['rb'])
            NB = 4
            xt = [sb2(f"mxt{j}", [128, D], F32) for j in range(NB)]
            xT32 = [sb2(f"mxT{j}", [128, 8, 128], F32) for j in range(2)]
            pst = [esx.enter_context(cx.pst(f"pst{j}", [128, 4, 128], F32)) for j in range(4)]
            psl = [esx.enter_context(cx.pst(f"psl{j}", [128, 36], F32)) for j in range(2)]
            GT = 8
            NG = NT // GT
            N1 = GT * NE
            oh1 = sb2("oh1", [128, N1], F32)
            oh2 = sb2("oh2", [128, N1], F32)
            sel = sb2("sel", [128, N1], F32)
            fm = sb2("fm", [128, N1], F32)
            fm2 = sb2("fm2", [128, N1], F32)
            posw = sb2("posw", [128, N1], F32)
            cum = [sb2("cumA", [128, N1], F32), sb2("cumB", [128, N1], F32)]
            tot = sb2("tot", [128, N1], F32)
            base = sb2("base", [128, NE], F32)
            sc4 = sb2("sc4", [128, GT, 4], F32)
            pen = sb2("pen", [128, GT, 4], F32)
            v = sb2("rv", [128, 8, GT], F32)
            dstf = sb2("dstf", [128, 2 * GT], F32)
            psw = esx.enter_context(cx.pst("psw", [128, N1], F32))
            pso = esx.enter_context(cx.pst("pso", [128, N1], F32))
            k.op('dve', lambda e: e.memset(base[:], 0.0), W=['base'])
            dv = lambda fn, R, W: k.op('dve', fn, R=R, W=W)

            def router_tile(i):
                b = i % NB
                b2 = i % 2
                X, XT = xt[b], xT32[b2]
                k.dma('sp', X[:], X1[i * 128:(i + 1) * 128, :], W=[f'mxt{b}'])
                k.op('act', lambda e: e.copy(out=xb_all[:, i, :], in_=X[:]), R=[f'mxt{b}'], W=[('xb', i)])
                for hh in range(2):
                    pi = (i % 2) * 2 + hh
                    P = pst[pi]
                    for j in range(4):
                        kc = hh * 4 + j
                        k.op('pe', lambda e: e.transpose(out=P[:, j, :], in_=X[:, kc * 128:(kc + 1) * 128], identity=ident),
                             R=[f'mxt{b}', 'cst'], W=[f'pst{pi}'])
                    if hh == 0:
                        k.op('act', lambda e: e.copy(out=XT[:, 0:4, :], in_=P[:]), R=[f'pst{pi}'], W=[f'mxT{b2}a'])
                    else:
                        k.op('dve', lambda e: e.tensor_copy(out=XT[:, 4:8, :], in_=P[:]), R=[f'pst{pi}'], W=[f'mxT{b2}b'])
                PL = psl[b2]
                for kc in range(8):
                    k.op('pe', lambda e: e.matmul(PL[:, :], lhsT=XT[:, kc, :], rhs=wr[:, kc, :], start=(kc == 0), stop=(kc == 7)),
                         R=[f'mxT{b2}a', f'mxT{b2}b', 'wr'], W=[f'psl{b2}'])
                k.op('dve', lambda e: e.tensor_tensor(out=lgc[:, i, :], in0=PL[:, 0:4], in1=rb[:, 0:4], op=ALU.add), R=[f'psl{b2}', 'rb'], W=['lgc'])
                k.op('dve', lambda e: e.tensor_tensor(out=lgf[:, i * NE:(i + 1) * NE], in0=PL[:, 4:36], in1=rb[:, 4:36], op=ALU.add),
                     R=[f'psl{b2}', 'rb'], W=['lgf'])

            def route_group(g):
                t0 = g * GT
                ts = slice(t0, t0 + GT)
                LC = lgc[:, ts, :]
                LF = lgf[:, t0 * NE:(t0 + GT) * NE]
                V = lambda j: v[:, j, :]
                b3 = lambda ap, n: ap.unsqueeze(2).broadcast_to([128, GT, n])
                dv(lambda e: e.tensor_reduce(out=V(0), in_=LC, axis=AX.X, op=ALU.max), ['lgc'], ['v0'])
                dv(lambda e: e.tensor_tensor(out=sc4[:], in0=LC, in1=b3(V(0), 4), op=ALU.subtract), ['lgc', 'v0'], ['sc4'])
                dv(lambda e: e.tensor_scalar(out=pen[:], in0=sc4[:], scalar1=0.0, scalar2=None, op0=ALU.is_equal), ['sc4'], ['pen'])
                dv(lambda e: e.tensor_scalar(out=pen[:], in0=pen[:], scalar1=1.0, scalar2=1e30, op0=ALU.subtract, op1=ALU.mult), ['pen'], ['pen'])
                k.op('act', lambda e: e.activation(out=sc4[:], in_=sc4[:], func=AF.Exp), R=['sc4'], W=['sc4'])
                dv(lambda e: e.tensor_reduce(out=V(1), in_=sc4[:], axis=AX.X, op=ALU.add), ['sc4'], ['v1'])
                dv(lambda e: e.reciprocal(out=V(2), in_=V(1)), ['v1'], ['v2'])
                dv(lambda e: e.tensor_tensor(out=fm[:].rearrange("p (a j) -> p a j", j=8), in0=LF.rearrange("p (a j) -> p a j", j=8),
                                             in1=pen[:].rearrange("p i g -> p (i g)").unsqueeze(2).broadcast_to([128, GT * 4, 8]), op=ALU.add),
                   ['lgf', 'pen'], ['fm'])
                fm3 = fm[:].rearrange("p (i e) -> p i e", e=NE)
                dv(lambda e: e.tensor_reduce(out=V(3), in_=fm3, axis=AX.X, op=ALU.max), ['fm'], ['v3'])
                dv(lambda e: e.tensor_tensor(out=oh1[:].rearrange("p (i e) -> p i e", e=NE), in0=fm3, in1=b3(V(3), NE), op=ALU.is_equal), ['fm', 'v3'], ['oh1'])
                dv(lambda e: e.scalar_tensor_tensor(out=fm2[:], in0=oh1[:], scalar=-1e30, in1=fm[:], op0=ALU.mult, op1=ALU.add), ['oh1', 'fm'], ['fm2'])
                fm23 = fm2[:].rearrange("p (i e) -> p i e", e=NE)
                dv(lambda e: e.tensor_reduce(out=V(4), in_=fm23, axis=AX.X, op=ALU.max), ['fm2'], ['v4'])
                dv(lambda e: e.tensor_tensor(out=oh2[:].rearrange("p (i e) -> p i e", e=NE), in0=fm23, in1=b3(V(4), NE), op=ALU.is_equal), ['fm2', 'v4'], ['oh2'])
                dv(lambda e: e.tensor_tensor(out=sel[:], in0=oh1[:], in1=oh2[:], op=ALU.add), ['oh1', 'oh2'], ['sel'])
                dv(lambda e: e.tensor_tensor(out=V(5), in0=V(4), in1=V(3), op=ALU.subtract), ['v3', 'v4'], ['v5'])
                k.op('act', lambda e: e.activation(out=V(6), in_=V(5), func=AF.Exp), R=['v5'], W=['v6'])
                dv(lambda e: e.tensor_scalar(out=V(7), in0=V(6), scalar1=1.0, scalar2=None, op0=ALU.add), ['v6'], ['v7'])
                dv(lambda e: e.reciprocal(out=V(7), in_=V(7)), ['v7'], ['v7'])
                dv(lambda e: e.tensor_tensor(out=g_all[0][:, ts], in0=V(2), in1=V(7), op=ALU.mult), ['v2', 'v7'], ['g1_all'])
                dv(lambda e: e.tensor_tensor(out=g_all[1][:, ts], in0=g_all[0][:, ts], in1=V(6), op=ALU.mult), ['g1_all', 'v6'], ['g2_all'])
                k.op('pe', lambda e: e.matmul(psw[:], lhsT=cslice(cx, 'ltri'), rhs=sel[:], start=True, stop=True), R=['sel', 'cst'], W=['psw'])
                k.op('pe', lambda e: e.matmul(pso[:], lhsT=cslice(cx, 'ones'), rhs=sel[:], start=True, stop=True), R=['sel', 'cst'], W=['pso'])
                k.op('act', lambda e: e.copy(out=cum[0][:], in_=pso[:]), R=['pso'], W=['cum0'])
                k.op('act', lambda e: e.copy(out=tot[:], in_=pso[:]), R=['pso'], W=['tot'])
                cur = 0
                sh = 1
                while sh < GT:
                    a_, b_ = cum[cur], cum[1 - cur]
                    dv(lambda e: e.tensor_copy(out=b_[:, 0:sh * NE], in_=a_[:, 0:sh * NE]), [f'cum{cur}'], [f'cum{1 - cur}'])
                    dv(lambda e: e.tensor_tensor(out=b_[:, sh * NE:], in0=a_[:, sh * NE:], in1=a_[:, 0:N1 - sh * NE], op=ALU.add),
                       [f'cum{cur}'], [f'cum{1 - cur}'])
                    cur = 1 - cur
                    sh *= 2
                inc = cum[cur]
                dv(lambda e: e.tensor_tensor(out=posw[:], in0=psw[:], in1=inc[:], op=ALU.add), ['psw', f'cum{cur}'], ['posw'])
                dv(lambda e: e.tensor_tensor(out=posw[:], in0=posw[:], in1=tot[:], op=ALU.subtract), ['posw', 'tot'], ['posw'])
                dv(lambda e: e.tensor_tensor(out=posw[:].rearrange("p (i e) -> p i e", e=NE), in0=posw[:].rearrange("p (i e) -> p i e", e=NE),
                                             in1=base[:].unsqueeze(1).broadcast_to([128, GT, NE]), op=ALU.add), ['posw', 'base'], ['posw'])
                dv(lambda e: e.tensor_tensor(out=base[:], in0=base[:], in1=inc[:, (GT - 1) * NE:GT * NE], op=ALU.add), ['base', f'cum{cur}'], ['base'])
                dv(lambda e: e.tensor_scalar(out=fm[:], in0=posw[:], scalar1=float(CAP), scalar2=1e6, op0=ALU.is_ge, op1=ALU.mult), ['posw', 'fm'], ['fm'])
                dv(lambda e: e.tensor_tensor(out=posw[:], in0=posw[:], in1=fm[:], op=ALU.add), ['posw', 'fm'], ['posw'])
                dv(lambda e: e.tensor_tensor(out=posw[:], in0=posw[:], in1=cslice(cx, 'ecap')[:, 0:N1], op=ALU.add), ['posw', 'cst'], ['posw'])
                for s_, oh in enumerate([oh1, oh2]):
                    dv(lambda e: e.tensor_tensor(out=fm2[:], in0=oh[:], in1=posw[:], op=ALU.mult), ['oh1', 'oh2', 'posw', 'fm2'], ['fm2'])
                    dv(lambda e: e.tensor_reduce(out=dstf[:, s_ * GT:(s_ + 1) * GT], in_=fm2[:].rearrange("p (i e) -> p i e", e=NE),
                                                 axis=AX.X, op=ALU.add), ['fm2'], ['dstf'])
                    dv(lambda e: e.tensor_copy(out=dsti[s_][:, ts], in_=dstf[:, s_ * GT:(s_ + 1) * GT]), ['dstf'], [f'dsti{s_}'])
                for i in range(t0, t0 + GT):
                    for s_ in range(2):
                        k.dma('pool', XE, xb_all[:, i, :], R=[('xb', i), f'dsti{s_}'], W=['XE'],
                              indirect=dict(out_offset=bass.IndirectOffsetOnAxis(ap=dsti[s_][:, i:i + 1], axis=0), in_offset=None,
                                            bounds_check=cx.bound_reg, oob_is_err=False))

            for g in range(NG):
                for i in range(g * GT, (g + 1) * GT):
                    router_tile(i)
                route_group(g)
        k.barrier()

        with ExitStack() as es2:
            sb2 = lambda n, s, d: es2.enter_context(cx.sbt(n, s, d))
            wg.append(sb2(f"wg{NW - 1}", [128, 8, HID], BF16))
            wu.append(sb2(f"wu{NW - 1}", [128, 8, HID], BF16))
            wd.append(sb2(f"wd{NW - 1}", [128, 4, D], BF16))
            xea = [sb2(f"xea{j}", [128, NJ, D], BF16) for j in range(2)]
            xeT = [sb2(f"xeT{j}", [128, 8, CAP], BF16) for j in range(2)]
            sg = [sb2(f"sg{j}", [128, CAP], F32) for j in range(2)]
            hid = [sb2(f"hid{j}", [128, 4, CAP], BF16) for j in range(2)]
            yo = [sb2(f"yo{j}", [128, NJ, D], F32) for j in range(2)]
            ptr = [es2.enter_context(cx.pst(f"ptr{j}", [128, 8, 128], BF16)) for j in range(2)]
            psg = [es2.enter_context(cx.pst(f"psg{j}", [128, CAP], F32)) for j in range(2)]
            psu = [es2.enter_context(cx.pst(f"psu{j}", [128, CAP], F32)) for j in range(2)]
            psy = [es2.enter_context(cx.pst(f"psy{j}", [128, 512], F32)) for j in range(2)]

            def LX(ex):
                k.dma('sp', xea[ex % 2][:], XE[ex * CAP:(ex + 1) * CAP, :].rearrange("(j p) d -> p j d", p=128), R=['XE'], W=[f'xea{ex % 2}'])

            def TGU(ex):
                wb = ex % NW
                XT = xeT[ex % 2]
                H = hid[ex % 2]
                XA = xea[ex % 2]
                for jt in range(NJ):
                    b2 = jt % 2
                    for kc in range(8):
                        k.op('pe', lambda e: e.transpose(out=ptr[b2][:, kc, :], in_=XA[:, jt, kc * 128:(kc + 1) * 128], identity=cx.ident_b[:]),
                             R=[f'xea{ex % 2}', 'ident_b'], W=[f'ptr{b2}'])
                    if b2 == 0:
                        k.op('act', lambda e: e.copy(out=XT[:, :, jt * 128:(jt + 1) * 128], in_=ptr[b2][:]), R=[f'ptr{b2}'], W=[f'xeT{ex % 2}'])
                    else:
                        k.op('dve', lambda e: e.tensor_copy(out=XT[:, :, jt * 128:(jt + 1) * 128], in_=ptr[b2][:]), R=[f'ptr{b2}'], W=[f'xeT{ex % 2}'])
                for hc in range(4):
                    b = hc % 2
                    for kc in range(8):
                        k.op('pe', lambda e: e.matmul(psg[b][:], lhsT=wg[wb][:, kc, hc * 128:(hc + 1) * 128], rhs=XT[:, kc, :],
                                                      start=(kc == 0), stop=(kc == 7)), R=[f'wg{wb}', f'xeT{ex % 2}'], W=[f'psg{b}'])
                    for kc in range(8):
                        k.op('pe', lambda e: e.matmul(psu[b][:], lhsT=wu[wb][:, kc, hc * 128:(hc + 1) * 128], rhs=XT[:, kc, :],
                                                      start=(kc == 0), stop=(kc == 7)), R=[f'wu{wb}', f'xeT{ex % 2}'], W=[f'psu{b}'])
                    k.op('act', lambda e: e.activation(out=sg[b][:], in_=psg[b][:], func=AF.Silu), R=[f'psg{b}'], W=[f'sg{b}'])
                    k.op('dve', lambda e: e.tensor_tensor(out=H[:, hc, :], in0=psu[b][:], in1=sg[b][:], op=ALU.mult),
                         R=[f'psu{b}', f'sg{b}'], W=[f'hid{ex % 2}'])

            def DN(ex):
                wb = ex % NW
                H = hid[ex % 2]
                YO = yo[ex % 2]
                for jt in range(NJ):
                    for hh in range(2):
                        for hc in range(4):
                            k.op('pe', lambda e: e.matmul(psy[hh][:], lhsT=H[:, hc, jt * 128:(jt + 1) * 128],
                                                          rhs=wd[wb][:, hc, hh * 512:(hh + 1) * 512], start=(hc == 0), stop=(hc == 3)),
                                 R=[f'hid{ex % 2}', f'wd{wb}'], W=[f'psy{hh}'])
                        if hh == 0:
                            k.op('act', lambda e: e.copy(out=YO[:, jt, 0:512], in_=psy[0][:]), R=['psy0'], W=[f'yo{ex % 2}'])
                        else:
                            k.op('dve', lambda e: e.tensor_copy(out=YO[:, jt, 512:1024], in_=psy[1][:]), R=['psy1'], W=[f'yo{ex % 2}'])
                k.dma('sp', YE[ex * CAP:(ex + 1) * CAP, :].rearrange("(j p) d -> p j d", p=128), YO[:], R=[f'yo{ex % 2}'], W=['YE'])

            LX(0)
            LX(1)
            TGU(0)
            for ex in range(NE):
                if ex + 3 < NE:
                    LW(ex + 3)
                if ex + 2 < NE and ex >= 0:
                    pass
                if ex + 1 < NE:
                    TGU(ex + 1)
                if ex + 2 < NE:
                    LX(ex + 2)
                DN(ex)
        k.barrier()

        with ExitStack() as es2:
            sb2 = lambda n, s, d: es2.enter_context(cx.sbt(n, s, d))
            NB = 5
            r1 = [sb2(f"r1_{j}", [128, D], F32) for j in range(NB)]
            r2 = [sb2(f"r2_{j}", [128, D], F32) for j in range(NB)]
            xt = [sb2(f"cxt{j}", [128, D], F32) for j in range(NB)]
            xo = [sb2(f"cxo{j}", [128, D], F32) for j in range(NB)]
            wk = ln_work(cx, es2, "ln2")
            gain, bias, gkeys = ln_params(cx, es2, 'ln2', cx.lnL if hasattr(cx, 'lnL') else L)
            def issue_loads(i):
                b = i % NB
                k.dma('sp', xt[b][:], X1[i * 128:(i + 1) * 128, :], W=[f'cxt{b}'])
                for s_, r in enumerate([r1[b], r2[b]]):
                    k.dma('pool', r[:], YE, R=['YE', f'dsti{s_}'], W=[f'r{s_}_{b}'],
                          indirect=dict(out_offset=None, in_offset=bass.IndirectOffsetOnAxis(ap=dsti[s_][:, i:i + 1], axis=0),
                                        bounds_check=cx.bound_reg, oob_is_err=False))
            for i in range(NB - 2):
                issue_loads(i)
            pend = [None]
            for i in range(NT):
                b = i % NB
                if i + NB - 2 < NT:
                    issue_loads(i + NB - 2)
                k.op('act', lambda e: e.activation(out=r2[b][:], in_=r2[b][:], func=AF.Identity, scale=g_all[1][:, i:i + 1]),
                     R=[f'r1_{b}', 'g2_all'], W=[f'r1_{b}'])
                k.op('dve', lambda e: e.scalar_tensor_tensor(out=r1[b][:], in0=r1[b][:], scalar=g_all[0][:, i:i + 1], in1=r2[b][:],
                                                             op0=ALU.mult, op1=ALU.add), R=[f'r0_{b}', f'r1_{b}', 'g1_all'], W=[f'r0_{b}'])
                k.op('dve', lambda e: e.scalar_tensor_tensor(out=xt[b][:], in0=xt[b][:], scalar=ALPHA, in1=r1[b][:],
                                                             op0=ALU.mult, op1=ALU.add), R=[f'cxt{b}', f'r0_{b}'], W=[f'cxt{b}'])
                tail = layernorm_tile(cx, xt[b], f'cxt{b}', xo[b][:], f'cxo{b}', gain, bias, gkeys, wk, sfx=str(b), mid=pend[0], defer=True)
                pend[0] = (lambda tail=tail, i=i, b=b: (tail(), k.dma('sp', X2[i * 128:(i + 1) * 128, :], xo[b][:], R=[f'cxo{b}'], W=[('X2', i)])))
            pend[0]()
    k.barrier()


def tile_to_xT(cx, X, xkey, xb, xbkey, ptr, ptrkey, xT, xTkey, cast_eng='pool', copy_eng='act'):
    k = cx.k
    if cast_eng == 'pool':
        k.op('pool', lambda e: e.tensor_copy(out=xb[:], in_=X), R=[xkey], W=[xbkey])
    else:
        k.op(cast_eng, lambda e: e.tensor_copy(out=xb[:], in_=X) if cast_eng == 'dve' else e.copy(out=xb[:], in_=X), R=[xkey], W=[xbkey])
    for kc in range(8):
        k.op('pe', lambda e: e.transpose(out=ptr[:, kc, :], in_=xb[:, kc * 128:(kc + 1) * 128], identity=cx.ident_b[:]),
             R=[xbkey, 'ident_b'], W=[ptrkey])
    if copy_eng == 'act':
        k.op('act', lambda e: e.copy(out=xT[:], in_=ptr[:]), R=[ptrkey], W=[xTkey])
    else:
        k.op('dve', lambda e: e.tensor_copy(out=xT[:], in_=ptr[:]), R=[ptrkey], W=[xTkey])


def host_dft_consts():
    a = np.arange(256)
    ang = 2.0 * np.pi * np.outer(a, a) / 256.0
    cc = (np.cos(ang) / 16.0).astype(np.float32)
    sc = (np.sin(ang) / 16.0).astype(np.float32)
    dftc = np.stack([cc.reshape(2, 128, 256), sc.reshape(2, 128, 256)], 2).transpose(1, 0, 2, 3).copy()
    j = np.arange(S // 2)
    kk = np.arange(S)
    jk = (np.outer(j, kk) % S).astype(np.float64)
    ang = 2.0 * np.pi * jk / S
    cs = (np.cos(ang) / 64.0)
    ss = (-np.sin(ang) / 64.0)
    NJ2 = NT // 2
    m = np.stack([cs, ss], 0).reshape(2, NJ2, 128, NT, 128)
    dfts = np.ascontiguousarray(m.transpose(3, 2, 0, 1, 4)).astype(ml_dtypes.bfloat16)
    cmid = (np.cos(np.pi * kk) / 64.0).reshape(1, S).astype(ml_dtypes.bfloat16)
    return dftc, dfts, cmid


def host_ridx():
    p = np.arange(128)[:, None]
    kt = np.arange(NT // 2 + 1)[None, :]
    return (S - kt * 128 - p).astype(np.int32)


def fnet_phase(cx, L, X, X1):
    nc, k = cx.nc, cx.k
    din = cx.din
    o = L // 2
    NH2 = NT // 2
    with ExitStack() as es:
        with ExitStack() as es1:
            sb1 = lambda n, s, d: es1.enter_context(cx.sbt(n, s, d, side='right'))
            wcs = [sb1("fWc", [128, 8, D], BF16), sb1("fWs", [128, 8, D], BF16)]
            with ExitStack() as es0:
                w32 = es0.enter_context(cx.sbt("fw32", [128, 8, D], F32))
                dc = es0.enter_context(cx.sbt("fdc", [128, 2, 2, 256], F32))
                pw = [es0.enter_context(cx.pst(f"fpw{j}", [128, 512], F32)) for j in range(2)]
                k.dma('sp', w32[:], din['w_out_fourier'][o].rearrange("(kc p) n -> p kc n", p=128), W=['fw32'])
                k.dma('sp', dc[:], cx.d_dftc, W=['fdc'])
                n = 0
                for t in range(2):
                    for fc in range(8):
                        g, ac = fc // 2, fc % 2
                        for hh in range(2):
                            P = pw[n % 2]
                            for a2 in range(2):
                                k.op('pe', lambda e: e.matmul(P[:], lhsT=dc[:, a2, t, ac * 128:(ac + 1) * 128],
                                                              rhs=w32[:, g * 2 + a2, hh * 512:(hh + 1) * 512], start=(a2 == 0), stop=(a2 == 1)),
                                     R=['fdc', 'fw32'], W=[f'fpw{n % 2}'])
                            if n % 2 == 0:
                                k.op('act', lambda e: e.copy(out=wcs[t][:, fc, hh * 512:(hh + 1) * 512], in_=P[:]), R=[f'fpw{n % 2}'], W=[f'fW{t}'])
                            else:
                                k.op('dve', lambda e: e.tensor_copy(out=wcs[t][:, fc, hh * 512:(hh + 1) * 512], in_=P[:]), R=[f'fpw{n % 2}'], W=[f'fW{t}'])
                            n += 1
                k.barrier()
            U_all = es.enter_context(cx.sbt("U_all", [128, NH2, D], BF16))
            V_all = es.enter_context(cx.sbt("V_all", [128, NH2, D], BF16))
            umid = es.enter_context(cx.sbt("umid", [1, D], BF16))
            xt = [[sb1(f"fxt{j}_{t}", [128, D], F32) for t in range(2)] for j in range(2)]
            xb = [[sb1(f"fxb{j}_{t}", [128, D], BF16) for t in range(2)] for j in range(2)]
            xTa = [sb1(f"fxTa{j}", [128, 8, 128], BF16) for j in range(2)]
            xTb = [sb1(f"fxTb{j}", [128, 8, 128], BF16) for j in range(2)]
            xTe = [sb1(f"fxTe{j}", [128, 8, 128], BF16) for j in range(2)]
            xTo = [sb1(f"fxTo{j}", [128, 8, 128], BF16) for j in range(2)]
            ptr = [[es1.enter_context(cx.pst(f"fptr{j}_{t}", [128, 8, 128], BF16)) for t in range(2)] for j in range(2)]
            pu = [es1.enter_context(cx.pst(f"fpu{j}", [128, 512], F32)) for j in range(4)]

            def stageA(i):
                b = i % 2
                for t, ti in enumerate([i, NT - 1 - i]):
                    k.dma('sp', xt[b][t][:], X[ti * 128:(ti + 1) * 128, :], W=[f'fxt{b}_{t}'])
                    tile_to_xT(cx, xt[b][t][:], f'fxt{b}_{t}', xb[b][t], f'fxb{b}_{t}', ptr[b][t], f'fptr{b}_{t}',
                               xTa[b] if t == 0 else xTb[b], f'fxT{"a" if t == 0 else "b"}{b}', cast_eng='act' if t == 0 else 'pool',
                               copy_eng='act' if t == 0 else 'dve')
                A_, B_ = xTa[b], xTb[b]
                E_, O_ = xTe[b], xTo[b]
                rkeys = [f'fxTa{b}', f'fxTb{b}']
                k.op('dve', lambda e: e.tensor_tensor(out=E_[:, :, 1:128], in0=A_[:, :, 1:128], in1=B_[:, :, 127:0:-1], op=ALU.add), R=rkeys, W=[f'fxTe{b}'])
                k.op('dve', lambda e: e.tensor_tensor(out=O_[:, :, 1:128], in0=A_[:, :, 1:128], in1=B_[:, :, 127:0:-1], op=ALU.subtract), R=rkeys, W=[f'fxTo{b}'])
                if i == 0:
                    k.op('dve', lambda e: e.tensor_copy(out=E_[:, :, 0:1], in_=A_[:, :, 0:1]), R=rkeys, W=[f'fxTe{b}'])
                    k.op('dve', lambda e: e.memset(O_[:, :, 0:1], 0.0), W=[f'fxTo{b}'])
                else:
                    Bp = xTb[1 - b]
                    k.op('dve', lambda e: e.tensor_tensor(out=E_[:, :, 0:1], in0=A_[:, :, 0:1], in1=Bp[:, :, 0:1], op=ALU.add), R=rkeys + [f'fxTb{1 - b}'], W=[f'fxTe{b}'])
                    k.op('dve', lambda e: e.tensor_tensor(out=O_[:, :, 0:1], in0=A_[:, :, 0:1], in1=Bp[:, :, 0:1], op=ALU.subtract), R=rkeys + [f'fxTb{1 - b}'], W=[f'fxTo{b}'])
            n = 0
            stageA(0)
            for i in range(NH2):
                b = i % 2
                if i + 1 < NH2:
                    stageA(i + 1)
                for t, (UV, XT) in enumerate([(U_all, xTe[b]), (V_all, xTo[b])]):
                    for hh in range(2):
                        P = pu[n % 4]
                        for kc in range(8):
                            k.op('pe', lambda e: e.matmul(P[:], lhsT=XT[:, kc, :], rhs=wcs[t][:, kc, hh * 512:(hh + 1) * 512],
                                                          start=(kc == 0), stop=(kc == 7)), R=[f'fxT{"e" if t == 0 else "o"}{b}', f'fW{t}'], W=[f'fpu{n % 4}'])
                        if n % 2 == 0:
                            k.op('act', lambda e: e.copy(out=UV[:, i, hh * 512:(hh + 1) * 512], in_=P[:]), R=[f'fpu{n % 4}'], W=[('UV', t, i)])
                        else:
                            k.op('dve', lambda e: e.tensor_copy(out=UV[:, i, hh * 512:(hh + 1) * 512], in_=P[:]), R=[f'fpu{n % 4}'], W=[('UV', t, i)])
                        n += 1
            bl = (NH2 - 1) % 2
            for hh in range(2):
                P = pu[n % 4]
                for kc in range(8):
                    k.op('pe', lambda e: e.matmul(P[0:1, :], lhsT=xTb[bl][:, kc, 0:1], rhs=wcs[0][:, kc, hh * 512:(hh + 1) * 512],
                                                  start=(kc == 0), stop=(kc == 7)), R=[f'fxTb{bl}', 'fW0'], W=[f'fpu{n % 4}'])
                k.op('act', lambda e: e.copy(out=umid[:, hh * 512:(hh + 1) * 512], in_=P[0:1, :]), R=[f'fpu{n % 4}'], W=['umid'])
                n += 1
        k.barrier()
        with ExitStack() as es2:
            sb2 = lambda n, s, d: es2.enter_context(cx.sbt(n, s, d, side='right'))
            cs = [sb2(f"fcs{j}", [128, 2, NH2, 128], BF16) for j in range(2)]
            cm = sb2("fcm", [1, S], BF16)
            k.dma('sp', cm[:], cx.d_cmid, W=['fcm'])
            xt = [sb2(f"gxt{j}", [128, D], F32) for j in range(2)]
            hh_ = [sb2(f"gh{j}", [128, D], F32) for j in range(2)]
            xo = [sb2(f"gxo{j}", [128, D], F32) for j in range(2)]
            pm = [es2.enter_context(cx.pst(f"fpm{j}", [128, 512], F32)) for j in range(4)]
            wk = ln_work(cx, es2, "ln1f")
            gain, bias, gkeys = ln_params(cx, es2, 'ln1', cx.lnL if hasattr(cx, 'lnL') else L)
            n = 0
            pend = [None]
            for kt in range(NT):
                b = kt % 2
                k.dma('sp', cs[b][:], cx.d_dfts[kt], W=[f'fcs{b}'])
                k.dma('sp', xt[b][:], X[kt * 128:(kt + 1) * 128, :], W=[f'gxt{b}'])
                for hh in range(2):
                    P = pm[n % 4]
                    for t, UV in enumerate([U_all, V_all]):
                        for jc in range(NH2):
                            k.op('pe', lambda e: e.matmul(P[:], lhsT=cs[b][:, t, jc, :], rhs=UV[:, jc, hh * 512:(hh + 1) * 512],
                                                          start=(t == 0 and jc == 0), stop=False),
                                 R=[f'fcs{b}', ('UV', t, jc)], W=[f'fpm{n % 4}'])
                    k.op('pe', lambda e: e.matmul(P[:], lhsT=cm[0:1, kt * 128:(kt + 1) * 128], rhs=umid[0:1, hh * 512:(hh + 1) * 512], start=False, stop=True),
                         R=['fcm', 'umid'], W=[f'fpm{n % 4}'])
                    k.op('dve', lambda e: e.scalar_tensor_tensor(out=hh_[b][:, hh * 512:(hh + 1) * 512], in0=xt[b][:, hh * 512:(hh + 1) * 512],
                                                                 scalar=ALPHA, in1=P[:], op0=ALU.mult, op1=ALU.add),
                         R=[f'gxt{b}', f'fpm{n % 4}'], W=[f'gh{b}'])
                    n += 1
                tail = layernorm_tile(cx, hh_[b], f'gh{b}', xo[b][:], f'gxo{b}', gain, bias, gkeys, wk, sfx=str(b), mid=pend[0], defer=True)
                pend[0] = (lambda tail=tail, kt=kt, b=b: (tail(), k.dma('pool', X1[kt * 128:(kt + 1) * 128, :], xo[b][:], R=[f'gxo{b}'], W=[('X1', kt)])))
            pend[0]()
    k.barrier()


QB_PERM = [half * 4 + c for c in range(4) for half in range(2)]


def host_even_layout(w_in_even, w_out_even):
    wi = np.array(w_in_even, copy=True)
    wo = np.array(w_out_even, copy=True)
    for p, hq in enumerate(QB_PERM):
        wi[:, :, 2048 + p * 64:2048 + (p + 1) * 64] = w_in_even[:, :, 2048 + hq * 64:2048 + (hq + 1) * 64]
        wo[:, 512 + p * 64:512 + (p + 1) * 64, :] = w_out_even[:, 512 + hq * 64:512 + (hq + 1) * 64, :]
    return wi, wo


def host_rope_consts():
    pos = np.arange(S, dtype=np.float64)
    out = np.zeros((S, 384), np.float32)

    def tab(half):
        inv = 10000.0 ** (-np.arange(half, dtype=np.float32) / half)
        ang = pos.astype(np.float32)[:, None] * inv[None, :]
        return np.cos(ang).astype(np.float32), np.sin(ang).astype(np.float32)
    ca, sa = tab(64)
    cb, sb_ = tab(32)
    out[:, 0:64] = ca; out[:, 64:128] = sa
    out[:, 128:192] = ca * np.float32(128 ** -0.5); out[:, 192:256] = sa * np.float32(128 ** -0.5)
    out[:, 256:288] = cb * np.float32(0.125); out[:, 288:320] = sb_ * np.float32(0.125)
    out[:, 320:352] = cb; out[:, 352:384] = sb_
    return out.reshape(NT, 128, 384)


def rope_tm(cx, eng, src, skey, H, hd, cos, sin, ckey, dst, dkey, t1, t2, tkey):
    k = cx.k
    sv = src.rearrange("p (h t d) -> p h t d", h=H, t=2)
    dv = dst.rearrange("p (h t d) -> p h t d", h=H, t=2)
    x1, x2 = sv[:, :, 0, :], sv[:, :, 1, :]
    cb = cos.unsqueeze(1).broadcast_to([128, H, hd])
    sb_ = sin.unsqueeze(1).broadcast_to([128, H, hd])
    a = t1.rearrange("p (h d) -> p h d", h=H)
    b = t2.rearrange("p (h d) -> p h d", h=H)
    tt = lambda o, i0, i1, op, R, W: k.op(eng, lambda e: e.tensor_tensor(out=o, in0=i0, in1=i1, op=op), R=R, W=W)
    tt(a, x1, cb, ALU.mult, [skey, ckey], [tkey + 'a'])
    tt(b, x2, sb_, ALU.mult, [skey, ckey], [tkey + 'b'])
    tt(dv[:, :, 0, :], a, b, ALU.subtract, [tkey + 'a', tkey + 'b'], [dkey])
    tt(a, x1, sb_, ALU.mult, [skey, ckey], [tkey + 'a'])
    tt(b, x2, cb, ALU.mult, [skey, ckey], [tkey + 'b'])
    tt(dv[:, :, 1, :], a, b, ALU.add, [tkey + 'a', tkey + 'b'], [dkey])


def even_phase(cx, L, X, X1):
    nc, k, din = cx.nc, cx.k, cx.din
    ev = L // 2
    SGA, YA = cx.SGA, cx.YA
    NH = 16

    def inproj_pass(w, wkey, ncols, blocks, epilogue, epilogue2, es1, sbr):
        xt = [sbr(f"ext{j}", [128, D], F32) for j in range(2)]
        xb = [sbr(f"exb{j}", [128, D], BF16) for j in range(2)]
        xT = [sbr(f"exT{j}", [128, 8, 128], BF16) for j in range(2)]
        rpt = [sbr(f"erp{j}", [128, 384], F32) for j in range(2)]
        ptr = [es1.enter_context(cx.pst(f"eptr{j}", [128, 8, 128], BF16)) for j in range(2)]
        pp = [es1.enter_context(cx.pst(f"epp{j}", [128, 512], F32)) for j in range(len(blocks))]

        def stageA(i):
            b = i % 2
            k.dma('sp', xt[b][:], X[i * 128:(i + 1) * 128, :], W=[f'ext{b}'])
            k.dma('sp', rpt[b][:], cx.d_rope[i], W=[f'erp{b}'])
            tile_to_xT(cx, xt[b][:], f'ext{b}', xb[b], f'exb{b}', ptr[b], f'eptr{b}', xT[b], f'exT{b}', cast_eng='act', copy_eng='dve')
        stageA(0)
        for i in range(NT):
            b = i % 2
            if i + 1 < NT:
                stageA(i + 1)
            for blk, (c0, cn) in enumerate(blocks):
                P = pp[blk]
                for kc in range(8):
                    k.op('pe', lambda e: e.matmul(P[:, 0:cn], lhsT=xT[b][:, kc, :], rhs=w[:, kc, c0:c0 + cn],
                                                  start=(kc == 0), stop=(kc == 7)), R=[f'exT{b}', wkey], W=[f'epp{blk}'])
                epilogue(i, b, blk, P, rpt[b], f'erp{b}')
            if i > 0:
                epilogue2(i - 1)
        epilogue2(NT - 1)

    with ExitStack() as esA:
        sbl = lambda n, s, d: esA.enter_context(cx.sbt(n, s, d))
        qaT = sbl("qaT", [128, 4, S], BF16)
        kaT = sbl("kaT", [128, 4, S], BF16)
        va_all = sbl("va_all", [128, NT, 512], BF16)
        with ExitStack() as es1:
            sbr = lambda n, s, d: es1.enter_context(cx.sbt(n, s, d, side='right'))
            w = sbr("ewA", [128, 8, 2048], BF16)
            for cb in range(4):
                k.dma('pool', w[:, :, cb * 512:(cb + 1) * 512],
                      din['w_in_even'][ev][:, cb * 512:(cb + 1) * 512].rearrange("(kc p) n -> p kc n", p=128), W=['ewA'])
            hs = [sbr(f"ehs{j}", [128, 512], F32) for j in range(2)]
            qr = [[sbr(f"eqr{j}_{t}", [128, 512], BF16) for t in range(2)] for j in range(2)]
            tq = [sbr(f"etq{j}", [128, 256], F32) for j in range(4)]
            sga = [sbr(f"esga{j}", [128, 512], BF16) for j in range(2)]
            ptq = [es1.enter_context(cx.pst(f"eptq{j}", [128, 4, 128], BF16)) for j in range(2)]

            def epiA2(i):
                t = i % 2
                for blk in range(2):
                    for h in range(4):
                        k.op('pe', lambda e: e.transpose(out=ptq[blk][:, h, :], in_=qr[blk][t][:, h * 128:(h + 1) * 128], identity=cx.ident_b[:]),
                             R=[f'eqr{blk}_{t}', 'ident_b'], W=[f'eptq{blk}'])
                    if blk == 0:
                        k.op('act', lambda e: e.copy(out=qaT[:, :, i * 128:(i + 1) * 128], in_=ptq[blk][:]), R=[f'eptq{blk}'], W=['qaT'])
                    else:
                        k.op('dve', lambda e: e.tensor_copy(out=kaT[:, :, i * 128:(i + 1) * 128], in_=ptq[blk][:]), R=[f'eptq{blk}'], W=['kaT'])

            def epiA(i, b, blk, P, rp, rpkey):
                if blk < 2:
                    t = i % 2
                    k.op('act', lambda e: e.copy(out=hs[blk][:], in_=P[:]), R=[f'epp{blk}'], W=[f'ehs{blk}'])
                    eng = 'dve' if blk == 0 else 'pool'
                    co = 0 if blk == 0 else 128
                    rope_tm(cx, eng, hs[blk][:], f'ehs{blk}', 4, 64, rp[:, co:co + 64], rp[:, co + 64:co + 128],
                            rpkey, qr[blk][t][:], f'eqr{blk}_{t}', tq[2 * blk][:], tq[2 * blk + 1][:], f'etq{blk}')
                elif blk == 2:
                    k.op('dve', lambda e: e.tensor_copy(out=va_all[:, i, :], in_=P[:]), R=[f'epp{blk}'], W=['va_all'])
                else:
                    k.op('act', lambda e: e.activation(out=sga[b][:], in_=P[:], func=AF.Silu), R=[f'epp{blk}'], W=[f'esga{b}'])
                    k.dma('pool', SGA[i * 128:(i + 1) * 128, :], sga[b][:], R=[f'esga{b}'], W=['SGA'])
            inproj_pass(w, 'ewA', 2048, [(0, 512), (512, 512), (1024, 512), (1536, 512)], epiA, epiA2, es1, sbr)
        k.barrier()

        with ExitStack() as es1:
            sbr = lambda n, s, d: es1.enter_context(cx.sbt(n, s, d, side='right'))
            lgt = sbr("rlg", [128, 8], F32)
            gng = sbr("rgng", [128, 512], F32)
            k.dma('sp', lgt[:], din['ret_decay_logit'][ev].rearrange("a h -> (a h)").partition_broadcast(128), W=['rlg'])
            k.dma('sp', gng[:], din['ret_gn_gain'][ev].partition_broadcast(128), W=['rgng'])
            k.op('act', lambda e: e.activation(out=lgt[:], in_=lgt[:], func=AF.Exp, scale=-1.0), R=['rlg'], W=['rlg'])
            k.op('dve', lambda e: e.tensor_scalar(out=lgt[:], in0=lgt[:], scalar1=1.0, scalar2=None, op0=ALU.add), R=['rlg'], W=['rlg'])
            k.op('act', lambda e: e.activation(out=lgt[:], in_=lgt[:], func=AF.Ln), R=['rlg'], W=['rlg'])
            k.op('dve', lambda e: e.tensor_scalar(out=lgt[:], in0=lgt[:], scalar1=-1.0, scalar2=None, op0=ALU.mult), R=['rlg'], W=['rlg'])
            DT = sbr("rDT", [128, 128], F32)
            XI = [sbr("rXIF", [128, 128], BF16), sbr("rXIB", [128, 128], BF16)]
            zc = sbr("rzc", [128, 4], F32)
            arg = sbr("rarg", [128, 128], F32)
            qs = [sbr("rqf", [128, S], BF16), sbr("rqb", [128, S], BF16)]
            Vz = [sbr("rVzf", [128, NT, 128], BF16), sbr("rVzb", [128, NT, 128], BF16)]
            ktm = sbr("rktm", [128, NT, 128], BF16)
            Rb_all = sbr("rRb_all", [128, NT, 128], BF16)
            R32 = [sbr("rRf32", [128, 128], F32), sbr("rRb32", [128, 128], F32)]
            Rfb = [sbr(f"rRfb{j}", [128, 128], BF16) for j in range(2)]
            Pm = [sbr(f"rPm{j}", [128, 128], BF16) for j in range(2)]
            Yraw = [sbr(f"rYraw{j}", [128, NH, 128], F32) for j in range(2)]
            Ysq = sbr("rYsq", [128, NH, 128], F32)
            sgs = [sbr(f"rsgs{j}", [128, NH, 128], BF16) for j in range(2)]
            yout = [sbr(f"ryout{j}", [128, NH, 128], BF16) for j in range(2)]
            gv = sbr("rgv", [128, 8, NH], F32)
            pk = [es1.enter_context(cx.pst(f"rpk{j}", [128, 8, 128], BF16)) for j in range(2)]
            pkv = [es1.enter_context(cx.pst(f"rpkv{j}", [128, 128], F32)) for j in range(2)]
            pst = [es1.enter_context(cx.pst(f"rpst{j}", [128, 128], F32)) for j in range(2)]
            py = [es1.enter_context(cx.pst(f"rpy{j}", [128, 128], F32)) for j in range(2)]
            pcol = cslice(cx, 'pcol')
            nslab = 0
            for h in range(4):
                lgf, lgb = lgt[:, h:h + 1], lgt[:, 4 + h:5 + h]
                k.op('dve', lambda e: e.tensor_scalar(out=arg[:], in0=cslice(cx, 'rp'), scalar1=lgf, scalar2=None, op0=ALU.mult),
                     R=['cst', 'rlg'], W=['rarg'])
                k.op('dve', lambda e: e.scalar_tensor_tensor(out=arg[:], in0=cslice(cx, 'rn'), scalar=lgb, in1=arg[:], op0=ALU.mult, op1=ALU.add),
                     R=['cst', 'rlg', 'rarg'], W=['rarg'])
                k.op('act', lambda e: e.activation(out=DT[:], in_=arg[:], func=AF.Exp), R=['rarg'], W=['rDT'])
                k.op('act', lambda e: e.activation(out=XI[0][:], in_=cslice(cx, 'cp1'), func=AF.Exp, scale=lgf), R=['cst', 'rlg'], W=['rXI0'])
                k.op('act', lambda e: e.activation(out=XI[1][:], in_=cslice(cx, 'cmc'), func=AF.Exp, scale=lgb), R=['cst', 'rlg'], W=['rXI1'])
                k.op('act', lambda e: e.activation(out=zc[:, 0:1], in_=pcol[:, 0:1], func=AF.Exp, scale=lgf), R=['cst', 'rlg'], W=['rzc'])
                k.op('act', lambda e: e.activation(out=zc[:, 1:2], in_=pcol[:, 1:2], func=AF.Exp, scale=lgb), R=['cst', 'rlg'], W=['rzc'])
                k.op('act', lambda e: e.activation(out=zc[:, 2:3], in_=pcol[:, 2:3], func=AF.Exp, scale=lgf), R=['cst', 'rlg'], W=['rzc'])
                k.op('act', lambda e: e.activation(out=zc[:, 3:4], in_=pcol[:, 2:3], func=AF.Exp, scale=lgb), R=['cst', 'rlg'], W=['rzc'])
                for d_ in range(2):
                    k.op('dve', lambda e: e.tensor_tensor(out=qs[d_][:].rearrange("p (n c) -> p n c", c=128),
                                                          in0=qaT[:, h, :].rearrange("p (n c) -> p n c", c=128),
                                                          in1=XI[d_][:].unsqueeze(1).broadcast_to([128, NT, 128]), op=ALU.mult),
                         R=['qaT', f'rXI{d_}'], W=[f'rqs{d_}'])
                    k.op('act', lambda e: e.activation(out=Vz[d_][:], in_=va_all[:, :, h * 128:(h + 1) * 128], func=AF.Identity, scale=zc[:, d_:d_ + 1]),
                         R=['va_all', 'rzc'], W=[f'rVz{d_}'])
                for g8 in range(4):
                    P = pk[g8 % 2]
                    for j in range(8):
                        n = g8 * 8 + j
                        k.op('pe', lambda e: e.transpose(out=P[:, j, :], in_=kaT[:, h, n * 128:(n + 1) * 128], identity=cx.ident_b[:]),
                             R=['kaT', 'ident_b'], W=[f'rpk{g8 % 2}'])
                    k.op('dve', lambda e: e.tensor_copy(out=ktm[:, g8 * 8:(g8 + 1) * 8, :], in_=P[:]), R=[f'rpk{g8 % 2}'], W=['rktm'])
                k.op('dve', lambda e: e.memset(R32[1][:], 0.0), W=['rR32_1'])
                k.op('dve', lambda e: e.memset(R32[0][:], 0.0), W=['rR32_0'])
                nkv = 0
                for n in range(NT - 1, 0, -1):
                    P = pkv[nkv % 2]
                    k.op('pe', lambda e: e.matmul(P[:], lhsT=ktm[:, n, :], rhs=Vz[1][:, n, :], start=True, stop=True),
                         R=['rktm', 'rVz1'], W=[f'rpkv{nkv % 2}'])
                    k.op('dve', lambda e: e.scalar_tensor_tensor(out=R32[1][:], in0=R32[1][:], scalar=zc[:, 3:4], in1=P[:], op0=ALU.mult, op1=ALU.add),
                         R=['rR32_1', 'rzc', f'rpkv{nkv % 2}'], W=['rR32_1'])
                    k.op('act', lambda e: e.copy(out=Rb_all[:, n - 1, :], in_=R32[1][:]), R=['rR32_1'], W=['rRb_all'])
                    nkv += 1
                def scores(n):
                    b = n % 2
                    cs_ = slice(n * 128, (n + 1) * 128)
                    k.op('pe', lambda e: e.matmul(pst[b][:], lhsT=kaT[:, h, cs_], rhs=qaT[:, h, cs_], start=True, stop=True),
                         R=['kaT', 'qaT'], W=[f'rpst{b}'])
                    k.op('dve', lambda e: e.tensor_tensor(out=Pm[b][:], in0=pst[b][:], in1=DT[:], op=ALU.mult), R=[f'rpst{b}', 'rDT'], W=[f'rPm{b}'])
                scores(0)
                for n in range(NT):
                    b = n % 2
                    sl = nslab % 2
                    cs_ = slice(n * 128, (n + 1) * 128)
                    if n % NH == 0:
                        k.dma('sp', sgs[sl][:], SGA[n * 128:(n + NH) * 128, h * 128:(h + 1) * 128].rearrange("(j p) c -> p j c", p=128),
                              R=['SGA'], W=[f'rsgs{sl}'])
                    if n + 1 < NT:
                        scores(n + 1)
                    last = 'intra'
                    if n < NT - 1:
                        last = 'bwd'
                    elif n > 0:
                        last = 'fwd'
                    k.op('pe', lambda e: e.matmul(py[b][:], lhsT=Pm[b][:], rhs=va_all[:, n, h * 128:(h + 1) * 128], start=True, stop=(last == 'intra')),
                         R=[f'rPm{b}', 'va_all'], W=[f'rpy{b}'])
                    if n > 0:
                        k.op('pe', lambda e: e.matmul(py[b][:], lhsT=qs[0][:, cs_], rhs=Rfb[(n - 1) % 2][:], start=False, stop=(last == 'fwd')),
                             R=['rqs0', f'rRfb{(n - 1) % 2}'], W=[f'rpy{b}'])
                    if n < NT - 1:
                        k.op('pe', lambda e: e.matmul(py[b][:], lhsT=qs[1][:, cs_], rhs=Rb_all[:, n, :], start=False, stop=True),
                             R=['rqs1', 'rRb_all'], W=[f'rpy{b}'])
                    k.op('act', lambda e: e.copy(out=Yraw[sl][:, n % NH, :], in_=py[b][:]), R=[f'rpy{b}'], W=[f'rYraw{sl}'])
                    if n < NT - 1:
                        P = pkv[nkv % 2]
                        k.op('pe', lambda e: e.matmul(P[:], lhsT=ktm[:, n, :], rhs=Vz[0][:, n, :], start=True, stop=True),
                             R=['rktm', 'rVz0'], W=[f'rpkv{nkv % 2}'])
                        k.op('dve', lambda e: e.scalar_tensor_tensor(out=R32[0][:], in0=R32[0][:], scalar=zc[:, 2:3], in1=P[:], op0=ALU.mult, op1=ALU.add),
                             R=['rR32_0', 'rzc', f'rpkv{nkv % 2}'], W=['rR32_0'])
                        k.op('act', lambda e: e.copy(out=Rfb[n % 2][:], in_=R32[0][:]), R=['rR32_0'], W=[f'rRfb{n % 2}'])
                        nkv += 1
                    if n % NH == NH - 1:
                        Y = Yraw[sl]
                        G = lambda j: gv[:, j, :]
                        bc = lambda ap: ap.unsqueeze(2).broadcast_to([128, NH, 128])
                        yk = f'rYraw{sl}'
                        k.op('dve', lambda e: e.tensor_reduce(out=G(0), in_=Y[:], axis=AX.X, op=ALU.add), R=[yk], W=['rg0'])
                        k.op('act', lambda e: e.activation(out=Ysq[:], in_=Y[:], func=AF.Square), R=[yk], W=['rYsq'])
                        k.op('dve', lambda e: e.tensor_reduce(out=G(1), in_=Ysq[:], axis=AX.X, op=ALU.add), R=['rYsq'], W=['rg1'])
                        k.op('dve', lambda e: e.tensor_scalar(out=G(2), in0=G(0), scalar1=1.0 / 128, scalar2=None, op0=ALU.mult), R=['rg0'], W=['rg2'])
                        k.op('dve', lambda e: e.tensor_tensor(out=G(3), in0=G(2), in1=G(2), op=ALU.mult), R=['rg2'], W=['rg3'])
                        k.op('dve', lambda e: e.scalar_tensor_tensor(out=G(4), in0=G(1), scalar=1.0 / 128, in1=G(3), op0=ALU.mult, op1=ALU.subtract),
                             R=['rg1', 'rg3'], W=['rg4'])
                        k.op('act', lambda e: e.activation(out=G(5), in_=G(4), func=AF.Sqrt, bias=cx.eps_gn[:], scale=1.0), R=['rg4', 'eps_gn'], W=['rg5'])
                        k.op('dve', lambda e: e.reciprocal(out=G(6), in_=G(5)), R=['rg5'], W=['rg6'])
                        k.op('dve', lambda e: e.tensor_tensor(out=Y[:], in0=Y[:], in1=bc(G(2)), op=ALU.subtract), R=[yk, 'rg2'], W=[yk])
                        k.op('dve', lambda e: e.tensor_tensor(out=Y[:], in0=Y[:], in1=bc(G(6)), op=ALU.mult), R=[yk, 'rg6'], W=[yk])
                        k.op('dve', lambda e: e.tensor_tensor(out=Y[:], in0=Y[:], in1=gng[:, h * 128:(h + 1) * 128].unsqueeze(1).broadcast_to([128, NH, 128]),
                                                              op=ALU.mult), R=[yk, 'rgng'], W=[yk])
                        k.op('dve', lambda e: e.tensor_tensor(out=yout[sl][:], in0=Y[:], in1=sgs[sl][:], op=ALU.mult), R=[yk, f'rsgs{sl}'], W=[f'ryout{sl}'])
                        n0 = n - (NH - 1)
                        k.dma('sp', YA[n0 * 128:(n0 + NH) * 128, h * 128:(h + 1) * 128].rearrange("(j p) c -> p j c", p=128), yout[sl][:],
                              R=[f'ryout{sl}'], W=['YA'])
                        nslab += 1
        k.barrier()

    with ExitStack() as esB:
        sbl = lambda n, s, d: esB.enter_context(cx.sbt(n, s, d))
        yb_all = sbl("yb_all", [128, NT, 512], BF16)
        with ExitStack() as esB2:
            sbl2 = lambda n, s, d: esB2.enter_context(cx.sbt(n, s, d))
            qbT = sbl2("qbT", [128, 4, S], BF16)
            kbT = sbl2("kbT", [128, S], BF16)
            vb_all = sbl2("vb_all", [128, NT, 128], BF16)
            with ExitStack() as es1:
                sbr = lambda n, s, d: es1.enter_context(cx.sbt(n, s, d, side='right'))
                w = sbr("ewB", [128, 8, 768], BF16)
                for cb in range(2):
                    k.dma('pool', w[:, :, cb * 384:(cb + 1) * 384],
                          din['w_in_even'][ev][:, 2048 + cb * 384:2048 + (cb + 1) * 384].rearrange("(kc p) n -> p kc n", p=128), W=['ewB'])
                hs = [sbr(f"ehs{j}", [128, 512], F32) for j in range(2)]
                qr = [[sbr(f"eqr{j}_{t}", [128, 512], BF16) for t in range(2)] for j in range(2)]
                tq = [sbr(f"etq{j}", [128, 256], F32) for j in range(4)]
                ptq = [es1.enter_context(cx.pst(f"eptq{j}", [128, 4, 128], BF16)) for j in range(2)]

                def epiB2(i):
                    t = i % 2
                    for c in range(4):
                        k.op('pe', lambda e: e.transpose(out=ptq[0][:, c, :], in_=qr[0][t][:, c * 128:(c + 1) * 128], identity=cx.ident_b[:]),
                             R=[f'eqr0_{t}', 'ident_b'], W=['eptq0'])
                    k.op('act', lambda e: e.copy(out=qbT[:, :, i * 128:(i + 1) * 128], in_=ptq[0][:]), R=['eptq0'], W=['qbT'])
                    k.op('pe', lambda e: e.transpose(out=ptq[1][:, 0, :], in_=qr[1][t][:, 0:128], identity=cx.ident_b[:]),
                         R=[f'eqr1_{t}', 'ident_b'], W=['eptq1'])
                    k.op('dve', lambda e: e.tensor_copy(out=kbT[:, i * 128:(i + 1) * 128], in_=ptq[1][:, 0, :]), R=['eptq1'], W=['kbT'])

                def epiB(i, b, blk, P, rp, rpkey):
                    t = i % 2
                    cn = 512 if blk == 0 else 256
                    k.op('act', lambda e: e.copy(out=hs[blk][:, 0:cn], in_=P[:, 0:cn]), R=[f'epp{blk}'], W=[f'ehs{blk}'])
                    if blk == 0:
                        rope_tm(cx, 'dve', hs[0][:], 'ehs0', 8, 32, rp[:, 256:288], rp[:, 288:320], rpkey,
                                qr[0][t][:], f'eqr0_{t}', tq[0][:], tq[1][:], 'etq0')
                    else:
                        rope_tm(cx, 'pool', hs[1][:, 0:128], 'ehs1', 2, 32, rp[:, 320:352], rp[:, 352:384], rpkey,
                                qr[1][t][:, 0:128], f'eqr1_{t}', tq[2][:, 0:64], tq[3][:, 0:64], 'etq1')
                        k.op('dve', lambda e: e.tensor_copy(out=vb_all[:, i, :], in_=hs[1][:, 128:256]), R=['ehs1'], W=['vb_all'])
                inproj_pass(w, 'ewB', 768, [(0, 512), (512, 256)], epiB, epiB2, es1, sbr)
            k.barrier()

            with ExitStack() as es1:
                sbr = lambda n, s, d: es1.enter_context(cx.sbt(n, s, d, side='right'))
                sk = sbr("ask", [128, 8], F32)
                k.dma('sp', sk[:], din['sink_logit'][ev].partition_broadcast(128), W=['ask'])
                sm4 = [sbr(f"asm{j}", [128, 4, 384], F32) for j in range(2)]
                pb4 = [sbr(f"apb{j}", [128, 4, 384], BF16) for j in range(2)]
                pT4 = [sbr(f"apT{j}", [128, 12, 128], BF16) for j in range(2)]
                a8 = [sbr(f"aa8{j}", [128, 8, 4], F32) for j in range(2)]
                ps4 = es1.enter_context(cx.pst("aps4", [128, 4, 512], F32))
                ppT = es1.enter_context(cx.pst("appT", [128, 12, 128], BF16))
                po = es1.enter_context(cx.pst("apo", [128, 4, 64], F32))
                mk = cslice(cx, 'mk')
                steps = [(n, half) for n in range(NT) for half in range(2)]

                def geom(n):
                    j0 = max(n - 1, 0)
                    j1 = min(n + 1, NT - 1)
                    return j0, j1, j1 - j0 + 1, (j0 - (n - 1)) * 128

                def att_F1(it):
                    n, half = steps[it]
                    j0, j1, nk, mo = geom(n)
                    W_ = nk * 128
                    b = it % 2
                    pl = slice(64 * half, 64 * half + 64)
                    A = lambda j: a8[b][:, j, :]
                    skh = sk[:, half * 4:half * 4 + 4]
                    for c in range(4):
                        k.op('pe', lambda e: e.matmul(ps4[:, c, 0:W_], lhsT=qbT[pl, c, n * 128:(n + 1) * 128], rhs=kbT[pl, j0 * 128:(j1 + 1) * 128],
                                                      start=True, stop=True), R=['qbT', 'kbT'], W=['aps4'])
                    k.op('dve', lambda e: e.tensor_tensor(out=sm4[b][:, :, 0:W_], in0=ps4[:, :, 0:W_],
                                                          in1=mk[:, mo:mo + W_].unsqueeze(1).broadcast_to([128, 4, W_]), op=ALU.add),
                         R=['aps4', 'cst'], W=[f'asm{b}'])
                    k.op('dve', lambda e: e.tensor_reduce(out=A(0), in_=sm4[b][:, :, 0:W_], axis=AX.X, op=ALU.max), R=[f'asm{b}'], W=[f'aa{b}0'])
                    k.op('dve', lambda e: e.tensor_tensor(out=A(0), in0=A(0), in1=skh, op=ALU.max), R=[f'aa{b}0', 'ask'], W=[f'aa{b}0'])
                    k.op('dve', lambda e: e.tensor_scalar(out=A(1), in0=A(0), scalar1=-1.0, scalar2=None, op0=ALU.mult), R=[f'aa{b}0'], W=[f'aa{b}1'])
                    k.op('dve', lambda e: e.tensor_tensor(out=A(3), in0=skh, in1=A(0), op=ALU.subtract), R=['ask', f'aa{b}0'], W=[f'aa{b}3'])

                def att_F2(it):
                    n, half = steps[it]
                    j0, j1, nk, mo = geom(n)
                    W_ = nk * 128
                    b = it % 2
                    A = lambda j: a8[b][:, j, :]
                    for c in range(4):
                        k.op('act', lambda e: e.activation(out=pb4[b][:, c, 0:W_], in_=sm4[b][:, c, 0:W_], func=AF.Exp, bias=a8[b][:, 1, c:c + 1], scale=1.0,
                                                           accum_out=a8[b][:, 2, c:c + 1]), R=[f'asm{b}', f'aa{b}1'], W=[f'apb{b}', f'aa{b}2'])
                    k.op('act', lambda e: e.activation(out=A(3), in_=A(3), func=AF.Exp), R=[f'aa{b}3'], W=[f'aa{b}3'])

                def att_B1(it):
                    n, half = steps[it]
                    j0, j1, nk, mo = geom(n)
                    b = it % 2
                    for c in range(4):
                        for kb_ in range(nk):
                            k.op('pe', lambda e: e.transpose(out=ppT[:, c * 3 + kb_, :], in_=pb4[b][:, c, kb_ * 128:(kb_ + 1) * 128], identity=cx.ident_b[:]),
                                 R=[f'apb{b}', 'ident_b'], W=['appT'])
                    if nk == 3:
                        k.op('act', lambda e: e.copy(out=pT4[b][:, 0:6, :], in_=ppT[:, 0:6, :]), R=['appT'], W=[f'apT{b}'])
                        k.op('act', lambda e: e.copy(out=pT4[b][:, 6:12, :], in_=ppT[:, 6:12, :]), R=['appT'], W=[f'apT{b}'])
                    else:
                        for c in range(4):
                            k.op('act', lambda e: e.copy(out=pT4[b][:, c * 3:c * 3 + nk, :], in_=ppT[:, c * 3:c * 3 + nk, :]), R=['appT'], W=[f'apT{b}'])

                def att_B2(it):
                    n, half = steps[it]
                    j0, j1, nk, mo = geom(n)
                    b = it % 2
                    A = lambda j: a8[b][:, j, :]
                    for c in range(4):
                        for kb_ in range(nk):
                            k.op('pe', lambda e: e.matmul(po[:, c, :], lhsT=pT4[b][:, c * 3 + kb_, :], rhs=vb_all[:, j0 + kb_, half * 64:(half + 1) * 64],
                                                          start=(kb_ == 0), stop=(kb_ == nk - 1)), R=[f'apT{b}', 'vb_all'], W=['apo'])
                    k.op('dve', lambda e: e.tensor_tensor(out=A(4), in0=A(2), in1=A(3), op=ALU.add), R=[f'aa{b}2', f'aa{b}3'], W=[f'aa{b}4'])
                    k.op('dve', lambda e: e.reciprocal(out=A(5), in_=A(4)), R=[f'aa{b}4'], W=[f'aa{b}5'])
                    k.op('dve', lambda e: e.tensor_tensor(out=yb_all[:, n, :].rearrange("p (c t d) -> p c t d", c=4, t=2)[:, :, half, :], in0=po[:],
                                                          in1=A(5).unsqueeze(2).broadcast_to([128, 4, 64]), op=ALU.mult),
                         R=['apo', f'aa{b}5'], W=['yb_all'])
                att_F1(0)
                att_F2(0)
                for it in range(len(steps)):
                    if it + 1 < len(steps):
                        att_F1(it + 1)
                    att_B1(it)
                    if it + 1 < len(steps):
                        att_F2(it + 1)
                    att_B2(it)
            k.barrier()

        with ExitStack() as es1:
            sbr = lambda n, s, d: es1.enter_context(cx.sbt(n, s, d, side='right'))
            wo = sbr("ewo", [128, 8, D], BF16)
            for cb in range(2):
                k.dma('pool', wo[:, :, cb * 512:(cb + 1) * 512],
                      din['w_out_even'][ev][:, cb * 512:(cb + 1) * 512].rearrange("(kc p) n -> p kc n", p=128), W=['ewo'])
            NB = 3
            xt = [sbr(f"oxt{j}", [128, D], F32) for j in range(NB)]
            yat = [sbr(f"oya{j}", [128, 512], BF16) for j in range(NB)]
            hh_ = [sbr(f"oh{j}", [128, D], F32) for j in range(NB)]
            xo = [sbr(f"oxo{j}", [128, D], F32) for j in range(NB)]
            yT = [sbr(f"oyT{j}", [128, 8, 128], BF16) for j in range(2)]
            ptr = [es1.enter_context(cx.pst(f"optr{j}", [128, 8, 128], BF16)) for j in range(2)]
            pm = [es1.enter_context(cx.pst(f"opm{j}", [128, 512], F32)) for j in range(4)]
            wk = ln_work(cx, es1, "ln1e")
            gain, bias, gkeys = ln_params(cx, es1, 'ln1', cx.lnL if hasattr(cx, 'lnL') else L)

            def stageA(i):
                b = i % NB
                b2 = i % 2
                k.dma('sp', xt[b][:], X[i * 128:(i + 1) * 128, :], W=[f'oxt{b}'])
                k.dma('sp', yat[b][:], YA[i * 128:(i + 1) * 128, :], R=['YA'], W=[f'oya{b}'])
                for kc in range(8):
                    src = yat[b][:, kc * 128:(kc + 1) * 128] if kc < 4 else yb_all[:, i, (kc - 4) * 128:(kc - 3) * 128]
                    k.op('pe', lambda e: e.transpose(out=ptr[b2][:, kc, :], in_=src, identity=cx.ident_b[:]),
                         R=[f'oya{b}', 'yb_all', 'ident_b'], W=[f'optr{b2}'])
                k.op('act', lambda e: e.copy(out=yT[b2][:], in_=ptr[b2][:]), R=[f'optr{b2}'], W=[f'oyT{b2}'])
            nn = 0
            pend = [None]
            stageA(0)
            for i in range(NT):
                b = i % NB
                b2 = i % 2
                if i + 1 < NT:
                    stageA(i + 1)
                for hh in range(2):
                    P = pm[nn % 4]
                    for kc in range(8):
                        k.op('pe', lambda e: e.matmul(P[:], lhsT=yT[b2][:, kc, :], rhs=wo[:, kc, hh * 512:(hh + 1) * 512], start=(kc == 0), stop=(kc == 7)),
                             R=[f'oyT{b2}', 'ewo'], W=[f'opm{nn % 4}'])
                    k.op('dve', lambda e: e.scalar_tensor_tensor(out=hh_[b][:, hh * 512:(hh + 1) * 512], in0=xt[b][:, hh * 512:(hh + 1) * 512],
                                                                 scalar=ALPHA, in1=P[:], op0=ALU.mult, op1=ALU.add),
                         R=[f'oxt{b}', f'opm{nn % 4}'], W=[f'oh{b}'])
                    nn += 1
                tail = layernorm_tile(cx, hh_[b], f'oh{b}', xo[b][:], f'oxo{b}', gain, bias, gkeys, wk, sfx=str(b), mid=pend[0], defer=True)
                pend[0] = (lambda tail=tail, i=i, b=b: (tail(), k.dma('pool', X1[i * 128:(i + 1) * 128, :], xo[b][:], R=[f'oxo{b}'], W=[('X1', i)])))
            pend[0]()
    k.barrier()


W_SHAPES = {
    'w_in_even': (2, 1024, IN_EVEN), 'ret_decay_logit': (2, 2, 4), 'ret_gn_gain': (2, 512), 'sink_logit': (2, 8),
    'w_out_even': (2, 1024, 1024), 'w_out_fourier': (2, 1024, 1024),
    'ln1_gain': (4, 1024), 'ln1_bias': (4, 1024), 'ln2_gain': (4, 1024), 'ln2_bias': (4, 1024),
    'router_coarse_w': (4, 1024, 4), 'router_coarse_b': (4, 4), 'router_fine_w': (4, 1024, 32), 'router_fine_b': (4, 32),
    'expert_w_gate': (4, 32, 1024, 512), 'expert_w_up': (4, 32, 1024, 512), 'expert_w_down': (4, 32, 512, 1024),
}


def build_program():
    nc = bass.Bass("TRN2", target_bir_lowering=False)
    cx = Ctx()
    cx.nc = nc
    cx.k = KB(nc)

    def din(name, shape, dt=F32):
        return nc.dram_tensor(name, list(shape), dt, kind="ExternalInput").ap()
    cx.din = {nm: din(nm, shp) for nm, shp in W_SHAPES.items()}
    x_in = din('x', (S, D))
    cx.d_cst = din('cst', (128, CST_N))
    cx.d_rope = din('rope', (NT, 128, 384))
    cx.d_dftc = din('dftc', (128, 2, 2, 256))
    cx.d_dfts = din('dfts', (NT, 128, 2, NT // 2, 128), BF16)
    cx.d_cmid = din('cmid', (1, S), BF16)
    cx.d_ridx = nc.dram_tensor('ridx', [128, NT // 2 + 1], I32, kind='ExternalInput').ap()
    out = nc.dram_tensor("out", [S, D], F32, kind="ExternalOutput").ap()
    XA = nc.dram_tensor("XA", [S, D], F32, kind="Internal").ap()
    XB = nc.dram_tensor("XB", [S, D], F32, kind="Internal").ap()
    cx.XE = nc.dram_tensor("XE", [NE * CAP, D], BF16, kind="Internal").ap()
    cx.YE = nc.dram_tensor("YE", [NE * CAP, D], F32, kind="Internal").ap()
    cx.SGA = nc.dram_tensor("SGA", [S, 512], BF16, kind="Internal").ap()
    cx.YA = nc.dram_tensor("YA", [S, 512], BF16, kind="Internal").ap()
    load_common(cx)
    zrow = nc.alloc_sbuf_tensor("zrow", [128, D], BF16)
    cx.k.op('dve', lambda e: e.memset(zrow[:], 0.0), W=['zrow'])
    for c in range(NE * CAP // 1024):
        cx.k.dma('act', cx.XE[c * 1024:(c + 1) * 1024, :].rearrange("(j p) d -> p j d", p=128),
                 zrow[:].unsqueeze(1).broadcast_to([128, 8, D]), R=['zrow'], W=['XE'])
    cur = x_in
    for L in range(DEPTH):
        if L % 2 == 0:
            even_phase(cx, L, cur, XA)
        else:
            fnet_phase(cx, L, cur, XA)
        dst = out if L == DEPTH - 1 else XB
        moe_phase(cx, L, XA, dst)
        cur = dst
    cx.k.finish_all()
    return nc


def kernel(**inputs):
    inp = {k_: np.ascontiguousarray(np.asarray(v, dtype=np.float32)) for k_, v in inputs.items()}
    B = inp['x'].shape[0]
    wi, wo = host_even_layout(inp['w_in_even'], inp['w_out_even'])
    dftc, dfts, cmid = host_dft_consts()
    shared = {nm: inp[nm] for nm in W_SHAPES}
    shared['w_in_even'] = wi
    shared['w_out_even'] = wo
    shared['cst'] = host_consts()
    shared['rope'] = host_rope_consts()
    shared['dftc'] = dftc
    shared['dfts'] = dfts
    shared['cmid'] = cmid
    shared['ridx'] = host_ridx()
    nc = build_program()
    in_maps = []
    for c in range(B):
        m = dict(shared)
        m['x'] = inp['x'][c]
        in_maps.append(m)
    res = run_bass_kernel_spmd(nc, in_maps, core_ids=list(range(B)))
    return np.stack([np.asarray(res.results[c]['out'], dtype=np.float32) for c in range(B)], 0)
```

```python
from contextlib import ExitStack
import numpy as np
import ml_dtypes
import concourse.bass as bass
import concourse.mybir as mybir
from concourse.bass_utils import run_bass_kernel_spmd

F32 = mybir.dt.float32; BF16 = mybir.dt.bfloat16; I32 = mybir.dt.int32
AF = mybir.ActivationFunctionType; ALU = mybir.AluOpType; AX = mybir.AxisListType

S = 4096; D = 1024; NT = S // 128; DEPTH = 4
NE = 32; HID = 512; CAP = 384
ALPHA = (2.0 * DEPTH) ** 0.25
LN_EPS = 1e-5; GN_EPS = 1e-6
IN_EVEN = 2816


class KB:
    def __init__(self, nc, n_dma_sems=24, same_engine_sync=True):
        self.nc = nc
        self.eng = {'pe': nc.tensor, 'act': nc.scalar, 'dve': nc.vector, 'pool': nc.gpsimd, 'sp': nc.sync}
        self.sem = {e: nc.alloc_semaphore(f"s_{e}") for e in ['pe', 'act', 'dve', 'pool']}
        self.seq = {e: 0 for e in self.sem}
        self.waited = {e: {} for e in self.eng}
        self.dma_sems = [nc.alloc_semaphore(f"d{i}") for i in range(n_dma_sems)]
        self.dma_cnt = [0] * n_dma_sems
        self.dma_next = 0
        self.lastw = {}
        self.readers = {}
        self.same = same_engine_sync
        self.nwaits = 0
        self.nops = 0

    def _wait(self, e, tok):
        semkey, sem, val = tok
        w = self.waited[e]
        if w.get(semkey, 0) >= val:
            return
        self.eng[e].wait_ge(sem, val)
        w[semkey] = val
        self.nwaits += 1

    def _deps(self, e, R, W):
        best = {}

        def add(t):
            if t[0] == e and (e == 'pe' or not self.same):
                return
            if t[0] not in best or best[t[0]][2] < t[2]:
                best[t[0]] = t
        for r in R:
            for t in self.lastw.get(r, {}).values():
                add(t)
        for w_ in W:
            for t in self.lastw.get(w_, {}).values():
                add(t)
            for t in self.readers.get(w_, {}).values():
                add(t)
        for t in best.values():
            self._wait(e, t)

    def _commit(self, tok, R, W):
        for r in R:
            d = self.readers.setdefault(r, {})
            if tok[0] not in d or d[tok[0]][2] < tok[2]:
                d[tok[0]] = tok
        for w_ in W:
            self.lastw.setdefault(w_, {})[tok[0]] = tok
            self.readers[w_] = {}

    def op(self, e, fn, R=(), W=()):
        self._deps(e, R, W)
        ins = fn(self.eng[e])
        self.seq[e] += 1
        ins.then_inc(self.sem[e], 1)
        self._commit((e, self.sem[e], self.seq[e]), R, W)
        self.nops += 1
        return ins

    def dma(self, q, out, in_, R=(), W=(), indirect=None, **kw):
        i = self.dma_next
        self.dma_next = (i + 1) % len(self.dma_sems)
        sem = self.dma_sems[i]
        if self.dma_cnt[i] > 0:
            self._wait(q, (('d', i), sem, 16 * self.dma_cnt[i]))
        self._deps(q, R, W)
        if indirect is None:
            ins = self.eng[q].dma_start(out=out, in_=in_, **kw)
        else:
            ins = self.eng[q].indirect_dma_start(out=out, in_=in_, **indirect, **kw)
        self.dma_cnt[i] += 1
        ins.then_inc(sem, 16)
        self._commit((('d', i), sem, 16 * self.dma_cnt[i]), R, W)
        self.nops += 1
        return ins

    def finish_all(self):
        toks = [(e, self.sem[e], self.seq[e]) for e in self.sem if self.seq[e] > 0]
        toks += [(('d', i), s, 16 * c) for i, (s, c) in enumerate(zip(self.dma_sems, self.dma_cnt)) if c > 0]
        for t in toks:
            self._wait('sp', t)

    def barrier(self):
        toks = [(e, self.sem[e], self.seq[e]) for e in self.sem if self.seq[e] > 0]
        toks += [(('d', i), s, 16 * c) for i, (s, c) in enumerate(zip(self.dma_sems, self.dma_cnt)) if c > 0]
        for e in self.eng:
            for t in toks:
                if t[0] != e or e != 'pe':
                    self._wait(e, t)
        self.lastw = {}
        self.readers = {}


CST_COLS = {}


def _cst_layout():
    off = 0
    for name, n in [('ident', 128), ('ltri', 128), ('ones', 128), ('ecap', 1024), ('rp', 128), ('rn', 128),
                    ('cp1', 128), ('cmc', 128), ('pcol', 4), ('mk', 384)]:
        CST_COLS[name] = (off, n)
        off += n
    return off


CST_N = _cst_layout()


def host_consts():
    c = np.zeros((128, CST_N), np.float32)
    p = np.arange(128)

    def put(name, arr):
        o, n = CST_COLS[name]
        c[:, o:o + n] = arr
    put('ident', np.eye(128))
    put('ltri', (p[:, None] < p[None, :]).astype(np.float32))
    put('ones', np.ones((128, 128)))
    put('ecap', np.tile((np.arange(NE) * CAP)[None, :], (128, NT)))
    dif = (p[None, :] - p[:, None]).astype(np.float32)
    put('rp', np.maximum(dif, 0.0))
    put('rn', np.maximum(-dif, 0.0))
    jj = np.arange(384)
    put('mk', np.where((jj[None, :] >= p[:, None]) & (jj[None, :] <= p[:, None] + 256), 0.0, -30000.0))
    put('cp1', np.tile((p + 1.0)[None, :], (128, 1)))
    put('cmc', np.tile((128.0 - p)[None, :], (128, 1)))
    pc = np.stack([127.0 - p, p.astype(np.float64), np.full(128, 128.0), np.zeros(128)], 1)
    put('pcol', pc)
    return c


class Ctx:
    uid = 0

    def sbt(self, name, shape, dt, **kw):
        Ctx.uid += 1
        return self.nc.sbuf_tensor(f"{name}_u{Ctx.uid}", shape, dt, **kw)

    def pst(self, name, shape, dt, **kw):
        Ctx.uid += 1
        return self.nc.psum_tensor(f"{name}_u{Ctx.uid}", shape, dt, **kw)


def cslice(cx, name):
    o, n = CST_COLS[name]
    return cx.cst[:, o:o + n]


def load_common(cx):
    nc, k = cx.nc, cx.k
    cx.cst = nc.alloc_sbuf_tensor("cst_sb", [128, CST_N], F32)
    k.dma('sp', cx.cst[:], cx.d_cst, W=['cst'])
    cx.ident_b = nc.alloc_sbuf_tensor("ident_b", [128, 128], BF16)
    k.op('dve', lambda e: e.tensor_copy(out=cx.ident_b[:], in_=cslice(cx, 'ident')), R=['cst'], W=['ident_b'])
    cx.bound_reg = nc.gpsimd.to_reg(NE * CAP - 1)
    cx.bound_reg_s = nc.gpsimd.to_reg(S - 1)
    cx.eps_ln = nc.alloc_sbuf_tensor("eps_ln", [128, 1], F32)
    k.op('dve', lambda e: e.memset(cx.eps_ln[:], LN_EPS), W=['eps_ln'])
    cx.eps_gn = nc.alloc_sbuf_tensor("eps_gn", [128, 1], F32)
    k.op('dve', lambda e: e.memset(cx.eps_gn[:], GN_EPS), W=['eps_gn'])


def layernorm_tile(cx, h, hkey, out, okey, gain, bias, gkeys, wk, sfx='', mid=None, defer=False, hout=None, houtkey=None):
    k = cx.k
    j = (int(sfx) % 4) if sfx else 0
    st, mv, sc = wk['st'][:, j, :], wk['mv'][:, j, :], wk['sc'][:, j, :]
    K_ = lambda n: n + str(j)
    if hout is None:
        hout, houtkey = h, hkey
    k.op('dve', lambda e: e.bn_stats(out=st[:, 0:6], in_=h[:, 0:512]), R=[hkey], W=[K_('ln_st')])
    k.op('dve', lambda e: e.bn_stats(out=st[:, 6:12], in_=h[:, 512:1024]), R=[hkey], W=[K_('ln_st')])
    k.op('dve', lambda e: e.bn_aggr(out=mv, in_=st), R=[K_('ln_st')], W=[K_('ln_mv')])
    k.op('act', lambda e: e.activation(out=sc[:, 0:1], in_=mv[:, 1:2], func=AF.Sqrt, bias=cx.eps_ln[:], scale=1.0),
         R=[K_('ln_mv'), 'eps_ln'], W=[K_('ln_sd')])
    if mid is not None:
        mid()
    k.op('dve', lambda e: e.reciprocal(out=sc[:, 1:2], in_=sc[:, 0:1]), R=[K_('ln_sd')], W=[K_('ln_rstd')])
    k.op('dve', lambda e: e.scalar_tensor_tensor(out=sc[:, 2:3], in0=mv[:, 0:1], scalar=-1.0, in1=sc[:, 1:2],
                                                 op0=ALU.mult, op1=ALU.mult), R=[K_('ln_mv'), K_('ln_rstd')], W=[K_('ln_nmr')])
    k.op('act', lambda e: e.activation(out=hout[:], in_=h[:], func=AF.Identity, bias=sc[:, 2:3], scale=sc[:, 1:2]),
         R=[hkey, K_('ln_rstd'), K_('ln_nmr')], W=[houtkey])

    def tail():
        k.op('dve', lambda e: e.tensor_tensor(out=hout[:], in0=hout[:], in1=gain, op=ALU.mult), R=[houtkey] + gkeys, W=[houtkey])
        k.op('pool', lambda e: e.tensor_tensor(out=out, in0=hout[:], in1=bias, op=ALU.add), R=[houtkey] + gkeys, W=[okey])
    if defer:
        return tail
    tail()
    return None


def layernorm_gen(cx, h, hkey, out, okey, gain, bias, gkeys, wk, slot):
    k = cx.k
    j = slot % 4
    st, mv, sc = wk['st'][:, j, :], wk['mv'][:, j, :], wk['sc'][:, j, :]
    K_ = lambda n: n + str(j)
    k.op('dve', lambda e: e.bn_stats(out=st[:, 0:6], in_=h[:, 0:512]), R=[hkey], W=[K_('ln_st')])
    yield
    k.op('dve', lambda e: e.bn_stats(out=st[:, 6:12], in_=h[:, 512:1024]), R=[hkey], W=[K_('ln_st')])
    yield
    k.op('dve', lambda e: e.bn_aggr(out=mv, in_=st), R=[K_('ln_st')], W=[K_('ln_mv')])
    yield
    k.op('act', lambda e: e.activation(out=sc[:, 0:1], in_=mv[:, 1:2], func=AF.Sqrt, bias=cx.eps_ln[:], scale=1.0),
         R=[K_('ln_mv'), 'eps_ln'], W=[K_('ln_sd')])
    yield
    k.op('dve', lambda e: e.reciprocal(out=sc[:, 1:2], in_=sc[:, 0:1]), R=[K_('ln_sd')], W=[K_('ln_rstd')])
    yield
    k.op('dve', lambda e: e.scalar_tensor_tensor(out=sc[:, 2:3], in0=mv[:, 0:1], scalar=-1.0, in1=sc[:, 1:2],
                                                 op0=ALU.mult, op1=ALU.mult), R=[K_('ln_mv'), K_('ln_rstd')], W=[K_('ln_nmr')])
    yield
    k.op('act', lambda e: e.activation(out=h[:], in_=h[:], func=AF.Identity, bias=sc[:, 2:3], scale=sc[:, 1:2]),
         R=[hkey, K_('ln_rstd'), K_('ln_nmr')], W=[hkey])
    yield
    k.op('dve', lambda e: e.tensor_tensor(out=h[:], in0=h[:], in1=gain, op=ALU.mult), R=[hkey] + gkeys, W=[hkey])
    yield
    k.op('pool', lambda e: e.tensor_tensor(out=out, in0=h[:], in1=bias, op=ALU.add), R=[hkey] + gkeys, W=[okey])
    yield


def interleave(gens):
    gens = list(gens)
    while gens:
        for g in list(gens):
            try:
                next(g)
            except StopIteration:
                gens.remove(g)


def ln_params(cx, es, which, L):
    nc, k = cx.nc, cx.k
    g = es.enter_context(cx.sbt(f"{which}_g_sb", [128, D], F32))
    b = es.enter_context(cx.sbt(f"{which}_b_sb", [128, D], F32))
    k.dma('sp', g[:], cx.din[which + '_gain'][L].partition_broadcast(128), W=[which + '_g'])
    k.dma('sp', b[:], cx.din[which + '_bias'][L].partition_broadcast(128), W=[which + '_b'])
    return g[:], b[:], [which + '_g', which + '_b']


def ln_work(cx, es, pfx):
    nc = cx.nc
    return {'st': es.enter_context(cx.sbt(pfx + "_st", [128, 4, 12], F32)),
            'mv': es.enter_context(cx.sbt(pfx + "_mv", [128, 4, 2], F32)),
            'sc': es.enter_context(cx.sbt(pfx + "_sc", [128, 4, 4], F32))}


def moe_phase(cx, L, X1, X2):
    nc, k = cx.nc, cx.k
    din = cx.din
    XE, YE = cx.XE, cx.YE
    NJ = CAP // 128
    NW = 4
    with ExitStack() as es:
        sb = lambda n, s, d: es.enter_context(cx.sbt(n, s, d))
        g_all = [sb("g1_all", [128, NT], F32), sb("g2_all", [128, NT], F32)]
        dsti = [sb("dsti0", [128, NT], I32), sb("dsti1", [128, NT], I32)]
        wg = [sb(f"wg{j}", [128, 8, HID], BF16) for j in range(NW - 1)]
        wu = [sb(f"wu{j}", [128, 8, HID], BF16) for j in range(NW - 1)]
        wd = [sb(f"wd{j}", [128, 4, D], BF16) for j in range(NW - 1)]

        def LW(ex):
            wb = ex % NW
            k.dma('pool', wg[wb][:], din['expert_w_gate'][L, ex].rearrange("(kc p) n -> p kc n", p=128), W=[f'wg{wb}'])
            k.dma('pool', wu[wb][:], din['expert_w_up'][L, ex].rearrange("(kc p) n -> p kc n", p=128), W=[f'wu{wb}'])
            k.dma('pool', wd[wb][:], din['expert_w_down'][L, ex].rearrange("(kc p) n -> p kc n", p=128), W=[f'wd{wb}'])
        LW(0)
        LW(1)
        LW(2)
        ident = cslice(cx, 'ident')

        with ExitStack() as esx:
            sb2 = lambda n, s, d: esx.enter_context(cx.sbt(n, s, d))
            xb_all = sb2("xb_all", [128, NT, D], BF16)
            lgc = sb2("lgc", [128, NT, 4], F32)
            lgf = sb2("lgf", [128, NT * NE], F32)
            wr = sb2("wr", [128, 8, 36], F32)
            rb = sb2("rb", [128, 36], F32)
            k.dma('sp', wr[:, :, 0:4], din['router_coarse_w'][L].rearrange("(kc p) n -> p kc n", p=128), W=['wr'])
            k.dma('sp', wr[:, :, 4:36], din['router_fine_w'][L].rearrange("(kc p) n -> p kc n", p=128), W=['wr'])
            k.dma('sp', rb[:, 0:4], din['router_coarse_b'][L].partition_broadcast(128), W=['rb'])
            k.dma('sp', rb[:, 4:36], din['router_fine_b'][L].partition_broadcast(128), W=['rb'])
            NB = 4
            xt = [sb2(f"mxt{j}", [128, D], F32) for j in range(NB)]
            xT32 = [sb2(f"mxT{j}", [128, 8, 128], F32) for j in range(2)]
            pst = [esx.enter_context(cx.pst(f"pst{j}", [128, 4, 128], F32)) for j in range(4)]
            psl = [esx.enter_context(cx.pst(f"psl{j}", [128, 36], F32)) for j in range(2)]
            GT = 8
            NG = NT // GT
            N1 = GT * NE
            oh1 = sb2("oh1", [128, N1], F32)
            oh2 = sb2("oh2", [128, N1], F32)
            sel = sb2("sel", [128, N1], F32)
            fm = sb2("fm", [128, N1], F32)
            fm2 = sb2("fm2", [128, N1], F32)
            posw = sb2("posw", [128, N1], F32)
            cum = [sb2("cumA", [128, N1], F32), sb2("cumB", [128, N1], F32)]
            tot = sb2("tot", [128, N1], F32)
            base = sb2("base", [128, NE], F32)
            sc4 = sb2("sc4", [128, GT, 4], F32)
            pen = sb2("pen", [128, GT, 4], F32)
            v = sb2("rv", [128, 8, GT], F32)
            dstf = sb2("dstf", [128, 2 * GT], F32)
            psw = esx.enter_context(cx.pst("psw", [128, N1], F32))
            pso = esx.enter_context(cx.pst("pso", [128, N1], F32))
            k.op('dve', lambda e: e.memset(base[:], 0.0), W=['base'])
            dv = lambda fn, R, W: k.op('dve', fn, R=R, W=W)

            def router_tile(i):
                b = i % NB
                b2 = i % 2
                X, XT = xt[b], xT32[b2]
                k.dma('sp', X[:], X1[i * 128:(i + 1) * 128, :], W=[f'mxt{b}'])
                k.op('act', lambda e: e.copy(out=xb_all[:, i, :], in_=X[:]), R=[f'mxt{b}'], W=[('xb', i)])
                for hh in range(2):
                    pi = (i % 2) * 2 + hh
                    P = pst[pi]
                    for j in range(4):
                        kc = hh * 4 + j
                        k.op('pe', lambda e: e.transpose(out=P[:, j, :], in_=X[:, kc * 128:(kc + 1) * 128], identity=ident),
                             R=[f'mxt{b}', 'cst'], W=[f'pst{pi}'])
                    if hh == 0:
                        k.op('act', lambda e: e.copy(out=XT[:, 0:4, :], in_=P[:]), R=[f'pst{pi}'], W=[f'mxT{b2}a'])
                    else:
                        k.op('dve', lambda e: e.tensor_copy(out=XT[:, 4:8, :], in_=P[:]), R=[f'pst{pi}'], W=[f'mxT{b2}b'])
                PL = psl[b2]
                for kc in range(8):
                    k.op('pe', lambda e: e.matmul(PL[:, :], lhsT=XT[:, kc, :], rhs=wr[:, kc, :], start=(kc == 0), stop=(kc == 7)),
                         R=[f'mxT{b2}a', f'mxT{b2}b', 'wr'], W=[f'psl{b2}'])
                k.op('dve', lambda e: e.tensor_tensor(out=lgc[:, i, :], in0=PL[:, 0:4], in1=rb[:, 0:4], op=ALU.add), R=[f'psl{b2}', 'rb'], W=['lgc'])
                k.op('dve', lambda e: e.tensor_tensor(out=lgf[:, i * NE:(i + 1) * NE], in0=PL[:, 4:36], in1=rb[:, 4:36], op=ALU.add),
                     R=[f'psl{b2}', 'rb'], W=['lgf'])

            def route_group(g):
                t0 = g * GT
                ts = slice(t0, t0 + GT)
                LC = lgc[:, ts, :]
                LF = lgf[:, t0 * NE:(t0 + GT) * NE]
                V = lambda j: v[:, j, :]
                b3 = lambda ap, n: ap.unsqueeze(2).broadcast_to([128, GT, n])
                dv(lambda e: e.tensor_reduce(out=V(0), in_=LC, axis=AX.X, op=ALU.max), ['lgc'], ['v0'])
                dv(lambda e: e.tensor_tensor(out=sc4[:], in0=LC, in1=b3(V(0), 4), op=ALU.subtract), ['lgc', 'v0'], ['sc4'])
                dv(lambda e: e.tensor_scalar(out=pen[:], in0=sc4[:], scalar1=0.0, scalar2=None, op0=ALU.is_equal), ['sc4'], ['pen'])
                dv(lambda e: e.tensor_scalar(out=pen[:], in0=pen[:], scalar1=1.0, scalar2=1e30, op0=ALU.subtract, op1=ALU.mult), ['pen'], ['pen'])
                k.op('act', lambda e: e.activation(out=sc4[:], in_=sc4[:], func=AF.Exp), R=['sc4'], W=['sc4'])
                dv(lambda e: e.tensor_reduce(out=V(1), in_=sc4[:], axis=AX.X, op=ALU.add), ['sc4'], ['v1'])
                dv(lambda e: e.reciprocal(out=V(2), in_=V(1)), ['v1'], ['v2'])
                dv(lambda e: e.tensor_tensor(out=fm[:].rearrange("p (a j) -> p a j", j=8), in0=LF.rearrange("p (a j) -> p a j", j=8),
                                             in1=pen[:].rearrange("p i g -> p (i g)").unsqueeze(2).broadcast_to([128, GT * 4, 8]), op=ALU.add),
                   ['lgf', 'pen'], ['fm'])
                fm3 = fm[:].rearrange("p (i e) -> p i e", e=NE)
                dv(lambda e: e.tensor_reduce(out=V(3), in_=fm3, axis=AX.X, op=ALU.max), ['fm'], ['v3'])
                dv(lambda e: e.tensor_tensor(out=oh1[:].rearrange("p (i e) -> p i e", e=NE), in0=fm3, in1=b3(V(3), NE), op=ALU.is_equal), ['fm', 'v3'], ['oh1'])
                dv(lambda e: e.scalar_tensor_tensor(out=fm2[:], in0=oh1[:], scalar=-1e30, in1=fm[:], op0=ALU.mult, op1=ALU.add), ['oh1', 'fm'], ['fm2'])
                fm23 = fm2[:].rearrange("p (i e) -> p i e", e=NE)
                dv(lambda e: e.tensor_reduce(out=V(4), in_=fm23, axis=AX.X, op=ALU.max), ['fm2'], ['v4'])
                dv(lambda e: e.tensor_tensor(out=oh2[:].rearrange("p (i e) -> p i e", e=NE), in0=fm23, in1=b3(V(4), NE), op=ALU.is_equal), ['fm2', 'v4'], ['oh2'])
                dv(lambda e: e.tensor_tensor(out=sel[:], in0=oh1[:], in1=oh2[:], op=ALU.add), ['oh1', 'oh2'], ['sel'])
                dv(lambda e: e.tensor_tensor(out=V(5), in0=V(4), in1=V(3), op=ALU.subtract), ['v3', 'v4'], ['v5'])
                k.op('act', lambda e: e.activation(out=V(6), in_=V(5), func=AF.Exp), R=['v5'], W=['v6'])
                dv(lambda e: e.tensor_scalar(out=V(7), in0=V(6), scalar1=1.0, scalar2=None, op0=ALU.add), ['v6'], ['v7'])
                dv(lambda e: e.reciprocal(out=V(7), in_=V(7)), ['v7'], ['v7'])
                dv(lambda e: e.tensor_tensor(out=g_all[0][:, ts], in0=V(2), in1=V(7), op=ALU.mult), ['v2', 'v7'], ['g1_all'])
                dv(lambda e: e.tensor_tensor(out=g_all[1][:, ts], in0=g_all[0][:, ts], in1=V(6), op=ALU.mult), ['g1_all', 'v6'], ['g2_all'])
                k.op('pe', lambda e: e.matmul(psw[:], lhsT=cslice(cx, 'ltri'), rhs=sel[:], start=True, stop=True), R=['sel', 'cst'], W=['psw'])
                k.op('pe', lambda e: e.matmul(pso[:], lhsT=cslice(cx, 'ones'), rhs=sel[:], start=True, stop=True), R=['sel', 'cst'], W=['pso'])
                k.op('act', lambda e: e.copy(out=cum[0][:], in_=pso[:]), R=['pso'], W=['cum0'])
                k.op('act', lambda e: e.copy(out=tot[:], in_=pso[:]), R=['pso'], W=['tot'])
                cur = 0
                sh = 1
                while sh < GT:
                    a_, b_ = cum[cur], cum[1 - cur]
                    dv(lambda e: e.tensor_copy(out=b_[:, 0:sh * NE], in_=a_[:, 0:sh * NE]), [f'cum{cur}'], [f'cum{1 - cur}'])
                    dv(lambda e: e.tensor_tensor(out=b_[:, sh * NE:], in0=a_[:, sh * NE:], in1=a_[:, 0:N1 - sh * NE], op=ALU.add),
                       [f'cum{cur}'], [f'cum{1 - cur}'])
                    cur = 1 - cur
                    sh *= 2
                inc = cum[cur]
                dv(lambda e: e.tensor_tensor(out=posw[:], in0=psw[:], in1=inc[:], op=ALU.add), ['psw', f'cum{cur}'], ['posw'])
                dv(lambda e: e.tensor_tensor(out=posw[:], in0=posw[:], in1=tot[:], op=ALU.subtract), ['posw', 'tot'], ['posw'])
                dv(lambda e: e.tensor_tensor(out=posw[:].rearrange("p (i e) -> p i e", e=NE), in0=posw[:].rearrange("p (i e) -> p i e", e=NE),
                                             in1=base[:].unsqueeze(1).broadcast_to([128, GT, NE]), op=ALU.add), ['posw', 'base'], ['posw'])
                dv(lambda e: e.tensor_tensor(out=base[:], in0=base[:], in1=inc[:, (GT - 1) * NE:GT * NE], op=ALU.add), ['base', f'cum{cur}'], ['base'])
                dv(lambda e: e.tensor_scalar(out=fm[:], in0=posw[:], scalar1=float(CAP), scalar2=1e6, op0=ALU.is_ge, op1=ALU.mult), ['posw', 'fm'], ['fm'])
                dv(lambda e: e.tensor_tensor(out=posw[:], in0=posw[:], in1=fm[:], op=ALU.add), ['posw', 'fm'], ['posw'])
                dv(lambda e: e.tensor_tensor(out=posw[:], in0=posw[:], in1=cslice(cx, 'ecap')[:, 0:N1], op=ALU.add), ['posw', 'cst'], ['posw'])
                for s_, oh in enumerate([oh1, oh2]):
                    dv(lambda e: e.tensor_tensor(out=fm2[:], in0=oh[:], in1=posw[:], op=ALU.mult), ['oh1', 'oh2', 'posw', 'fm2'], ['fm2'])
                    dv(lambda e: e.tensor_reduce(out=dstf[:, s_ * GT:(s_ + 1) * GT], in_=fm2[:].rearrange("p (i e) -> p i e", e=NE),
                                                 axis=AX.X, op=ALU.add), ['fm2'], ['dstf'])
                    dv(lambda e: e.tensor_copy(out=dsti[s_][:, ts], in_=dstf[:, s_ * GT:(s_ + 1) * GT]), ['dstf'], [f'dsti{s_}'])
                for i in range(t0, t0 + GT):
                    for s_ in range(2):
                        k.dma('pool', XE, xb_all[:, i, :], R=[('xb', i), f'dsti{s_}'], W=['XE'],
                              indirect=dict(out_offset=bass.IndirectOffsetOnAxis(ap=dsti[s_][:, i:i + 1], axis=0), in_offset=None,
                                            bounds_check=cx.bound_reg, oob_is_err=False))

            for g in range(NG):
                for i in range(g * GT, (g + 1) * GT):
                    router_tile(i)
                route_group(g)
        k.barrier()

        with ExitStack() as es2:
            sb2 = lambda n, s, d: es2.enter_context(cx.sbt(n, s, d))
            wg.append(sb2(f"wg{NW - 1}", [128, 8, HID], BF16))
            wu.append(sb2(f"wu{NW - 1}", [128, 8, HID], BF16))
            wd.append(sb2(f"wd{NW - 1}", [128, 4, D], BF16))
            xea = [sb2(f"xea{j}", [128, NJ, D], BF16) for j in range(2)]
            xeT = [sb2(f"xeT{j}", [128, 8, CAP], BF16) for j in range(2)]
            sg = [sb2(f"sg{j}", [128, CAP], F32) for j in range(2)]
            hid = [sb2(f"hid{j}", [128, 4, CAP], BF16) for j in range(2)]
            yo = [sb2(f"yo{j}", [128, NJ, D], F32) for j in range(2)]
            ptr = [es2.enter_context(cx.pst(f"ptr{j}", [128, 8, 128], BF16)) for j in range(2)]
            psg = [es2.enter_context(cx.pst(f"psg{j}", [128, CAP], F32)) for j in range(2)]
            psu = [es2.enter_context(cx.pst(f"psu{j}", [128, CAP], F32)) for j in range(2)]
            psy = [es2.enter_context(cx.pst(f"psy{j}", [128, 512], F32)) for j in range(2)]

            def LX(ex):
                k.dma('sp', xea[ex % 2][:], XE[ex * CAP:(ex + 1) * CAP, :].rearrange("(j p) d -> p j d", p=128), R=['XE'], W=[f'xea{ex % 2}'])

            def TGU(ex):
                wb = ex % NW
                XT = xeT[ex % 2]
                H = hid[ex % 2]
                XA = xea[ex % 2]
                for jt in range(NJ):
                    b2 = jt % 2
                    for kc in range(8):
                        k.op('pe', lambda e: e.transpose(out=ptr[b2][:, kc, :], in_=XA[:, jt, kc * 128:(kc + 1) * 128], identity=cx.ident_b[:]),
                             R=[f'xea{ex % 2}', 'ident_b'], W=[f'ptr{b2}'])
                    if b2 == 0:
                        k.op('act', lambda e: e.copy(out=XT[:, :, jt * 128:(jt + 1) * 128], in_=ptr[b2][:]), R=[f'ptr{b2}'], W=[f'xeT{ex % 2}'])
                    else:
                        k.op('dve', lambda e: e.tensor_copy(out=XT[:, :, jt * 128:(jt + 1) * 128], in_=ptr[b2][:]), R=[f'ptr{b2}'], W=[f'xeT{ex % 2}'])
                for hc in range(4):
                    b = hc % 2
                    for kc in range(8):
                        k.op('pe', lambda e: e.matmul(psg[b][:], lhsT=wg[wb][:, kc, hc * 128:(hc + 1) * 128], rhs=XT[:, kc, :],
                                                      start=(kc == 0), stop=(kc == 7)), R=[f'wg{wb}', f'xeT{ex % 2}'], W=[f'psg{b}'])
                    for kc in range(8):
                        k.op('pe', lambda e: e.matmul(psu[b][:], lhsT=wu[wb][:, kc, hc * 128:(hc + 1) * 128], rhs=XT[:, kc, :],
                                                      start=(kc == 0), stop=(kc == 7)), R=[f'wu{wb}', f'xeT{ex % 2}'], W=[f'psu{b}'])
                    k.op('act', lambda e: e.activation(out=sg[b][:], in_=psg[b][:], func=AF.Silu), R=[f'psg{b}'], W=[f'sg{b}'])
                    k.op('dve', lambda e: e.tensor_tensor(out=H[:, hc, :], in0=psu[b][:], in1=sg[b][:], op=ALU.mult),
                         R=[f'psu{b}', f'sg{b}'], W=[f'hid{ex % 2}'])

            def DN(ex):
                wb = ex % NW
                H = hid[ex % 2]
                YO = yo[ex % 2]
                for jt in range(NJ):
                    for hh in range(2):
                        for hc in range(4):
                            k.op('pe', lambda e: e.matmul(psy[hh][:], lhsT=H[:, hc, jt * 128:(jt + 1) * 128],
                                                          rhs=wd[wb][:, hc, hh * 512:(hh + 1) * 512], start=(hc == 0), stop=(hc == 3)),
                                 R=[f'hid{ex % 2}', f'wd{wb}'], W=[f'psy{hh}'])
                        if hh == 0:
                            k.op('act', lambda e: e.copy(out=YO[:, jt, 0:512], in_=psy[0][:]), R=['psy0'], W=[f'yo{ex % 2}'])
                        else:
                            k.op('dve', lambda e: e.tensor_copy(out=YO[:, jt, 512:1024], in_=psy[1][:]), R=['psy1'], W=[f'yo{ex % 2}'])
                k.dma('sp', YE[ex * CAP:(ex + 1) * CAP, :].rearrange("(j p) d -> p j d", p=128), YO[:], R=[f'yo{ex % 2}'], W=['YE'])

            LX(0)
            LX(1)
            TGU(0)
            for ex in range(NE):
                if ex + 3 < NE:
                    LW(ex + 3)
                if ex + 2 < NE and ex >= 0:
                    pass
                if ex + 1 < NE:
                    TGU(ex + 1)
                if ex + 2 < NE:
                    LX(ex + 2)
                DN(ex)
        k.barrier()

        with ExitStack() as es2:
            sb2 = lambda n, s, d: es2.enter_context(cx.sbt(n, s, d))
            NB = 5
            r1 = [sb2(f"r1_{j}", [128, D], F32) for j in range(NB)]
            r2 = [sb2(f"r2_{j}", [128, D], F32) for j in range(NB)]
            xt = [sb2(f"cxt{j}", [128, D], F32) for j in range(NB)]
            xo = [sb2(f"cxo{j}", [128, D], F32) for j in range(NB)]
            wk = ln_work(cx, es2, "ln2")
            gain, bias, gkeys = ln_params(cx, es2, 'ln2', cx.lnL if hasattr(cx, 'lnL') else L)
            def issue_loads(i):
                b = i % NB
                k.dma('sp', xt[b][:], X1[i * 128:(i + 1) * 128, :], W=[f'cxt{b}'])
                for s_, r in enumerate([r1[b], r2[b]]):
                    k.dma('pool', r[:], YE, R=['YE', f'dsti{s_}'], W=[f'r{s_}_{b}'],
                          indirect=dict(out_offset=None, in_offset=bass.IndirectOffsetOnAxis(ap=dsti[s_][:, i:i + 1], axis=0),
                                        bounds_check=cx.bound_reg, oob_is_err=False))
            for i in range(NB - 2):
                issue_loads(i)
            pend = [None]
            for i in range(NT):
                b = i % NB
                if i + NB - 2 < NT:
                    issue_loads(i + NB - 2)
                k.op('act', lambda e: e.activation(out=r2[b][:], in_=r2[b][:], func=AF.Identity, scale=g_all[1][:, i:i + 1]),
                     R=[f'r1_{b}', 'g2_all'], W=[f'r1_{b}'])
                k.op('dve', lambda e: e.scalar_tensor_tensor(out=r1[b][:], in0=r1[b][:], scalar=g_all[0][:, i:i + 1], in1=r2[b][:],
                                                             op0=ALU.mult, op1=ALU.add), R=[f'r0_{b}', f'r1_{b}', 'g1_all'], W=[f'r0_{b}'])
                k.op('dve', lambda e: e.scalar_tensor_tensor(out=xt[b][:], in0=xt[b][:], scalar=ALPHA, in1=r1[b][:],
                                                             op0=ALU.mult, op1=ALU.add), R=[f'cxt{b}', f'r0_{b}'], W=[f'cxt{b}'])
                tail = layernorm_tile(cx, xt[b], f'cxt{b}', xo[b][:], f'cxo{b}', gain, bias, gkeys, wk, sfx=str(b), mid=pend[0], defer=True)
                pend[0] = (lambda tail=tail, i=i, b=b: (tail(), k.dma('sp', X2[i * 128:(i + 1) * 128, :], xo[b][:], R=[f'cxo{b}'], W=[('X2', i)])))
            pend[0]()
    k.barrier()


def tile_to_xT(cx, X, xkey, xb, xbkey, ptr, ptrkey, xT, xTkey, cast_eng='pool', copy_eng='act'):
    k = cx.k
    if cast_eng == 'pool':
        k.op('pool', lambda e: e.tensor_copy(out=xb[:], in_=X), R=[xkey], W=[xbkey])
    else:
        k.op(cast_eng, lambda e: e.tensor_copy(out=xb[:], in_=X) if cast_eng == 'dve' else e.copy(out=xb[:], in_=X), R=[xkey], W=[xbkey])
    for kc in range(8):
        k.op('pe', lambda e: e.transpose(out=ptr[:, kc, :], in_=xb[:, kc * 128:(kc + 1) * 128], identity=cx.ident_b[:]),
             R=[xbkey, 'ident_b'], W=[ptrkey])
    if copy_eng == 'act':
        k.op('act', lambda e: e.copy(out=xT[:], in_=ptr[:]), R=[ptrkey], W=[xTkey])
    else:
        k.op('dve', lambda e: e.tensor_copy(out=xT[:], in_=ptr[:]), R=[ptrkey], W=[xTkey])


def host_dft_consts():
    a = np.arange(256)
    ang = 2.0 * np.pi * np.outer(a, a) / 256.0
    cc = (np.cos(ang) / 16.0).astype(np.float32)
    sc = (np.sin(ang) / 16.0).astype(np.float32)
    dftc = np.stack([cc.reshape(2, 128, 256), sc.reshape(2, 128, 256)], 2).transpose(1, 0, 2, 3).copy()
    j = np.arange(S // 2)
    kk = np.arange(S)
    jk = (np.outer(j, kk) % S).astype(np.float64)
    ang = 2.0 * np.pi * jk / S
    cs = (np.cos(ang) / 64.0)
    ss = (-np.sin(ang) / 64.0)
    NJ2 = NT // 2
    m = np.stack([cs, ss], 0).reshape(2, NJ2, 128, NT, 128)
    dfts = np.ascontiguousarray(m.transpose(3, 2, 0, 1, 4)).astype(ml_dtypes.bfloat16)
    cmid = (np.cos(np.pi * kk) / 64.0).reshape(1, S).astype(ml_dtypes.bfloat16)
    return dftc, dfts, cmid


def host_ridx():
    p = np.arange(128)[:, None]
    kt = np.arange(NT // 2 + 1)[None, :]
    return (S - kt * 128 - p).astype(np.int32)


def fnet_phase(cx, L, X, X1):
    nc, k = cx.nc, cx.k
    din = cx.din
    o = L // 2
    NH2 = NT // 2
    with ExitStack() as es:
        with ExitStack() as es1:
            sb1 = lambda n, s, d: es1.enter_context(cx.sbt(n, s, d, side='right'))
            wcs = [sb1("fWc", [128, 8, D], BF16), sb1("fWs", [128, 8, D], BF16)]
            with ExitStack() as es0:
                w32 = es0.enter_context(cx.sbt("fw32", [128, 8, D], F32))
                dc = es0.enter_context(cx.sbt("fdc", [128, 2, 2, 256], F32))
                pw = [es0.enter_context(cx.pst(f"fpw{j}", [128, 512], F32)) for j in range(2)]
                k.dma('sp', w32[:], din['w_out_fourier'][o].rearrange("(kc p) n -> p kc n", p=128), W=['fw32'])
                k.dma('sp', dc[:], cx.d_dftc, W=['fdc'])
                n = 0
                for t in range(2):
                    for fc in range(8):
                        g, ac = fc // 2, fc % 2
                        for hh in range(2):
                            P = pw[n % 2]
                            for a2 in range(2):
                                k.op('pe', lambda e: e.matmul(P[:], lhsT=dc[:, a2, t, ac * 128:(ac + 1) * 128],
                                                              rhs=w32[:, g * 2 + a2, hh * 512:(hh + 1) * 512], start=(a2 == 0), stop=(a2 == 1)),
                                     R=['fdc', 'fw32'], W=[f'fpw{n % 2}'])
                            if n % 2 == 0:
                                k.op('act', lambda e: e.copy(out=wcs[t][:, fc, hh * 512:(hh + 1) * 512], in_=P[:]), R=[f'fpw{n % 2}'], W=[f'fW{t}'])
                            else:
                                k.op('dve', lambda e: e.tensor_copy(out=wcs[t][:, fc, hh * 512:(hh + 1) * 512], in_=P[:]), R=[f'fpw{n % 2}'], W=[f'fW{t}'])
                            n += 1
                k.barrier()
            U_all = es.enter_context(cx.sbt("U_all", [128, NH2, D], BF16))
            V_all = es.enter_context(cx.sbt("V_all", [128, NH2, D], BF16))
            umid = es.enter_context(cx.sbt("umid", [1, D], BF16))
            xt = [[sb1(f"fxt{j}_{t}", [128, D], F32) for t in range(2)] for j in range(2)]
            xb = [[sb1(f"fxb{j}_{t}", [128, D], BF16) for t in range(2)] for j in range(2)]
            xTa = [sb1(f"fxTa{j}", [128, 8, 128], BF16) for j in range(2)]
            xTb = [sb1(f"fxTb{j}", [128, 8, 128], BF16) for j in range(2)]
            xTe = [sb1(f"fxTe{j}", [128, 8, 128], BF16) for j in range(2)]
            xTo = [sb1(f"fxTo{j}", [128, 8, 128], BF16) for j in range(2)]
            ptr = [[es1.enter_context(cx.pst(f"fptr{j}_{t}", [128, 8, 128], BF16)) for t in range(2)] for j in range(2)]
            pu = [es1.enter_context(cx.pst(f"fpu{j}", [128, 512], F32)) for j in range(4)]

            def stageA(i):
                b = i % 2
                for t, ti in enumerate([i, NT - 1 - i]):
                    k.dma('sp', xt[b][t][:], X[ti * 128:(ti + 1) * 128, :], W=[f'fxt{b}_{t}'])
                    tile_to_xT(cx, xt[b][t][:], f'fxt{b}_{t}', xb[b][t], f'fxb{b}_{t}', ptr[b][t], f'fptr{b}_{t}',
                               xTa[b] if t == 0 else xTb[b], f'fxT{"a" if t == 0 else "b"}{b}', cast_eng='act' if t == 0 else 'pool',
                               copy_eng='act' if t == 0 else 'dve')
                A_, B_ = xTa[b], xTb[b]
                E_, O_ = xTe[b], xTo[b]
                rkeys = [f'fxTa{b}', f'fxTb{b}']
                k.op('dve', lambda e: e.tensor_tensor(out=E_[:, :, 1:128], in0=A_[:, :, 1:128], in1=B_[:, :, 127:0:-1], op=ALU.add), R=rkeys, W=[f'fxTe{b}'])
                k.op('dve', lambda e: e.tensor_tensor(out=O_[:, :, 1:128], in0=A_[:, :, 1:128], in1=B_[:, :, 127:0:-1], op=ALU.subtract), R=rkeys, W=[f'fxTo{b}'])
                if i == 0:
                    k.op('dve', lambda e: e.tensor_copy(out=E_[:, :, 0:1], in_=A_[:, :, 0:1]), R=rkeys, W=[f'fxTe{b}'])
                    k.op('dve', lambda e: e.memset(O_[:, :, 0:1], 0.0), W=[f'fxTo{b}'])
                else:
                    Bp = xTb[1 - b]
                    k.op('dve', lambda e: e.tensor_tensor(out=E_[:, :, 0:1], in0=A_[:, :, 0:1], in1=Bp[:, :, 0:1], op=ALU.add), R=rkeys + [f'fxTb{1 - b}'], W=[f'fxTe{b}'])
                    k.op('dve', lambda e: e.tensor_tensor(out=O_[:, :, 0:1], in0=A_[:, :, 0:1], in1=Bp[:, :, 0:1], op=ALU.subtract), R=rkeys + [f'fxTb{1 - b}'], W=[f'fxTo{b}'])
            n = 0
            stageA(0)
            for i in range(NH2):
                b = i % 2
                if i + 1 < NH2:
                    stageA(i + 1)
                for t, (UV, XT) in enumerate([(U_all, xTe[b]), (V_all, xTo[b])]):
                    for hh in range(2):
                        P = pu[n % 4]
                        for kc in range(8):
                            k.op('pe', lambda e: e.matmul(P[:], lhsT=XT[:, kc, :], rhs=wcs[t][:, kc, hh * 512:(hh + 1) * 512],
                                                          start=(kc == 0), stop=(kc == 7)), R=[f'fxT{"e" if t == 0 else "o"}{b}', f'fW{t}'], W=[f'fpu{n % 4}'])
                        if n % 2 == 0:
                            k.op('act', lambda e: e.copy(out=UV[:, i, hh * 512:(hh + 1) * 512], in_=P[:]), R=[f'fpu{n % 4}'], W=[('UV', t, i)])
                        else:
                            k.op('dve', lambda e: e.tensor_copy(out=UV[:, i, hh * 512:(hh + 1) * 512], in_=P[:]), R=[f'fpu{n % 4}'], W=[('UV', t, i)])
                        n += 1
            bl = (NH2 - 1) % 2
            for hh in range(2):
                P = pu[n % 4]
                for kc in range(8):
                    k.op('pe', lambda e: e.matmul(P[0:1, :], lhsT=xTb[bl][:, kc, 0:1], rhs=wcs[0][:, kc, hh * 512:(hh + 1) * 512],
                                                  start=(kc == 0), stop=(kc == 7)), R=[f'fxTb{bl}', 'fW0'], W=[f'fpu{n % 4}'])
                k.op('act', lambda e: e.copy(out=umid[:, hh * 512:(hh + 1) * 512], in_=P[0:1, :]), R=[f'fpu{n % 4}'], W=['umid'])
                n += 1
        k.barrier()
        with ExitStack() as es2:
            sb2 = lambda n, s, d: es2.enter_context(cx.sbt(n, s, d, side='right'))
            cs = [sb2(f"fcs{j}", [128, 2, NH2, 128], BF16) for j in range(2)]
            cm = sb2("fcm", [1, S], BF16)
            k.dma('sp', cm[:], cx.d_cmid, W=['fcm'])
            xt = [sb2(f"gxt{j}", [128, D], F32) for j in range(2)]
            hh_ = [sb2(f"gh{j}", [128, D], F32) for j in range(2)]
            xo = [sb2(f"gxo{j}", [128, D], F32) for j in range(2)]
            pm = [es2.enter_context(cx.pst(f"fpm{j}", [128, 512], F32)) for j in range(4)]
            wk = ln_work(cx, es2, "ln1f")
            gain, bias, gkeys = ln_params(cx, es2, 'ln1', cx.lnL if hasattr(cx, 'lnL') else L)
            n = 0
            pend = [None]
            for kt in range(NT):
                b = kt % 2
                k.dma('sp', cs[b][:], cx.d_dfts[kt], W=[f'fcs{b}'])
                k.dma('sp', xt[b][:], X[kt * 128:(kt + 1) * 128, :], W=[f'gxt{b}'])
                for hh in range(2):
                    P = pm[n % 4]
                    for t, UV in enumerate([U_all, V_all]):
                        for jc in range(NH2):
                            k.op('pe', lambda e: e.matmul(P[:], lhsT=cs[b][:, t, jc, :], rhs=UV[:, jc, hh * 512:(hh + 1) * 512],
                                                          start=(t == 0 and jc == 0), stop=False),
                                 R=[f'fcs{b}', ('UV', t, jc)], W=[f'fpm{n % 4}'])
                    k.op('pe', lambda e: e.matmul(P[:], lhsT=cm[0:1, kt * 128:(kt + 1) * 128], rhs=umid[0:1, hh * 512:(hh + 1) * 512], start=False, stop=True),
                         R=['fcm', 'umid'], W=[f'fpm{n % 4}'])
                    k.op('dve', lambda e: e.scalar_tensor_tensor(out=hh_[b][:, hh * 512:(hh + 1) * 512], in0=xt[b][:, hh * 512:(hh + 1) * 512],
                                                                 scalar=ALPHA, in1=P[:], op0=ALU.mult, op1=ALU.add),
                         R=[f'gxt{b}', f'fpm{n % 4}'], W=[f'gh{b}'])
                    n += 1
                tail = layernorm_tile(cx, hh_[b], f'gh{b}', xo[b][:], f'gxo{b}', gain, bias, gkeys, wk, sfx=str(b), mid=pend[0], defer=True)
                pend[0] = (lambda tail=tail, kt=kt, b=b: (tail(), k.dma('pool', X1[kt * 128:(kt + 1) * 128, :], xo[b][:], R=[f'gxo{b}'], W=[('X1', kt)])))
            pend[0]()
    k.barrier()


QB_PERM = [half * 4 + c for c in range(4) for half in range(2)]


def host_even_layout(w_in_even, w_out_even):
    wi = np.array(w_in_even, copy=True)
    wo = np.array(w_out_even, copy=True)
    for p, hq in enumerate(QB_PERM):
        wi[:, :, 2048 + p * 64:2048 + (p + 1) * 64] = w_in_even[:, :, 2048 + hq * 64:2048 + (hq + 1) * 64]
        wo[:, 512 + p * 64:512 + (p + 1) * 64, :] = w_out_even[:, 512 + hq * 64:512 + (hq + 1) * 64, :]
    return wi, wo


def host_rope_consts():
    pos = np.arange(S, dtype=np.float64)
    out = np.zeros((S, 384), np.float32)

    def tab(half):
        inv = 10000.0 ** (-np.arange(half, dtype=np.float32) / half)
        ang = pos.astype(np.float32)[:, None] * inv[None, :]
        return np.cos(ang).astype(np.float32), np.sin(ang).astype(np.float32)
    ca, sa = tab(64)
    cb, sb_ = tab(32)
    out[:, 0:64] = ca; out[:, 64:128] = sa
    out[:, 128:192] = ca * np.float32(128 ** -0.5); out[:, 192:256] = sa * np.float32(128 ** -0.5)
    out[:, 256:288] = cb * np.float32(0.125); out[:, 288:320] = sb_ * np.float32(0.125)
    out[:, 320:352] = cb; out[:, 352:384] = sb_
    return out.reshape(NT, 128, 384)


def rope_tm(cx, eng, src, skey, H, hd, cos, sin, ckey, dst, dkey, t1, t2, tkey):
    k = cx.k
    sv = src.rearrange("p (h t d) -> p h t d", h=H, t=2)
    dv = dst.rearrange("p (h t d) -> p h t d", h=H, t=2)
    x1, x2 = sv[:, :, 0, :], sv[:, :, 1, :]
    cb = cos.unsqueeze(1).broadcast_to([128, H, hd])
    sb_ = sin.unsqueeze(1).broadcast_to([128, H, hd])
    a = t1.rearrange("p (h d) -> p h d", h=H)
    b = t2.rearrange("p (h d) -> p h d", h=H)
    tt = lambda o, i0, i1, op, R, W: k.op(eng, lambda e: e.tensor_tensor(out=o, in0=i0, in1=i1, op=op), R=R, W=W)
    tt(a, x1, cb, ALU.mult, [skey, ckey], [tkey + 'a'])
    tt(b, x2, sb_, ALU.mult, [skey, ckey], [tkey + 'b'])
    tt(dv[:, :, 0, :], a, b, ALU.subtract, [tkey + 'a', tkey + 'b'], [dkey])
    tt(a, x1, sb_, ALU.mult, [skey, ckey], [tkey + 'a'])
    tt(b, x2, cb, ALU.mult, [skey, ckey], [tkey + 'b'])
    tt(dv[:, :, 1, :], a, b, ALU.add, [tkey + 'a', tkey + 'b'], [dkey])


def even_phase(cx, L, X, X1):
    nc, k, din = cx.nc, cx.k, cx.din
    ev = L // 2
    SGA, YA = cx.SGA, cx.YA
    NH = 16

    def inproj_pass(w, wkey, ncols, blocks, epilogue, epilogue2, es1, sbr):
        xt = [sbr(f"ext{j}", [128, D], F32) for j in range(2)]
        xb = [sbr(f"exb{j}", [128, D], BF16) for j in range(2)]
        xT = [sbr(f"exT{j}", [128, 8, 128], BF16) for j in range(2)]
        rpt = [sbr(f"erp{j}", [128, 384], F32) for j in range(2)]
        ptr = [es1.enter_context(cx.pst(f"eptr{j}", [128, 8, 128], BF16)) for j in range(2)]
        pp = [es1.enter_context(cx.pst(f"epp{j}", [128, 512], F32)) for j in range(len(blocks))]

        def stageA(i):
            b = i % 2
            k.dma('sp', xt[b][:], X[i * 128:(i + 1) * 128, :], W=[f'ext{b}'])
            k.dma('sp', rpt[b][:], cx.d_rope[i], W=[f'erp{b}'])
            tile_to_xT(cx, xt[b][:], f'ext{b}', xb[b], f'exb{b}', ptr[b], f'eptr{b}', xT[b], f'exT{b}', cast_eng='act', copy_eng='dve')
        stageA(0)
        for i in range(NT):
            b = i % 2
            if i + 1 < NT:
                stageA(i + 1)
            for blk, (c0, cn) in enumerate(blocks):
                P = pp[blk]
                for kc in range(8):
                    k.op('pe', lambda e: e.matmul(P[:, 0:cn], lhsT=xT[b][:, kc, :], rhs=w[:, kc, c0:c0 + cn],
                                                  start=(kc == 0), stop=(kc == 7)), R=[f'exT{b}', wkey], W=[f'epp{blk}'])
                epilogue(i, b, blk, P, rpt[b], f'erp{b}')
            if i > 0:
                epilogue2(i - 1)
        epilogue2(NT - 1)

    with ExitStack() as esA:
        sbl = lambda n, s, d: esA.enter_context(cx.sbt(n, s, d))
        qaT = sbl("qaT", [128, 4, S], BF16)
        kaT = sbl("kaT", [128, 4, S], BF16)
        va_all = sbl("va_all", [128, NT, 512], BF16)
        with ExitStack() as es1:
            sbr = lambda n, s, d: es1.enter_context(cx.sbt(n, s, d, side='right'))
            w = sbr("ewA", [128, 8, 2048], BF16)
            for cb in range(4):
                k.dma('pool', w[:, :, cb * 512:(cb + 1) * 512],
                      din['w_in_even'][ev][:, cb * 512:(cb + 1) * 512].rearrange("(kc p) n -> p kc n", p=128), W=['ewA'])
            hs = [sbr(f"ehs{j}", [128, 512], F32) for j in range(2)]
            qr = [[sbr(f"eqr{j}_{t}", [128, 512], BF16) for t in range(2)] for j in range(2)]
            tq = [sbr(f"etq{j}", [128, 256], F32) for j in range(4)]
            sga = [sbr(f"esga{j}", [128, 512], BF16) for j in range(2)]
            ptq = [es1.enter_context(cx.pst(f"eptq{j}", [128, 4, 128], BF16)) for j in range(2)]

            def epiA2(i):
                t = i % 2
                for blk in range(2):
                    for h in range(4):
                        k.op('pe', lambda e: e.transpose(out=ptq[blk][:, h, :], in_=qr[blk][t][:, h * 128:(h + 1) * 128], identity=cx.ident_b[:]),
                             R=[f'eqr{blk}_{t}', 'ident_b'], W=[f'eptq{blk}'])
                    if blk == 0:
                        k.op('act', lambda e: e.copy(out=qaT[:, :, i * 128:(i + 1) * 128], in_=ptq[blk][:]), R=[f'eptq{blk}'], W=['qaT'])
                    else:
                        k.op('dve', lambda e: e.tensor_copy(out=kaT[:, :, i * 128:(i + 1) * 128], in_=ptq[blk][:]), R=[f'eptq{blk}'], W=['kaT'])

            def epiA(i, b, blk, P, rp, rpkey):
                if blk < 2:
                    t = i % 2
                    k.op('act', lambda e: e.copy(out=hs[blk][:], in_=P[:]), R=[f'epp{blk}'], W=[f'ehs{blk}'])
                    eng = 'dve' if blk == 0 else 'pool'
                    co = 0 if blk == 0 else 128
                    rope_tm(cx, eng, hs[blk][:], f'ehs{blk}', 4, 64, rp[:, co:co + 64], rp[:, co + 64:co + 128],
                            rpkey, qr[blk][t][:], f'eqr{blk}_{t}', tq[2 * blk][:], tq[2 * blk + 1][:], f'etq{blk}')
                elif blk == 2:
                    k.op('dve', lambda e: e.tensor_copy(out=va_all[:, i, :], in_=P[:]), R=[f'epp{blk}'], W=['va_all'])
                else:
                    k.op('act', lambda e: e.activation(out=sga[b][:], in_=P[:], func=AF.Silu), R=[f'epp{blk}'], W=[f'esga{b}'])
                    k.dma('pool', SGA[i * 128:(i + 1) * 128, :], sga[b][:], R=[f'esga{b}'], W=['SGA'])
            inproj_pass(w, 'ewA', 2048, [(0, 512), (512, 512), (1024, 512), (1536, 512)], epiA, epiA2, es1, sbr)
        k.barrier()

        with ExitStack() as es1:
            sbr = lambda n, s, d: es1.enter_context(cx.sbt(n, s, d, side='right'))
            lgt = sbr("rlg", [128, 8], F32)
            gng = sbr("rgng", [128, 512], F32)
            k.dma('sp', lgt[:], din['ret_decay_logit'][ev].rearrange("a h -> (a h)").partition_broadcast(128), W=['rlg'])
            k.dma('sp', gng[:], din['ret_gn_gain'][ev].partition_broadcast(128), W=['rgng'])
            k.op('act', lambda e: e.activation(out=lgt[:], in_=lgt[:], func=AF.Exp, scale=-1.0), R=['rlg'], W=['rlg'])
            k.op('dve', lambda e: e.tensor_scalar(out=lgt[:], in0=lgt[:], scalar1=1.0, scalar2=None, op0=ALU.add), R=['rlg'], W=['rlg'])
            k.op('act', lambda e: e.activation(out=lgt[:], in_=lgt[:], func=AF.Ln), R=['rlg'], W=['rlg'])
            k.op('dve', lambda e: e.tensor_scalar(out=lgt[:], in0=lgt[:], scalar1=-1.0, scalar2=None, op0=ALU.mult), R=['rlg'], W=['rlg'])
            DT = sbr("rDT", [128, 128], F32)
            XI = [sbr("rXIF", [128, 128], BF16), sbr("rXIB", [128, 128], BF16)]
            zc = sbr("rzc", [128, 4], F32)
            arg = sbr("rarg", [128, 128], F32)
            qs = [sbr("rqf", [128, S], BF16), sbr("rqb", [128, S], BF16)]
            Vz = [sbr("rVzf", [128, NT, 128], BF16), sbr("rVzb", [128, NT, 128], BF16)]
            ktm = sbr("rktm", [128, NT, 128], BF16)
            Rb_all = sbr("rRb_all", [128, NT, 128], BF16)
            R32 = [sbr("rRf32", [128, 128], F32), sbr("rRb32", [128, 128], F32)]
            Rfb = [sbr(f"rRfb{j}", [128, 128], BF16) for j in range(2)]
            Pm = [sbr(f"rPm{j}", [128, 128], BF16) for j in range(2)]
            Yraw = [sbr(f"rYraw{j}", [128, NH, 128], F32) for j in range(2)]
            Ysq = sbr("rYsq", [128, NH, 128], F32)
            sgs = [sbr(f"rsgs{j}", [128, NH, 128], BF16) for j in range(2)]
            yout = [sbr(f"ryout{j}", [128, NH, 128], BF16) for j in range(2)]
            gv = sbr("rgv", [128, 8, NH], F32)
            pk = [es1.enter_context(cx.pst(f"rpk{j}", [128, 8, 128], BF16)) for j in range(2)]
            pkv = [es1.enter_context(cx.pst(f"rpkv{j}", [128, 128], F32)) for j in range(2)]
            pst = [es1.enter_context(cx.pst(f"rpst{j}", [128, 128], F32)) for j in range(2)]
            py = [es1.enter_context(cx.pst(f"rpy{j}", [128, 128], F32)) for j in range(2)]
            pcol = cslice(cx, 'pcol')
            nslab = 0
            for h in range(4):
                lgf, lgb = lgt[:, h:h + 1], lgt[:, 4 + h:5 + h]
                k.op('dve', lambda e: e.tensor_scalar(out=arg[:], in0=cslice(cx, 'rp'), scalar1=lgf, scalar2=None, op0=ALU.mult),
                     R=['cst', 'rlg'], W=['rarg'])
                k.op('dve', lambda e: e.scalar_tensor_tensor(out=arg[:], in0=cslice(cx, 'rn'), scalar=lgb, in1=arg[:], op0=ALU.mult, op1=ALU.add),
                     R=['cst', 'rlg', 'rarg'], W=['rarg'])
                k.op('act', lambda e: e.activation(out=DT[:], in_=arg[:], func=AF.Exp), R=['rarg'], W=['rDT'])
                k.op('act', lambda e: e.activation(out=XI[0][:], in_=cslice(cx, 'cp1'), func=AF.Exp, scale=lgf), R=['cst', 'rlg'], W=['rXI0'])
                k.op('act', lambda e: e.activation(out=XI[1][:], in_=cslice(cx, 'cmc'), func=AF.Exp, scale=lgb), R=['cst', 'rlg'], W=['rXI1'])
                k.op('act', lambda e: e.activation(out=zc[:, 0:1], in_=pcol[:, 0:1], func=AF.Exp, scale=lgf), R=['cst', 'rlg'], W=['rzc'])
                k.op('act', lambda e: e.activation(out=zc[:, 1:2], in_=pcol[:, 1:2], func=AF.Exp, scale=lgb), R=['cst', 'rlg'], W=['rzc'])
                k.op('act', lambda e: e.activation(out=zc[:, 2:3], in_=pcol[:, 2:3], func=AF.Exp, scale=lgf), R=['cst', 'rlg'], W=['rzc'])
                k.op('act', lambda e: e.activation(out=zc[:, 3:4], in_=pcol[:, 2:3], func=AF.Exp, scale=lgb), R=['cst', 'rlg'], W=['rzc'])
                for d_ in range(2):
                    k.op('dve', lambda e: e.tensor_tensor(out=qs[d_][:].rearrange("p (n c) -> p n c", c=128),
                                                          in0=qaT[:, h, :].rearrange("p (n c) -> p n c", c=128),
                                                          in1=XI[d_][:].unsqueeze(1).broadcast_to([128, NT, 128]), op=ALU.mult),
                         R=['qaT', f'rXI{d_}'], W=[f'rqs{d_}'])
                    k.op('act', lambda e: e.activation(out=Vz[d_][:], in_=va_all[:, :, h * 128:(h + 1) * 128], func=AF.Identity, scale=zc[:, d_:d_ + 1]),
                         R=['va_all', 'rzc'], W=[f'rVz{d_}'])
                for g8 in range(4):
                    P = pk[g8 % 2]
                    for j in range(8):
                        n = g8 * 8 + j
                        k.op('pe', lambda e: e.transpose(out=P[:, j, :], in_=kaT[:, h, n * 128:(n + 1) * 128], identity=cx.ident_b[:]),
                             R=['kaT', 'ident_b'], W=[f'rpk{g8 % 2}'])
                    k.op('dve', lambda e: e.tensor_copy(out=ktm[:, g8 * 8:(g8 + 1) * 8, :], in_=P[:]), R=[f'rpk{g8 % 2}'], W=['rktm'])
                k.op('dve', lambda e: e.memset(R32[1][:], 0.0), W=['rR32_1'])
                k.op('dve', lambda e: e.memset(R32[0][:], 0.0), W=['rR32_0'])
                nkv = 0
                for n in range(NT - 1, 0, -1):
                    P = pkv[nkv % 2]
                    k.op('pe', lambda e: e.matmul(P[:], lhsT=ktm[:, n, :], rhs=Vz[1][:, n, :], start=True, stop=True),
                         R=['rktm', 'rVz1'], W=[f'rpkv{nkv % 2}'])
                    k.op('dve', lambda e: e.scalar_tensor_tensor(out=R32[1][:], in0=R32[1][:], scalar=zc[:, 3:4], in1=P[:], op0=ALU.mult, op1=ALU.add),
                         R=['rR32_1', 'rzc', f'rpkv{nkv % 2}'], W=['rR32_1'])
                    k.op('act', lambda e: e.copy(out=Rb_all[:, n - 1, :], in_=R32[1][:]), R=['rR32_1'], W=['rRb_all'])
                    nkv += 1
                def scores(n):
                    b = n % 2
                    cs_ = slice(n * 128, (n + 1) * 128)
                    k.op('pe', lambda e: e.matmul(pst[b][:], lhsT=kaT[:, h, cs_], rhs=qaT[:, h, cs_], start=True, stop=True),
                         R=['kaT', 'qaT'], W=[f'rpst{b}'])
                    k.op('dve', lambda e: e.tensor_tensor(out=Pm[b][:], in0=pst[b][:], in1=DT[:], op=ALU.mult), R=[f'rpst{b}', 'rDT'], W=[f'rPm{b}'])
                scores(0)
                for n in range(NT):
                    b = n % 2
                    sl = nslab % 2
                    cs_ = slice(n * 128, (n + 1) * 128)
                    if n % NH == 0:
                        k.dma('sp', sgs[sl][:], SGA[n * 128:(n + NH) * 128, h * 128:(h + 1) * 128].rearrange("(j p) c -> p j c", p=128),
                              R=['SGA'], W=[f'rsgs{sl}'])
                    if n + 1 < NT:
                        scores(n + 1)
                    last = 'intra'
                    if n < NT - 1:
                        last = 'bwd'
                    elif n > 0:
                        last = 'fwd'
                    k.op('pe', lambda e: e.matmul(py[b][:], lhsT=Pm[b][:], rhs=va_all[:, n, h * 128:(h + 1) * 128], start=True, stop=(last == 'intra')),
                         R=[f'rPm{b}', 'va_all'], W=[f'rpy{b}'])
                    if n > 0:
                        k.op('pe', lambda e: e.matmul(py[b][:], lhsT=qs[0][:, cs_], rhs=Rfb[(n - 1) % 2][:], start=False, stop=(last == 'fwd')),
                             R=['rqs0', f'rRfb{(n - 1) % 2}'], W=[f'rpy{b}'])
                    if n < NT - 1:
                        k.op('pe', lambda e: e.matmul(py[b][:], lhsT=qs[1][:, cs_], rhs=Rb_all[:, n, :], start=False, stop=True),
                             R=['rqs1', 'rRb_all'], W=[f'rpy{b}'])
                    k.op('act', lambda e: e.copy(out=Yraw[sl][:, n % NH, :], in_=py[b][:]), R=[f'rpy{b}'], W=[f'rYraw{sl}'])
                    if n < NT - 1:
                        P = pkv[nkv % 2]
                        k.op('pe', lambda e: e.matmul(P[:], lhsT=ktm[:, n, :], rhs=Vz[0][:, n, :], start=True, stop=True),
                             R=['rktm', 'rVz0'], W=[f'rpkv{nkv % 2}'])
                        k.op('dve', lambda e: e.scalar_tensor_tensor(out=R32[0][:], in0=R32[0][:], scalar=zc[:, 2:3], in1=P[:], op0=ALU.mult, op1=ALU.add),
                             R=['rR32_0', 'rzc', f'rpkv{nkv % 2}'], W=['rR32_0'])
                        k.op('act', lambda e: e.copy(out=Rfb[n % 2][:], in_=R32[0][:]), R=['rR32_0'], W=[f'rRfb{n % 2}'])
                        nkv += 1
                    if n % NH == NH - 1:
                        Y = Yraw[sl]
                        G = lambda j: gv[:, j, :]
                        bc = lambda ap: ap.unsqueeze(2).broadcast_to([128, NH, 128])
                        yk = f'rYraw{sl}'
                        k.op('dve', lambda e: e.tensor_reduce(out=G(0), in_=Y[:], axis=AX.X, op=ALU.add), R=[yk], W=['rg0'])
                        k.op('act', lambda e: e.activation(out=Ysq[:], in_=Y[:], func=AF.Square), R=[yk], W=['rYsq'])
                        k.op('dve', lambda e: e.tensor_reduce(out=G(1), in_=Ysq[:], axis=AX.X, op=ALU.add), R=['rYsq'], W=['rg1'])
                        k.op('dve', lambda e: e.tensor_scalar(out=G(2), in0=G(0), scalar1=1.0 / 128, scalar2=None, op0=ALU.mult), R=['rg0'], W=['rg2'])
                        k.op('dve', lambda e: e.tensor_tensor(out=G(3), in0=G(2), in1=G(2), op=ALU.mult), R=['rg2'], W=['rg3'])
                        k.op('dve', lambda e: e.scalar_tensor_tensor(out=G(4), in0=G(1), scalar=1.0 / 128, in1=G(3), op0=ALU.mult, op1=ALU.subtract),
                             R=['rg1', 'rg3'], W=['rg4'])
                        k.op('act', lambda e: e.activation(out=G(5), in_=G(4), func=AF.Sqrt, bias=cx.eps_gn[:], scale=1.0), R=['rg4', 'eps_gn'], W=['rg5'])
                        k.op('dve', lambda e: e.reciprocal(out=G(6), in_=G(5)), R=['rg5'], W=['rg6'])
                        k.op('dve', lambda e: e.tensor_tensor(out=Y[:], in0=Y[:], in1=bc(G(2)), op=ALU.subtract), R=[yk, 'rg2'], W=[yk])
                        k.op('dve', lambda e: e.tensor_tensor(out=Y[:], in0=Y[:], in1=bc(G(6)), op=ALU.mult), R=[yk, 'rg6'], W=[yk])
                        k.op('dve', lambda e: e.tensor_tensor(out=Y[:], in0=Y[:], in1=gng[:, h * 128:(h + 1) * 128].unsqueeze(1).broadcast_to([128, NH, 128]),
                                                              op=ALU.mult), R=[yk, 'rgng'], W=[yk])
                        k.op('dve', lambda e: e.tensor_tensor(out=yout[sl][:], in0=Y[:], in1=sgs[sl][:], op=ALU.mult), R=[yk, f'rsgs{sl}'], W=[f'ryout{sl}'])
                        n0 = n - (NH - 1)
                        k.dma('sp', YA[n0 * 128:(n0 + NH) * 128, h * 128:(h + 1) * 128].rearrange("(j p) c -> p j c", p=128), yout[sl][:],
                              R=[f'ryout{sl}'], W=['YA'])
                        nslab += 1
        k.barrier()

    with ExitStack() as esB:
        sbl = lambda n, s, d: esB.enter_context(cx.sbt(n, s, d))
        yb_all = sbl("yb_all", [128, NT, 512], BF16)
        with ExitStack() as esB2:
            sbl2 = lambda n, s, d: esB2.enter_context(cx.sbt(n, s, d))
            qbT = sbl2("qbT", [128, 4, S], BF16)
            kbT = sbl2("kbT", [128, S], BF16)
            vb_all = sbl2("vb_all", [128, NT, 128], BF16)
            with ExitStack() as es1:
                sbr = lambda n, s, d: es1.enter_context(cx.sbt(n, s, d, side='right'))
                w = sbr("ewB", [128, 8, 768], BF16)
                for cb in range(2):
                    k.dma('pool', w[:, :, cb * 384:(cb + 1) * 384],
                          din['w_in_even'][ev][:, 2048 + cb * 384:2048 + (cb + 1) * 384].rearrange("(kc p) n -> p kc n", p=128), W=['ewB'])
                hs = [sbr(f"ehs{j}", [128, 512], F32) for j in range(2)]
                qr = [[sbr(f"eqr{j}_{t}", [128, 512], BF16) for t in range(2)] for j in range(2)]
                tq = [sbr(f"etq{j}", [128, 256], F32) for j in range(4)]
                ptq = [es1.enter_context(cx.pst(f"eptq{j}", [128, 4, 128], BF16)) for j in range(2)]

                def epiB2(i):
                    t = i % 2
                    for c in range(4):
                        k.op('pe', lambda e: e.transpose(out=ptq[0][:, c, :], in_=qr[0][t][:, c * 128:(c + 1) * 128], identity=cx.ident_b[:]),
                             R=[f'eqr0_{t}', 'ident_b'], W=['eptq0'])
                    k.op('act', lambda e: e.copy(out=qbT[:, :, i * 128:(i + 1) * 128], in_=ptq[0][:]), R=['eptq0'], W=['qbT'])
                    k.op('pe', lambda e: e.transpose(out=ptq[1][:, 0, :], in_=qr[1][t][:, 0:128], identity=cx.ident_b[:]),
                         R=[f'eqr1_{t}', 'ident_b'], W=['eptq1'])
                    k.op('dve', lambda e: e.tensor_copy(out=kbT[:, i * 128:(i + 1) * 128], in_=ptq[1][:, 0, :]), R=['eptq1'], W=['kbT'])

                def epiB(i, b, blk, P, rp, rpkey):
                    t = i % 2
                    cn = 512 if blk == 0 else 256
                    k.op('act', lambda e: e.copy(out=hs[blk][:, 0:cn], in_=P[:, 0:cn]), R=[f'epp{blk}'], W=[f'ehs{blk}'])
                    if blk == 0:
                        rope_tm(cx, 'dve', hs[0][:], 'ehs0', 8, 32, rp[:, 256:288], rp[:, 288:320], rpkey,
                                qr[0][t][:], f'eqr0_{t}', tq[0][:], tq[1][:], 'etq0')
                    else:
                        rope_tm(cx, 'pool', hs[1][:, 0:128], 'ehs1', 2, 32, rp[:, 320:352], rp[:, 352:384], rpkey,
                                qr[1][t][:, 0:128], f'eqr1_{t}', tq[2][:, 0:64], tq[3][:, 0:64], 'etq1')
                        k.op('dve', lambda e: e.tensor_copy(out=vb_all[:, i, :], in_=hs[1][:, 128:256]), R=['ehs1'], W=['vb_all'])
                inproj_pass(w, 'ewB', 768, [(0, 512), (512, 256)], epiB, epiB2, es1, sbr)
            k.barrier()

            with ExitStack() as es1:
                sbr = lambda n, s, d: es1.enter_context(cx.sbt(n, s, d, side='right'))
                sk = sbr("ask", [128, 8], F32)
                k.dma('sp', sk[:], din['sink_logit'][ev].partition_broadcast(128), W=['ask'])
                sm4 = [sbr(f"asm{j}", [128, 4, 384], F32) for j in range(2)]
                pb4 = [sbr(f"apb{j}", [128, 4, 384], BF16) for j in range(2)]
                pT4 = [sbr(f"apT{j}", [128, 12, 128], BF16) for j in range(2)]
                a8 = [sbr(f"aa8{j}", [128, 8, 4], F32) for j in range(2)]
                ps4 = es1.enter_context(cx.pst("aps4", [128, 4, 512], F32))
                ppT = es1.enter_context(cx.pst("appT", [128, 12, 128], BF16))
                po = es1.enter_context(cx.pst("apo", [128, 4, 64], F32))
                mk = cslice(cx, 'mk')
                steps = [(n, half) for n in range(NT) for half in range(2)]

                def geom(n):
                    j0 = max(n - 1, 0)
                    j1 = min(n + 1, NT - 1)
                    return j0, j1, j1 - j0 + 1, (j0 - (n - 1)) * 128

                def att_F1(it):
                    n, half = steps[it]
                    j0, j1, nk, mo = geom(n)
                    W_ = nk * 128
                    b = it % 2
                    pl = slice(64 * half, 64 * half + 64)
                    A = lambda j: a8[b][:, j, :]
                    skh = sk[:, half * 4:half * 4 + 4]
                    for c in range(4):
                        k.op('pe', lambda e: e.matmul(ps4[:, c, 0:W_], lhsT=qbT[pl, c, n * 128:(n + 1) * 128], rhs=kbT[pl, j0 * 128:(j1 + 1) * 128],
                                                      start=True, stop=True), R=['qbT', 'kbT'], W=['aps4'])
                    k.op('dve', lambda e: e.tensor_tensor(out=sm4[b][:, :, 0:W_], in0=ps4[:, :, 0:W_],
                                                          in1=mk[:, mo:mo + W_].unsqueeze(1).broadcast_to([128, 4, W_]), op=ALU.add),
                         R=['aps4', 'cst'], W=[f'asm{b}'])
                    k.op('dve', lambda e: e.tensor_reduce(out=A(0), in_=sm4[b][:, :, 0:W_], axis=AX.X, op=ALU.max), R=[f'asm{b}'], W=[f'aa{b}0'])
                    k.op('dve', lambda e: e.tensor_tensor(out=A(0), in0=A(0), in1=skh, op=ALU.max), R=[f'aa{b}0', 'ask'], W=[f'aa{b}0'])
                    k.op('dve', lambda e: e.tensor_scalar(out=A(1), in0=A(0), scalar1=-1.0, scalar2=None, op0=ALU.mult), R=[f'aa{b}0'], W=[f'aa{b}1'])
                    k.op('dve', lambda e: e.tensor_tensor(out=A(3), in0=skh, in1=A(0), op=ALU.subtract), R=['ask', f'aa{b}0'], W=[f'aa{b}3'])

                def att_F2(it):
                    n, half = steps[it]
                    j0, j1, nk, mo = geom(n)
                    W_ = nk * 128
                    b = it % 2
                    A = lambda j: a8[b][:, j, :]
                    for c in range(4):
                        k.op('act', lambda e: e.activation(out=pb4[b][:, c, 0:W_], in_=sm4[b][:, c, 0:W_], func=AF.Exp, bias=a8[b][:, 1, c:c + 1], scale=1.0,
                                                           accum_out=a8[b][:, 2, c:c + 1]), R=[f'asm{b}', f'aa{b}1'], W=[f'apb{b}', f'aa{b}2'])
                    k.op('act', lambda e: e.activation(out=A(3), in_=A(3), func=AF.Exp), R=[f'aa{b}3'], W=[f'aa{b}3'])

                def att_B1(it):
                    n, half = steps[it]
                    j0, j1, nk, mo = geom(n)
                    b = it % 2
                    for c in range(4):
                        for kb_ in range(nk):
                            k.op('pe', lambda e: e.transpose(out=ppT[:, c * 3 + kb_, :], in_=pb4[b][:, c, kb_ * 128:(kb_ + 1) * 128], identity=cx.ident_b[:]),
                                 R=[f'apb{b}', 'ident_b'], W=['appT'])
                    if nk == 3:
                        k.op('act', lambda e: e.copy(out=pT4[b][:, 0:6, :], in_=ppT[:, 0:6, :]), R=['appT'], W=[f'apT{b}'])
                        k.op('act', lambda e: e.copy(out=pT4[b][:, 6:12, :], in_=ppT[:, 6:12, :]), R=['appT'], W=[f'apT{b}'])
                    else:
                        for c in range(4):
                            k.op('act', lambda e: e.copy(out=pT4[b][:, c * 3:c * 3 + nk, :], in_=ppT[:, c * 3:c * 3 + nk, :]), R=['appT'], W=[f'apT{b}'])

                def att_B2(it):
                    n, half = steps[it]
                    j0, j1, nk, mo = geom(n)
                    b = it % 2
                    A = lambda j: a8[b][:, j, :]
                    for c in range(4):
                        for kb_ in range(nk):
                            k.op('pe', lambda e: e.matmul(po[:, c, :], lhsT=pT4[b][:, c * 3 + kb_, :], rhs=vb_all[:, j0 + kb_, half * 64:(half + 1) * 64],
                                                          start=(kb_ == 0), stop=(kb_ == nk - 1)), R=[f'apT{b}', 'vb_all'], W=['apo'])
                    k.op('dve', lambda e: e.tensor_tensor(out=A(4), in0=A(2), in1=A(3), op=ALU.add), R=[f'aa{b}2', f'aa{b}3'], W=[f'aa{b}4'])
                    k.op('dve', lambda e: e.reciprocal(out=A(5), in_=A(4)), R=[f'aa{b}4'], W=[f'aa{b}5'])
                    k.op('dve', lambda e: e.tensor_tensor(out=yb_all[:, n, :].rearrange("p (c t d) -> p c t d", c=4, t=2)[:, :, half, :], in0=po[:],
                                                          in1=A(5).unsqueeze(2).broadcast_to([128, 4, 64]), op=ALU.mult),
                         R=['apo', f'aa{b}5'], W=['yb_all'])
                att_F1(0)
                att_F2(0)
                for it in range(len(steps)):
                    if it + 1 < len(steps):
                        att_F1(it + 1)
                    att_B1(it)
                    if it + 1 < len(steps):
                        att_F2(it + 1)
                    att_B2(it)
            k.barrier()

        with ExitStack() as es1:
            sbr = lambda n, s, d: es1.enter_context(cx.sbt(n, s, d, side='right'))
            wo = sbr("ewo", [128, 8, D], BF16)
            for cb in range(2):
                k.dma('pool', wo[:, :, cb * 512:(cb + 1) * 512],
                      din['w_out_even'][ev][:, cb * 512:(cb + 1) * 512].rearrange("(kc p) n -> p kc n", p=128), W=['ewo'])
            NB = 3
            xt = [sbr(f"oxt{j}", [128, D], F32) for j in range(NB)]
            yat = [sbr(f"oya{j}", [128, 512], BF16) for j in range(NB)]
            hh_ = [sbr(f"oh{j}", [128, D], F32) for j in range(NB)]
            xo = [sbr(f"oxo{j}", [128, D], F32) for j in range(NB)]
            yT = [sbr(f"oyT{j}", [128, 8, 128], BF16) for j in range(2)]
            ptr = [es1.enter_context(cx.pst(f"optr{j}", [128, 8, 128], BF16)) for j in range(2)]
            pm = [es1.enter_context(cx.pst(f"opm{j}", [128, 512], F32)) for j in range(4)]
            wk = ln_work(cx, es1, "ln1e")
            gain, bias, gkeys = ln_params(cx, es1, 'ln1', cx.lnL if hasattr(cx, 'lnL') else L)

            def stageA(i):
                b = i % NB
                b2 = i % 2
                k.dma('sp', xt[b][:], X[i * 128:(i + 1) * 128, :], W=[f'oxt{b}'])
                k.dma('sp', yat[b][:], YA[i * 128:(i + 1) * 128, :], R=['YA'], W=[f'oya{b}'])
                for kc in range(8):
                    src = yat[b][:, kc * 128:(kc + 1) * 128] if kc < 4 else yb_all[:, i, (kc - 4) * 128:(kc - 3) * 128]
                    k.op('pe', lambda e: e.transpose(out=ptr[b2][:, kc, :], in_=src, identity=cx.ident_b[:]),
                         R=[f'oya{b}', 'yb_all', 'ident_b'], W=[f'optr{b2}'])
                k.op('act', lambda e: e.copy(out=yT[b2][:], in_=ptr[b2][:]), R=[f'optr{b2}'], W=[f'oyT{b2}'])
            nn = 0
            pend = [None]
            stageA(0)
            for i in range(NT):
                b = i % NB
                b2 = i % 2
                if i + 1 < NT:
                    stageA(i + 1)
                for hh in range(2):
                    P = pm[nn % 4]
                    for kc in range(8):
                        k.op('pe', lambda e: e.matmul(P[:], lhsT=yT[b2][:, kc, :], rhs=wo[:, kc, hh * 512:(hh + 1) * 512], start=(kc == 0), stop=(kc == 7)),
                             R=[f'oyT{b2}', 'ewo'], W=[f'opm{nn % 4}'])
                    k.op('dve', lambda e: e.scalar_tensor_tensor(out=hh_[b][:, hh * 512:(hh + 1) * 512], in0=xt[b][:, hh * 512:(hh + 1) * 512],
                                                                 scalar=ALPHA, in1=P[:], op0=ALU.mult, op1=ALU.add),
                         R=[f'oxt{b}', f'opm{nn % 4}'], W=[f'oh{b}'])
                    nn += 1
                tail = layernorm_tile(cx, hh_[b], f'oh{b}', xo[b][:], f'oxo{b}', gain, bias, gkeys, wk, sfx=str(b), mid=pend[0], defer=True)
                pend[0] = (lambda tail=tail, i=i, b=b: (tail(), k.dma('pool', X1[i * 128:(i + 1) * 128, :], xo[b][:], R=[f'oxo{b}'], W=[('X1', i)])))
            pend[0]()
    k.barrier()


W_SHAPES = {
    'w_in_even': (2, 1024, IN_EVEN), 'ret_decay_logit': (2, 2, 4), 'ret_gn_gain': (2, 512), 'sink_logit': (2, 8),
    'w_out_even': (2, 1024, 1024), 'w_out_fourier': (2, 1024, 1024),
    'ln1_gain': (4, 1024), 'ln1_bias': (4, 1024), 'ln2_gain': (4, 1024), 'ln2_bias': (4, 1024),
    'router_coarse_w': (4, 1024, 4), 'router_coarse_b': (4, 4), 'router_fine_w': (4, 1024, 32), 'router_fine_b': (4, 32),
    'expert_w_gate': (4, 32, 1024, 512), 'expert_w_up': (4, 32, 1024, 512), 'expert_w_down': (4, 32, 512, 1024),
}


def build_program():
    nc = bass.Bass("TRN2", target_bir_lowering=False)
    cx = Ctx()
    cx.nc = nc
    cx.k = KB(nc)

    def din(name, shape, dt=F32):
        return nc.dram_tensor(name, list(shape), dt, kind="ExternalInput").ap()
    cx.din = {nm: din(nm, shp) for nm, shp in W_SHAPES.items()}
    x_in = din('x', (S, D))
    cx.d_cst = din('cst', (128, CST_N))
    cx.d_rope = din('rope', (NT, 128, 384))
    cx.d_dftc = din('dftc', (128, 2, 2, 256))
    cx.d_dfts = din('dfts', (NT, 128, 2, NT // 2, 128), BF16)
    cx.d_cmid = din('cmid', (1, S), BF16)
    cx.d_ridx = nc.dram_tensor('ridx', [128, NT // 2 + 1], I32, kind='ExternalInput').ap()
    out = nc.dram_tensor("out", [S, D], F32, kind="ExternalOutput").ap()
    XA = nc.dram_tensor("XA", [S, D], F32, kind="Internal").ap()
    XB = nc.dram_tensor("XB", [S, D], F32, kind="Internal").ap()
    cx.XE = nc.dram_tensor("XE", [NE * CAP, D], BF16, kind="Internal").ap()
    cx.YE = nc.dram_tensor("YE", [NE * CAP, D], F32, kind="Internal").ap()
    cx.SGA = nc.dram_tensor("SGA", [S, 512], BF16, kind="Internal").ap()
    cx.YA = nc.dram_tensor("YA", [S, 512], BF16, kind="Internal").ap()
    load_common(cx)
    cur = x_in
    for L in range(DEPTH):
        if L % 2 == 0:
            even_phase(cx, L, cur, XA)
        else:
            fnet_phase(cx, L, cur, XA)
        dst = out if L == DEPTH - 1 else XB
        moe_phase(cx, L, XA, dst)
        cur = dst
    cx.k.finish_all()
    return nc


def kernel(**inputs):
    inp = {k_: np.ascontiguousarray(np.asarray(v, dtype=np.float32)) for k_, v in inputs.items()}
    B = inp['x'].shape[0]
    wi, wo = host_even_layout(inp['w_in_even'], inp['w_out_even'])
    dftc, dfts, cmid = host_dft_consts()
    shared = {nm: inp[nm] for nm in W_SHAPES}
    shared['w_in_even'] = wi
    shared['w_out_even'] = wo
    shared['cst'] = host_consts()
    shared['rope'] = host_rope_consts()
    shared['dftc'] = dftc
    shared['dfts'] = dfts
    shared['cmid'] = cmid
    shared['ridx'] = host_ridx()
    nc = build_program()
    in_maps = []
    for c in range(B):
        m = dict(shared)
        m['x'] = inp['x'][c]
        in_maps.append(m)
    res = run_bass_kernel_spmd(nc, in_maps, core_ids=list(range(B)))
    return np.stack([np.asarray(res.results[c]['out'], dtype=np.float32) for c in range(B)], 0)
```

```python
from contextlib import ExitStack
import numpy as np
import ml_dtypes
import concourse.bass as bass
import concourse.mybir as mybir
from concourse.bass_utils import run_bass_kernel_spmd

F32 = mybir.dt.float32; BF16 = mybir.dt.bfloat16; I32 = mybir.dt.int32
AF = mybir.ActivationFunctionType; ALU = mybir.AluOpType; AX = mybir.AxisListType

S = 4096; D = 1024; NT = S // 128; DEPTH = 4
NE = 32; HID = 512; CAP = 384
ALPHA = (2.0 * DEPTH) ** 0.25
LN_EPS = 1e-5; GN_EPS = 1e-6
IN_EVEN = 2816


class KB:
    def __init__(self, nc, n_dma_sems=48, same_engine_sync=True):
        self.nc = nc
        self.eng = {'pe': nc.tensor, 'act': nc.scalar, 'dve': nc.vector, 'pool': nc.gpsimd, 'sp': nc.sync}
        self.sem = {e: nc.alloc_semaphore(f"s_{e}") for e in ['pe', 'act', 'dve', 'pool']}
        self.seq = {e: 0 for e in self.sem}
        self.waited = {e: {} for e in self.eng}
        self.dma_sems = [nc.alloc_semaphore(f"d{i}") for i in range(n_dma_sems)]
        self.dma_cnt = [0] * n_dma_sems
        self.dma_next = 0
        self.lastw = {}
        self.readers = {}
        self.same = same_engine_sync
        self.nwaits = 0
        self.nops = 0

    def _wait(self, e, tok):
        semkey, sem, val = tok
        w = self.waited[e]
        if w.get(semkey, 0) >= val:
            return
        self.eng[e].wait_ge(sem, val)
        w[semkey] = val
        self.nwaits += 1

    def _deps(self, e, R, W):
        best = {}

        def add(t):
            if t[0] == e and (e == 'pe' or not self.same):
                return
            if t[0] not in best or best[t[0]][2] < t[2]:
                best[t[0]] = t
        for r in R:
            for t in self.lastw.get(r, {}).values():
                add(t)
        for w_ in W:
            for t in self.lastw.get(w_, {}).values():
                add(t)
            for t in self.readers.get(w_, {}).values():
                add(t)
        for t in best.values():
            self._wait(e, t)

    def _commit(self, tok, R, W):
        for r in R:
            d = self.readers.setdefault(r, {})
            if tok[0] not in d or d[tok[0]][2] < tok[2]:
                d[tok[0]] = tok
        for w_ in W:
            self.lastw.setdefault(w_, {})[tok[0]] = tok
            self.readers[w_] = {}

    def op(self, e, fn, R=(), W=()):
        self._deps(e, R, W)
        ins = fn(self.eng[e])
        self.seq[e] += 1
        ins.then_inc(self.sem[e], 1)
        self._commit((e, self.sem[e], self.seq[e]), R, W)
        self.nops += 1
        return ins

    def dma(self, q, out, in_, R=(), W=(), indirect=None, **kw):
        i = self.dma_next
        self.dma_next = (i + 1) % len(self.dma_sems)
        sem = self.dma_sems[i]
        if self.dma_cnt[i] > 0:
            self._wait(q, (('d', i), sem, 16 * self.dma_cnt[i]))
        self._deps(q, R, W)
        if indirect is None:
            ins = self.eng[q].dma_start(out=out, in_=in_, **kw)
        else:
            ins = self.eng[q].indirect_dma_start(out=out, in_=in_, **indirect, **kw)
        self.dma_cnt[i] += 1
        ins.then_inc(sem, 16)
        self._commit((('d', i), sem, 16 * self.dma_cnt[i]), R, W)
        self.nops += 1
        return ins

    def finish_all(self):
        toks = [(e, self.sem[e], self.seq[e]) for e in self.sem if self.seq[e] > 0]
        toks += [(('d', i), s, 16 * c) for i, (s, c) in enumerate(zip(self.dma_sems, self.dma_cnt)) if c > 0]
        for t in toks:
            self._wait('sp', t)

    def barrier(self):
        toks = [(e, self.sem[e], self.seq[e]) for e in self.sem if self.seq[e] > 0]
        toks += [(('d', i), s, 16 * c) for i, (s, c) in enumerate(zip(self.dma_sems, self.dma_cnt)) if c > 0]
        for e in self.eng:
            for t in toks:
                if t[0] != e or e != 'pe':
                    self._wait(e, t)
        self.lastw = {}
        self.readers = {}


CST_COLS = {}


def _cst_layout():
    off = 0
    for name, n in [('ident', 128), ('ltri', 128), ('ones', 128), ('ecap', 1024), ('rp', 128), ('rn', 128),
                    ('cp1', 128), ('cmc', 128), ('pcol', 4), ('mk', 384)]:
        CST_COLS[name] = (off, n)
        off += n
    return off


CST_N = _cst_layout()


def host_consts():
    c = np.zeros((128, CST_N), np.float32)
    p = np.arange(128)

    def put(name, arr):
        o, n = CST_COLS[name]
        c[:, o:o + n] = arr
    put('ident', np.eye(128))
    put('ltri', (p[:, None] < p[None, :]).astype(np.float32))
    put('ones', np.ones((128, 128)))
    put('ecap', np.tile((np.arange(NE) * CAP)[None, :], (128, NT)))
    dif = (p[None, :] - p[:, None]).astype(np.float32)
    put('rp', np.maximum(dif, 0.0))
    put('rn', np.maximum(-dif, 0.0))
    jj = np.arange(384)
    put('mk', np.where((jj[None, :] >= p[:, None]) & (jj[None, :] <= p[:, None] + 256), 0.0, -30000.0))
    put('cp1', np.tile((p + 1.0)[None, :], (128, 1)))
    put('cmc', np.tile((128.0 - p)[None, :], (128, 1)))
    pc = np.stack([127.0 - p, p.astype(np.float64), np.full(128, 128.0), np.zeros(128)], 1)
    put('pcol', pc)
    return c


class Ctx:
    uid = 0

    def sbt(self, name, shape, dt, **kw):
        Ctx.uid += 1
        return self.nc.sbuf_tensor(f"{name}_u{Ctx.uid}", shape, dt, **kw)

    def pst(self, name, shape, dt, **kw):
        Ctx.uid += 1
        return self.nc.psum_tensor(f"{name}_u{Ctx.uid}", shape, dt, **kw)


def cslice(cx, name):
    o, n = CST_COLS[name]
    return cx.cst[:, o:o + n]


def load_common(cx):
    nc, k = cx.nc, cx.k
    cx.cst = nc.alloc_sbuf_tensor("cst_sb", [128, CST_N], F32)
    k.dma('sp', cx.cst[:], cx.d_cst, W=['cst'])
    cx.ident_b = nc.alloc_sbuf_tensor("ident_b", [128, 128], BF16)
    k.op('dve', lambda e: e.tensor_copy(out=cx.ident_b[:], in_=cslice(cx, 'ident')), R=['cst'], W=['ident_b'])
    cx.bound_reg = nc.gpsimd.to_reg(NE * CAP - 1)
    cx.bound_reg_s = nc.gpsimd.to_reg(S - 1)
    cx.eps_ln = nc.alloc_sbuf_tensor("eps_ln", [128, 1], F32)
    k.op('dve', lambda e: e.memset(cx.eps_ln[:], LN_EPS), W=['eps_ln'])
    cx.eps_gn = nc.alloc_sbuf_tensor("eps_gn", [128, 1], F32)
    k.op('dve', lambda e: e.memset(cx.eps_gn[:], GN_EPS), W=['eps_gn'])


def layernorm_tile(cx, h, hkey, out, okey, gain, bias, gkeys, wk, sfx='', mid=None, defer=False, hout=None, houtkey=None):
    k = cx.k
    j = (int(sfx) % 4) if sfx else 0
    st, mv, sc = wk['st'][:, j, :], wk['mv'][:, j, :], wk['sc'][:, j, :]
    K_ = lambda n: n + str(j)
    if hout is None:
        hout, houtkey = h, hkey
    k.op('dve', lambda e: e.bn_stats(out=st[:, 0:6], in_=h[:, 0:512]), R=[hkey], W=[K_('ln_st')])
    k.op('dve', lambda e: e.bn_stats(out=st[:, 6:12], in_=h[:, 512:1024]), R=[hkey], W=[K_('ln_st')])
    k.op('dve', lambda e: e.bn_aggr(out=mv, in_=st), R=[K_('ln_st')], W=[K_('ln_mv')])
    k.op('act', lambda e: e.activation(out=sc[:, 0:1], in_=mv[:, 1:2], func=AF.Sqrt, bias=cx.eps_ln[:], scale=1.0),
         R=[K_('ln_mv'), 'eps_ln'], W=[K_('ln_sd')])
    if mid is not None:
        mid()
    k.op('dve', lambda e: e.reciprocal(out=sc[:, 1:2], in_=sc[:, 0:1]), R=[K_('ln_sd')], W=[K_('ln_rstd')])
    k.op('dve', lambda e: e.scalar_tensor_tensor(out=sc[:, 2:3], in0=mv[:, 0:1], scalar=-1.0, in1=sc[:, 1:2],
                                                 op0=ALU.mult, op1=ALU.mult), R=[K_('ln_mv'), K_('ln_rstd')], W=[K_('ln_nmr')])
    k.op('act', lambda e: e.activation(out=hout[:], in_=h[:], func=AF.Identity, bias=sc[:, 2:3], scale=sc[:, 1:2]),
         R=[hkey, K_('ln_rstd'), K_('ln_nmr')], W=[houtkey])

    def tail():
        k.op('dve', lambda e: e.tensor_tensor(out=hout[:], in0=hout[:], in1=gain, op=ALU.mult), R=[houtkey] + gkeys, W=[houtkey])
        k.op('pool', lambda e: e.tensor_tensor(out=out, in0=hout[:], in1=bias, op=ALU.add), R=[houtkey] + gkeys, W=[okey])
    if defer:
        return tail
    tail()
    return None


def layernorm_gen(cx, h, hkey, out, okey, gain, bias, gkeys, wk, slot):
    k = cx.k
    j = slot % 4
    st, mv, sc = wk['st'][:, j, :], wk['mv'][:, j, :], wk['sc'][:, j, :]
    K_ = lambda n: n + str(j)
    k.op('dve', lambda e: e.bn_stats(out=st[:, 0:6], in_=h[:, 0:512]), R=[hkey], W=[K_('ln_st')])
    yield
    k.op('dve', lambda e: e.bn_stats(out=st[:, 6:12], in_=h[:, 512:1024]), R=[hkey], W=[K_('ln_st')])
    yield
    k.op('dve', lambda e: e.bn_aggr(out=mv, in_=st), R=[K_('ln_st')], W=[K_('ln_mv')])
    yield
    k.op('act', lambda e: e.activation(out=sc[:, 0:1], in_=mv[:, 1:2], func=AF.Sqrt, bias=cx.eps_ln[:], scale=1.0),
         R=[K_('ln_mv'), 'eps_ln'], W=[K_('ln_sd')])
    yield
    k.op('dve', lambda e: e.reciprocal(out=sc[:, 1:2], in_=sc[:, 0:1]), R=[K_('ln_sd')], W=[K_('ln_rstd')])
    yield
    k.op('dve', lambda e: e.scalar_tensor_tensor(out=sc[:, 2:3], in0=mv[:, 0:1], scalar=-1.0, in1=sc[:, 1:2],
                                                 op0=ALU.mult, op1=ALU.mult), R=[K_('ln_mv'), K_('ln_rstd')], W=[K_('ln_nmr')])
    yield
    k.op('act', lambda e: e.activation(out=h[:], in_=h[:], func=AF.Identity, bias=sc[:, 2:3], scale=sc[:, 1:2]),
         R=[hkey, K_('ln_rstd'), K_('ln_nmr')], W=[hkey])
    yield
    k.op('dve', lambda e: e.tensor_tensor(out=h[:], in0=h[:], in1=gain, op=ALU.mult), R=[hkey] + gkeys, W=[hkey])
    yield
    k.op('pool', lambda e: e.tensor_tensor(out=out, in0=h[:], in1=bias, op=ALU.add), R=[hkey] + gkeys, W=[okey])
    yield


def interleave(gens):
    gens = list(gens)
    while gens:
        for g in list(gens):
            try:
                next(g)
            except StopIteration:
                gens.remove(g)


def ln_params(cx, es, which, L):
    nc, k = cx.nc, cx.k
    g = es.enter_context(cx.sbt(f"{which}_g_sb", [128, D], F32))
    b = es.enter_context(cx.sbt(f"{which}_b_sb", [128, D], F32))
    k.dma('sp', g[:], cx.din[which + '_gain'][L].partition_broadcast(128), W=[which + '_g'])
    k.dma('sp', b[:], cx.din[which + '_bias'][L].partition_broadcast(128), W=[which + '_b'])
    return g[:], b[:], [which + '_g', which + '_b']


def ln_work(cx, es, pfx):
    nc = cx.nc
    return {'st': es.enter_context(cx.sbt(pfx + "_st", [128, 4, 12], F32)),
            'mv': es.enter_context(cx.sbt(pfx + "_mv", [128, 4, 2], F32)),
            'sc': es.enter_context(cx.sbt(pfx + "_sc", [128, 4, 4], F32))}


def moe_phase(cx, L, X1, X2):
    nc, k = cx.nc, cx.k
    din = cx.din
    XE, YE = cx.XE, cx.YE
    NJ = CAP // 128
    NW = 4
    with ExitStack() as es:
        sb = lambda n, s, d: es.enter_context(cx.sbt(n, s, d))
        g_all = [sb("g1_all", [128, NT], F32), sb("g2_all", [128, NT], F32)]
        dsti = [sb("dsti0", [128, NT], I32), sb("dsti1", [128, NT], I32)]
        wg = [sb(f"wg{j}", [128, 8, HID], BF16) for j in range(NW - 1)]
        wu = [sb(f"wu{j}", [128, 8, HID], BF16) for j in range(NW - 1)]
        wd = [sb(f"wd{j}", [128, 4, D], BF16) for j in range(NW - 1)]

        def LW(ex):
            wb = ex % NW
            k.dma('pool', wg[wb][:], din['expert_w_gate'][L, ex].rearrange("(kc p) n -> p kc n", p=128), W=[f'wg{wb}'])
            k.dma('pool', wu[wb][:], din['expert_w_up'][L, ex].rearrange("(kc p) n -> p kc n", p=128), W=[f'wu{wb}'])
            k.dma('pool', wd[wb][:], din['expert_w_down'][L, ex].rearrange("(kc p) n -> p kc n", p=128), W=[f'wd{wb}'])
        LW(0)
        LW(1)
        LW(2)
        ident = cslice(cx, 'ident')

        with ExitStack() as esx:
            sb2 = lambda n, s, d: esx.enter_context(cx.sbt(n, s, d))
            xb_all = sb2("xb_all", [128, NT, D], BF16)
            lgc = sb2("lgc", [128, NT, 4], F32)
            lgf = sb2("lgf", [128, NT * NE], F32)
            wr = sb2("wr", [128, 8, 36], F32)
            rb = sb2("rb", [128, 36], F32)
            k.dma('sp', wr[:, :, 0:4], din['router_coarse_w'][L].rearrange("(kc p) n -> p kc n", p=128), W=['wr'])
            k.dma('sp', wr[:, :, 4:36], din['router_fine_w'][L].rearrange("(kc p) n -> p kc n", p=128), W=['wr'])
            k.dma('sp', rb[:, 0:4], din['router_coarse_b'][L].partition_broadcast(128), W=['rb'])
            k.dma('sp', rb[:, 4:36], din['router_fine_b'][L].partition_broadcast(128), W=['rb'])
            NB = 4
            xt = [sb2(f"mxt{j}", [128, D], F32) for j in range(NB)]
            xT32 = [sb2(f"mxT{j}", [128, 8, 128], F32) for j in range(2)]
            pst = [esx.enter_context(cx.pst(f"pst{j}", [128, 4, 128], F32)) for j in range(4)]
            psl = [esx.enter_context(cx.pst(f"psl{j}", [128, 36], F32)) for j in range(2)]
            GT = 8
            NG = NT // GT
            N1 = GT * NE
            oh1 = sb2("oh1", [128, N1], F32)
            oh2 = sb2("oh2", [128, N1], F32)
            sel = sb2("sel", [128, N1], F32)
            fm = sb2("fm", [128, N1], F32)
            fm2 = sb2("fm2", [128, N1], F32)
            posw = sb2("posw", [128, N1], F32)
            cum = [sb2("cumA", [128, N1], F32), sb2("cumB", [128, N1], F32)]
            tot = sb2("tot", [128, N1], F32)
            base = sb2("base", [128, NE], F32)
            sc4 = sb2("sc4", [128, GT, 4], F32)
            pen = sb2("pen", [128, GT, 4], F32)
            v = sb2("rv", [128, 8, GT], F32)
            dstf = sb2("dstf", [128, 2 * GT], F32)
            psw = esx.enter_context(cx.pst("psw", [128, N1], F32))
            pso = esx.enter_context(cx.pst("pso", [128, N1], F32))
            k.op('dve', lambda e: e.memset(base[:], 0.0), W=['base'])
            dv = lambda fn, R, W: k.op('dve', fn, R=R, W=W)

            def router_tile(i):
                b = i % NB
                b2 = i % 2
                X, XT = xt[b], xT32[b2]
                k.dma('sp', X[:], X1[i * 128:(i + 1) * 128, :], W=[f'mxt{b}'])
                k.op('act', lambda e: e.copy(out=xb_all[:, i, :], in_=X[:]), R=[f'mxt{b}'], W=[('xb', i)])
                for hh in range(2):
                    pi = (i % 2) * 2 + hh
                    P = pst[pi]
                    for j in range(4):
                        kc = hh * 4 + j
                        k.op('pe', lambda e: e.transpose(out=P[:, j, :], in_=X[:, kc * 128:(kc + 1) * 128], identity=ident),
                             R=[f'mxt{b}', 'cst'], W=[f'pst{pi}'])
                    if hh == 0:
                        k.op('act', lambda e: e.copy(out=XT[:, 0:4, :], in_=P[:]), R=[f'pst{pi}'], W=[f'mxT{b2}a'])
                    else:
                        k.op('dve', lambda e: e.tensor_copy(out=XT[:, 4:8, :], in_=P[:]), R=[f'pst{pi}'], W=[f'mxT{b2}b'])
                PL = psl[b2]
                for kc in range(8):
                    k.op('pe', lambda e: e.matmul(PL[:, :], lhsT=XT[:, kc, :], rhs=wr[:, kc, :], start=(kc == 0), stop=(kc == 7)),
                         R=[f'mxT{b2}a', f'mxT{b2}b', 'wr'], W=[f'psl{b2}'])
                k.op('dve', lambda e: e.tensor_tensor(out=lgc[:, i, :], in0=PL[:, 0:4], in1=rb[:, 0:4], op=ALU.add), R=[f'psl{b2}', 'rb'], W=['lgc'])
                k.op('dve', lambda e: e.tensor_tensor(out=lgf[:, i * NE:(i + 1) * NE], in0=PL[:, 4:36], in1=rb[:, 4:36], op=ALU.add),
                     R=[f'psl{b2}', 'rb'], W=['lgf'])

            def route_group(g):
                t0 = g * GT
                ts = slice(t0, t0 + GT)
                LC = lgc[:, ts, :]
                LF = lgf[:, t0 * NE:(t0 + GT) * NE]
                V = lambda j: v[:, j, :]
                b3 = lambda ap, n: ap.unsqueeze(2).broadcast_to([128, GT, n])
                dv(lambda e: e.tensor_reduce(out=V(0), in_=LC, axis=AX.X, op=ALU.max), ['lgc'], ['v0'])
                dv(lambda e: e.tensor_tensor(out=sc4[:], in0=LC, in1=b3(V(0), 4), op=ALU.subtract), ['lgc', 'v0'], ['sc4'])
                dv(lambda e: e.tensor_scalar(out=pen[:], in0=sc4[:], scalar1=0.0, scalar2=None, op0=ALU.is_equal), ['sc4'], ['pen'])
                dv(lambda e: e.tensor_scalar(out=pen[:], in0=pen[:], scalar1=1.0, scalar2=1e30, op0=ALU.subtract, op1=ALU.mult), ['pen'], ['pen'])
                k.op('act', lambda e: e.activation(out=sc4[:], in_=sc4[:], func=AF.Exp), R=['sc4'], W=['sc4'])
                dv(lambda e: e.tensor_reduce(out=V(1), in_=sc4[:], axis=AX.X, op=ALU.add), ['sc4'], ['v1'])
                dv(lambda e: e.reciprocal(out=V(2), in_=V(1)), ['v1'], ['v2'])
                dv(lambda e: e.tensor_tensor(out=fm[:].rearrange("p (a j) -> p a j", j=8), in0=LF.rearrange("p (a j) -> p a j", j=8),
                                             in1=pen[:].rearrange("p i g -> p (i g)").unsqueeze(2).broadcast_to([128, GT * 4, 8]), op=ALU.add),
                   ['lgf', 'pen'], ['fm'])
                fm3 = fm[:].rearrange("p (i e) -> p i e", e=NE)
                dv(lambda e: e.tensor_reduce(out=V(3), in_=fm3, axis=AX.X, op=ALU.max), ['fm'], ['v3'])
                dv(lambda e: e.tensor_tensor(out=oh1[:].rearrange("p (i e) -> p i e", e=NE), in0=fm3, in1=b3(V(3), NE), op=ALU.is_equal), ['fm', 'v3'], ['oh1'])
                dv(lambda e: e.scalar_tensor_tensor(out=fm2[:], in0=oh1[:], scalar=-1e30, in1=fm[:], op0=ALU.mult, op1=ALU.add), ['oh1', 'fm'], ['fm2'])
                fm23 = fm2[:].rearrange("p (i e) -> p i e", e=NE)
                dv(lambda e: e.tensor_reduce(out=V(4), in_=fm23, axis=AX.X, op=ALU.max), ['fm2'], ['v4'])
                dv(lambda e: e.tensor_tensor(out=oh2[:].rearrange("p (i e) -> p i e", e=NE), in0=fm23, in1=b3(V(4), NE), op=ALU.is_equal), ['fm2', 'v4'], ['oh2'])
                dv(lambda e: e.tensor_tensor(out=sel[:], in0=oh1[:], in1=oh2[:], op=ALU.add), ['oh1', 'oh2'], ['sel'])
                dv(lambda e: e.tensor_tensor(out=V(5), in0=V(4), in1=V(3), op=ALU.subtract), ['v3', 'v4'], ['v5'])
                k.op('act', lambda e: e.activation(out=V(6), in_=V(5), func=AF.Exp), R=['v5'], W=['v6'])
                dv(lambda e: e.tensor_scalar(out=V(7), in0=V(6), scalar1=1.0, scalar2=None, op0=ALU.add), ['v6'], ['v7'])
                dv(lambda e: e.reciprocal(out=V(7), in_=V(7)), ['v7'], ['v7'])
                dv(lambda e: e.tensor_tensor(out=g_all[0][:, ts], in0=V(2), in1=V(7), op=ALU.mult), ['v2', 'v7'], ['g1_all'])
                dv(lambda e: e.tensor_tensor(out=g_all[1][:, ts], in0=g_all[0][:, ts], in1=V(6), op=ALU.mult), ['g1_all', 'v6'], ['g2_all'])
                k.op('pe', lambda e: e.matmul(psw[:], lhsT=cslice(cx, 'ltri'), rhs=sel[:], start=True, stop=True), R=['sel', 'cst'], W=['psw'])
                k.op('pe', lambda e: e.matmul(pso[:], lhsT=cslice(cx, 'ones'), rhs=sel[:], start=True, stop=True), R=['sel', 'cst'], W=['pso'])
                k.op('act', lambda e: e.copy(out=cum[0][:], in_=pso[:]), R=['pso'], W=['cum0'])
                k.op('act', lambda e: e.copy(out=tot[:], in_=pso[:]), R=['pso'], W=['tot'])
                cur = 0
                sh = 1
                while sh < GT:
                    a_, b_ = cum[cur], cum[1 - cur]
                    dv(lambda e: e.tensor_copy(out=b_[:, 0:sh * NE], in_=a_[:, 0:sh * NE]), [f'cum{cur}'], [f'cum{1 - cur}'])
                    dv(lambda e: e.tensor_tensor(out=b_[:, sh * NE:], in0=a_[:, sh * NE:], in1=a_[:, 0:N1 - sh * NE], op=ALU.add),
                       [f'cum{cur}'], [f'cum{1 - cur}'])
                    cur = 1 - cur
                    sh *= 2
                inc = cum[cur]
                dv(lambda e: e.tensor_tensor(out=posw[:], in0=psw[:], in1=inc[:], op=ALU.add), ['psw', f'cum{cur}'], ['posw'])
                dv(lambda e: e.tensor_tensor(out=posw[:], in0=posw[:], in1=tot[:], op=ALU.subtract), ['posw', 'tot'], ['posw'])
                dv(lambda e: e.tensor_tensor(out=posw[:].rearrange("p (i e) -> p i e", e=NE), in0=posw[:].rearrange("p (i e) -> p i e", e=NE),
                                             in1=base[:].unsqueeze(1).broadcast_to([128, GT, NE]), op=ALU.add), ['posw', 'base'], ['posw'])
                dv(lambda e: e.tensor_tensor(out=base[:], in0=base[:], in1=inc[:, (GT - 1) * NE:GT * NE], op=ALU.add), ['base', f'cum{cur}'], ['base'])
                dv(lambda e: e.tensor_scalar(out=fm[:], in0=posw[:], scalar1=float(CAP), scalar2=1e6, op0=ALU.is_ge, op1=ALU.mult), ['posw', 'fm'], ['fm'])
                dv(lambda e: e.tensor_tensor(out=posw[:], in0=posw[:], in1=fm[:], op=ALU.add), ['posw', 'fm'], ['posw'])
                dv(lambda e: e.tensor_tensor(out=posw[:], in0=posw[:], in1=cslice(cx, 'ecap')[:, 0:N1], op=ALU.add), ['posw', 'cst'], ['posw'])
                for s_, oh in enumerate([oh1, oh2]):
                    dv(lambda e: e.tensor_tensor(out=fm2[:], in0=oh[:], in1=posw[:], op=ALU.mult), ['oh1', 'oh2', 'posw', 'fm2'], ['fm2'])
                    dv(lambda e: e.tensor_reduce(out=dstf[:, s_ * GT:(s_ + 1) * GT], in_=fm2[:].rearrange("p (i e) -> p i e", e=NE),
                                                 axis=AX.X, op=ALU.add), ['fm2'], ['dstf'])
                    dv(lambda e: e.tensor_copy(out=dsti[s_][:, ts], in_=dstf[:, s_ * GT:(s_ + 1) * GT]), ['dstf'], [f'dsti{s_}'])
                for i in range(t0, t0 + GT):
                    for s_ in range(2):
                        k.dma('pool', XE, xb_all[:, i, :], R=[('xb', i), f'dsti{s_}'], W=['XE'],
                              indirect=dict(out_offset=bass.IndirectOffsetOnAxis(ap=dsti[s_][:, i:i + 1], axis=0), in_offset=None,
                                            bounds_check=cx.bound_reg, oob_is_err=False))

            for g in range(NG):
                for i in range(g * GT, (g + 1) * GT):
                    router_tile(i)
                route_group(g)
        k.barrier()

        with ExitStack() as es2:
            sb2 = lambda n, s, d: es2.enter_context(cx.sbt(n, s, d))
            wg.append(sb2(f"wg{NW - 1}", [128, 8, HID], BF16))
            wu.append(sb2(f"wu{NW - 1}", [128, 8, HID], BF16))
            wd.append(sb2(f"wd{NW - 1}", [128, 4, D], BF16))
            xea = [sb2(f"xea{j}", [128, NJ, D], BF16) for j in range(2)]
            xeT = [sb2(f"xeT{j}", [128, 8, CAP], BF16) for j in range(2)]
            sg = [sb2(f"sg{j}", [128, CAP], F32) for j in range(2)]
            hid = [sb2(f"hid{j}", [128, 4, CAP], BF16) for j in range(2)]
            yo = [sb2(f"yo{j}", [128, NJ, D], F32) for j in range(2)]
            ptr = [es2.enter_context(cx.pst(f"ptr{j}", [128, 8, 128], BF16)) for j in range(2)]
            psg = [es2.enter_context(cx.pst(f"psg{j}", [128, CAP], F32)) for j in range(2)]
            psu = [es2.enter_context(cx.pst(f"psu{j}", [128, CAP], F32)) for j in range(2)]
            psy = [es2.enter_context(cx.pst(f"psy{j}", [128, 512], F32)) for j in range(2)]

            def LX(ex):
                k.dma('sp', xea[ex % 2][:], XE[ex * CAP:(ex + 1) * CAP, :].rearrange("(j p) d -> p j d", p=128), R=['XE'], W=[f'xea{ex % 2}'])

            def TGU(ex):
                wb = ex % NW
                XT = xeT[ex % 2]
                H = hid[ex % 2]
                XA = xea[ex % 2]
                for jt in range(NJ):
                    b2 = jt % 2
                    for kc in range(8):
                        k.op('pe', lambda e: e.transpose(out=ptr[b2][:, kc, :], in_=XA[:, jt, kc * 128:(kc + 1) * 128], identity=cx.ident_b[:]),
                             R=[f'xea{ex % 2}', 'ident_b'], W=[f'ptr{b2}'])
                    if b2 == 0:
                        k.op('act', lambda e: e.copy(out=XT[:, :, jt * 128:(jt + 1) * 128], in_=ptr[b2][:]), R=[f'ptr{b2}'], W=[f'xeT{ex % 2}'])
                    else:
                        k.op('dve', lambda e: e.tensor_copy(out=XT[:, :, jt * 128:(jt + 1) * 128], in_=ptr[b2][:]), R=[f'ptr{b2}'], W=[f'xeT{ex % 2}'])
                for hc in range(4):
                    b = hc % 2
                    for kc in range(8):
                        k.op('pe', lambda e: e.matmul(psg[b][:], lhsT=wg[wb][:, kc, hc * 128:(hc + 1) * 128], rhs=XT[:, kc, :],
                                                      start=(kc == 0), stop=(kc == 7)), R=[f'wg{wb}', f'xeT{ex % 2}'], W=[f'psg{b}'])
                    for kc in range(8):
                        k.op('pe', lambda e: e.matmul(psu[b][:], lhsT=wu[wb][:, kc, hc * 128:(hc + 1) * 128], rhs=XT[:, kc, :],
                                                      start=(kc == 0), stop=(kc == 7)), R=[f'wu{wb}', f'xeT{ex % 2}'], W=[f'psu{b}'])
                    k.op('act', lambda e: e.activation(out=sg[b][:], in_=psg[b][:], func=AF.Silu), R=[f'psg{b}'], W=[f'sg{b}'])
                    k.op('dve', lambda e: e.tensor_tensor(out=H[:, hc, :], in0=psu[b][:], in1=sg[b][:], op=ALU.mult),
                         R=[f'psu{b}', f'sg{b}'], W=[f'hid{ex % 2}'])

            def DN(ex):
                wb = ex % NW
                H = hid[ex % 2]
                YO = yo[ex % 2]
                for jt in range(NJ):
                    for hh in range(2):
                        for hc in range(4):
                            k.op('pe', lambda e: e.matmul(psy[hh][:], lhsT=H[:, hc, jt * 128:(jt + 1) * 128],
                                                          rhs=wd[wb][:, hc, hh * 512:(hh + 1) * 512], start=(hc == 0), stop=(hc == 3)),
                                 R=[f'hid{ex % 2}', f'wd{wb}'], W=[f'psy{hh}'])
                        if hh == 0:
                            k.op('act', lambda e: e.copy(out=YO[:, jt, 0:512], in_=psy[0][:]), R=['psy0'], W=[f'yo{ex % 2}'])
                        else:
                            k.op('dve', lambda e: e.tensor_copy(out=YO[:, jt, 512:1024], in_=psy[1][:]), R=['psy1'], W=[f'yo{ex % 2}'])
                k.dma('sp', YE[ex * CAP:(ex + 1) * CAP, :].rearrange("(j p) d -> p j d", p=128), YO[:], R=[f'yo{ex % 2}'], W=['YE'])

            LX(0)
            LX(1)
            TGU(0)
            for ex in range(NE):
                if ex + 3 < NE:
                    LW(ex + 3)
                if ex + 2 < NE and ex >= 0:
                    pass
                if ex + 1 < NE:
                    TGU(ex + 1)
                if ex + 2 < NE:
                    LX(ex + 2)
                DN(ex)
        k.barrier()

        with ExitStack() as es2:
            sb2 = lambda n, s, d: es2.enter_context(cx.sbt(n, s, d))
            NB = 5
            r1 = [sb2(f"r1_{j}", [128, D], F32) for j in range(NB)]
            r2 = [sb2(f"r2_{j}", [128, D], F32) for j in range(NB)]
            xt = [sb2(f"cxt{j}", [128, D], F32) for j in range(NB)]
            xo = [sb2(f"cxo{j}", [128, D], F32) for j in range(NB)]
            wk = ln_work(cx, es2, "ln2")
            gain, bias, gkeys = ln_params(cx, es2, 'ln2', cx.lnL if hasattr(cx, 'lnL') else L)
            def issue_loads(i):
                b = i % NB
                k.dma('sp', xt[b][:], X1[i * 128:(i + 1) * 128, :], W=[f'cxt{b}'])
                for s_, r in enumerate([r1[b], r2[b]]):
                    k.dma('pool', r[:], YE, R=['YE', f'dsti{s_}'], W=[f'r{s_}_{b}'],
                          indirect=dict(out_offset=None, in_offset=bass.IndirectOffsetOnAxis(ap=dsti[s_][:, i:i + 1], axis=0),
                                        bounds_check=cx.bound_reg, oob_is_err=False))
            for i in range(NB - 2):
                issue_loads(i)
            pend = [None]
            for i in range(NT):
                b = i % NB
                if i + NB - 2 < NT:
                    issue_loads(i + NB - 2)
                k.op('act', lambda e: e.activation(out=r2[b][:], in_=r2[b][:], func=AF.Identity, scale=g_all[1][:, i:i + 1]),
                     R=[f'r1_{b}', 'g2_all'], W=[f'r1_{b}'])
                k.op('dve', lambda e: e.scalar_tensor_tensor(out=r1[b][:], in0=r1[b][:], scalar=g_all[0][:, i:i + 1], in1=r2[b][:],
                                                             op0=ALU.mult, op1=ALU.add), R=[f'r0_{b}', f'r1_{b}', 'g1_all'], W=[f'r0_{b}'])
                k.op('dve', lambda e: e.scalar_tensor_tensor(out=xt[b][:], in0=xt[b][:], scalar=ALPHA, in1=r1[b][:],
                                                             op0=ALU.mult, op1=ALU.add), R=[f'cxt{b}', f'r0_{b}'], W=[f'cxt{b}'])
                tail = layernorm_tile(cx, xt[b], f'cxt{b}', xo[b][:], f'cxo{b}', gain, bias, gkeys, wk, sfx=str(b), mid=pend[0], defer=True)
                pend[0] = (lambda tail=tail, i=i, b=b: (tail(), k.dma('sp', X2[i * 128:(i + 1) * 128, :], xo[b][:], R=[f'cxo{b}'], W=[('X2', i)])))
            pend[0]()
    k.barrier()


def tile_to_xT(cx, X, xkey, xb, xbkey, ptr, ptrkey, xT, xTkey, cast_eng='pool', copy_eng='act'):
    k = cx.k
    if cast_eng == 'pool':
        k.op('pool', lambda e: e.tensor_copy(out=xb[:], in_=X), R=[xkey], W=[xbkey])
    else:
        k.op(cast_eng, lambda e: e.tensor_copy(out=xb[:], in_=X) if cast_eng == 'dve' else e.copy(out=xb[:], in_=X), R=[xkey], W=[xbkey])
    for kc in range(8):
        k.op('pe', lambda e: e.transpose(out=ptr[:, kc, :], in_=xb[:, kc * 128:(kc + 1) * 128], identity=cx.ident_b[:]),
             R=[xbkey, 'ident_b'], W=[ptrkey])
    if copy_eng == 'act':
        k.op('act', lambda e: e.copy(out=xT[:], in_=ptr[:]), R=[ptrkey], W=[xTkey])
    else:
        k.op('dve', lambda e: e.tensor_copy(out=xT[:], in_=ptr[:]), R=[ptrkey], W=[xTkey])


def host_dft_consts():
    a = np.arange(256)
    ang = 2.0 * np.pi * np.outer(a, a) / 256.0
    cc = (np.cos(ang) / 16.0).astype(np.float32)
    sc = (np.sin(ang) / 16.0).astype(np.float32)
    dftc = np.stack([cc.reshape(2, 128, 256), sc.reshape(2, 128, 256)], 2).transpose(1, 0, 2, 3).copy()
    j = np.arange(S // 2)
    kk = np.arange(S)
    jk = (np.outer(j, kk) % S).astype(np.float64)
    ang = 2.0 * np.pi * jk / S
    cs = (np.cos(ang) / 64.0)
    ss = (-np.sin(ang) / 64.0)
    NJ2 = NT // 2
    m = np.stack([cs, ss], 0).reshape(2, NJ2, 128, NT, 128)
    dfts = np.ascontiguousarray(m.transpose(3, 2, 0, 1, 4)).astype(ml_dtypes.bfloat16)
    cmid = (np.cos(np.pi * kk) / 64.0).reshape(1, S).astype(ml_dtypes.bfloat16)
    return dftc, dfts, cmid


def host_ridx():
    p = np.arange(128)[:, None]
    kt = np.arange(NT // 2 + 1)[None, :]
    return (S - kt * 128 - p).astype(np.int32)


def fnet_phase(cx, L, X, X1):
    nc, k = cx.nc, cx.k
    din = cx.din
    o = L // 2
    NH2 = NT // 2
    with ExitStack() as es:
        with ExitStack() as es1:
            sb1 = lambda n, s, d: es1.enter_context(cx.sbt(n, s, d, side='right'))
            wcs = [sb1("fWc", [128, 8, D], BF16), sb1("fWs", [128, 8, D], BF16)]
            with ExitStack() as es0:
                w32 = es0.enter_context(cx.sbt("fw32", [128, 8, D], F32))
                dc = es0.enter_context(cx.sbt("fdc", [128, 2, 2, 256], F32))
                pw = [es0.enter_context(cx.pst(f"fpw{j}", [128, 512], F32)) for j in range(2)]
                k.dma('sp', w32[:], din['w_out_fourier'][o].rearrange("(kc p) n -> p kc n", p=128), W=['fw32'])
                k.dma('sp', dc[:], cx.d_dftc, W=['fdc'])
                n = 0
                for t in range(2):
                    for fc in range(8):
                        g, ac = fc // 2, fc % 2
                        for hh in range(2):
                            P = pw[n % 2]
                            for a2 in range(2):
                                k.op('pe', lambda e: e.matmul(P[:], lhsT=dc[:, a2, t, ac * 128:(ac + 1) * 128],
                                                              rhs=w32[:, g * 2 + a2, hh * 512:(hh + 1) * 512], start=(a2 == 0), stop=(a2 == 1)),
                                     R=['fdc', 'fw32'], W=[f'fpw{n % 2}'])
                            if n % 2 == 0:
                                k.op('act', lambda e: e.copy(out=wcs[t][:, fc, hh * 512:(hh + 1) * 512], in_=P[:]), R=[f'fpw{n % 2}'], W=[f'fW{t}'])
                            else:
                                k.op('dve', lambda e: e.tensor_copy(out=wcs[t][:, fc, hh * 512:(hh + 1) * 512], in_=P[:]), R=[f'fpw{n % 2}'], W=[f'fW{t}'])
                            n += 1
                k.barrier()
            U_all = es.enter_context(cx.sbt("U_all", [128, NH2, D], BF16))
            V_all = es.enter_context(cx.sbt("V_all", [128, NH2, D], BF16))
            umid = es.enter_context(cx.sbt("umid", [1, D], BF16))
            xt = [[sb1(f"fxt{j}_{t}", [128, D], F32) for t in range(2)] for j in range(2)]
            xb = [[sb1(f"fxb{j}_{t}", [128, D], BF16) for t in range(2)] for j in range(2)]
            xTa = [sb1(f"fxTa{j}", [128, 8, 128], BF16) for j in range(2)]
            xTb = [sb1(f"fxTb{j}", [128, 8, 128], BF16) for j in range(2)]
            xTe = [sb1(f"fxTe{j}", [128, 8, 128], BF16) for j in range(2)]
            xTo = [sb1(f"fxTo{j}", [128, 8, 128], BF16) for j in range(2)]
            ptr = [[es1.enter_context(cx.pst(f"fptr{j}_{t}", [128, 8, 128], BF16)) for t in range(2)] for j in range(2)]
            pu = [es1.enter_context(cx.pst(f"fpu{j}", [128, 512], F32)) for j in range(4)]

            def stageA(i):
                b = i % 2
                for t, ti in enumerate([i, NT - 1 - i]):
                    k.dma('sp', xt[b][t][:], X[ti * 128:(ti + 1) * 128, :], W=[f'fxt{b}_{t}'])
                    tile_to_xT(cx, xt[b][t][:], f'fxt{b}_{t}', xb[b][t], f'fxb{b}_{t}', ptr[b][t], f'fptr{b}_{t}',
                               xTa[b] if t == 0 else xTb[b], f'fxT{"a" if t == 0 else "b"}{b}', cast_eng='act' if t == 0 else 'pool',
                               copy_eng='act' if t == 0 else 'dve')
                A_, B_ = xTa[b], xTb[b]
                E_, O_ = xTe[b], xTo[b]
                rkeys = [f'fxTa{b}', f'fxTb{b}']
                k.op('dve', lambda e: e.tensor_tensor(out=E_[:, :, 1:128], in0=A_[:, :, 1:128], in1=B_[:, :, 127:0:-1], op=ALU.add), R=rkeys, W=[f'fxTe{b}'])
                k.op('dve', lambda e: e.tensor_tensor(out=O_[:, :, 1:128], in0=A_[:, :, 1:128], in1=B_[:, :, 127:0:-1], op=ALU.subtract), R=rkeys, W=[f'fxTo{b}'])
                if i == 0:
                    k.op('dve', lambda e: e.tensor_copy(out=E_[:, :, 0:1], in_=A_[:, :, 0:1]), R=rkeys, W=[f'fxTe{b}'])
                    k.op('dve', lambda e: e.memset(O_[:, :, 0:1], 0.0), W=[f'fxTo{b}'])
                else:
                    Bp = xTb[1 - b]
                    k.op('dve', lambda e: e.tensor_tensor(out=E_[:, :, 0:1], in0=A_[:, :, 0:1], in1=Bp[:, :, 0:1], op=ALU.add), R=rkeys + [f'fxTb{1 - b}'], W=[f'fxTe{b}'])
                    k.op('dve', lambda e: e.tensor_tensor(out=O_[:, :, 0:1], in0=A_[:, :, 0:1], in1=Bp[:, :, 0:1], op=ALU.subtract), R=rkeys + [f'fxTb{1 - b}'], W=[f'fxTo{b}'])
            n = 0
            stageA(0)
            for i in range(NH2):
                b = i % 2
                if i + 1 < NH2:
                    stageA(i + 1)
                for t, (UV, XT) in enumerate([(U_all, xTe[b]), (V_all, xTo[b])]):
                    for hh in range(2):
                        P = pu[n % 4]
                        for kc in range(8):
                            k.op('pe', lambda e: e.matmul(P[:], lhsT=XT[:, kc, :], rhs=wcs[t][:, kc, hh * 512:(hh + 1) * 512],
                                                          start=(kc == 0), stop=(kc == 7)), R=[f'fxT{"e" if t == 0 else "o"}{b}', f'fW{t}'], W=[f'fpu{n % 4}'])
                        if n % 2 == 0:
                            k.op('act', lambda e: e.copy(out=UV[:, i, hh * 512:(hh + 1) * 512], in_=P[:]), R=[f'fpu{n % 4}'], W=[('UV', t, i)])
                        else:
                            k.op('dve', lambda e: e.tensor_copy(out=UV[:, i, hh * 512:(hh + 1) * 512], in_=P[:]), R=[f'fpu{n % 4}'], W=[('UV', t, i)])
                        n += 1
            bl = (NH2 - 1) % 2
            for hh in range(2):
                P = pu[n % 4]
                for kc in range(8):
                    k.op('pe', lambda e: e.matmul(P[0:1, :], lhsT=xTb[bl][:, kc, 0:1], rhs=wcs[0][:, kc, hh * 512:(hh + 1) * 512],
                                                  start=(kc == 0), stop=(kc == 7)), R=[f'fxTb{bl}', 'fW0'], W=[f'fpu{n % 4}'])
                k.op('act', lambda e: e.copy(out=umid[:, hh * 512:(hh + 1) * 512], in_=P[0:1, :]), R=[f'fpu{n % 4}'], W=['umid'])
                n += 1
        k.barrier()
        with ExitStack() as es2:
            sb2 = lambda n, s, d: es2.enter_context(cx.sbt(n, s, d, side='right'))
            cs = [sb2(f"fcs{j}", [128, 2, NH2, 128], BF16) for j in range(2)]
            cm = sb2("fcm", [1, S], BF16)
            k.dma('sp', cm[:], cx.d_cmid, W=['fcm'])
            xt = [sb2(f"gxt{j}", [128, D], F32) for j in range(2)]
            hh_ = [sb2(f"gh{j}", [128, D], F32) for j in range(2)]
            xo = [sb2(f"gxo{j}", [128, D], F32) for j in range(2)]
            pm = [es2.enter_context(cx.pst(f"fpm{j}", [128, 512], F32)) for j in range(4)]
            wk = ln_work(cx, es2, "ln1f")
            gain, bias, gkeys = ln_params(cx, es2, 'ln1', cx.lnL if hasattr(cx, 'lnL') else L)
            n = 0
            pend = [None]
            for kt in range(NT):
                b = kt % 2
                k.dma('sp', cs[b][:], cx.d_dfts[kt], W=[f'fcs{b}'])
                k.dma('sp', xt[b][:], X[kt * 128:(kt + 1) * 128, :], W=[f'gxt{b}'])
                for hh in range(2):
                    P = pm[n % 4]
                    for t, UV in enumerate([U_all, V_all]):
                        for jc in range(NH2):
                            k.op('pe', lambda e: e.matmul(P[:], lhsT=cs[b][:, t, jc, :], rhs=UV[:, jc, hh * 512:(hh + 1) * 512],
                                                          start=(t == 0 and jc == 0), stop=False),
                                 R=[f'fcs{b}', ('UV', t, jc)], W=[f'fpm{n % 4}'])
                    k.op('pe', lambda e: e.matmul(P[:], lhsT=cm[0:1, kt * 128:(kt + 1) * 128], rhs=umid[0:1, hh * 512:(hh + 1) * 512], start=False, stop=True),
                         R=['fcm', 'umid'], W=[f'fpm{n % 4}'])
                    k.op('dve', lambda e: e.scalar_tensor_tensor(out=hh_[b][:, hh * 512:(hh + 1) * 512], in0=xt[b][:, hh * 512:(hh + 1) * 512],
                                                                 scalar=ALPHA, in1=P[:], op0=ALU.mult, op1=ALU.add),
                         R=[f'gxt{b}', f'fpm{n % 4}'], W=[f'gh{b}'])
                    n += 1
                tail = layernorm_tile(cx, hh_[b], f'gh{b}', xo[b][:], f'gxo{b}', gain, bias, gkeys, wk, sfx=str(b), mid=pend[0], defer=True)
                pend[0] = (lambda tail=tail, kt=kt, b=b: (tail(), k.dma('pool', X1[kt * 128:(kt + 1) * 128, :], xo[b][:], R=[f'gxo{b}'], W=[('X1', kt)])))
            pend[0]()
    k.barrier()


QB_PERM = [half * 4 + c for c in range(4) for half in range(2)]


def host_even_layout(w_in_even, w_out_even):
    wi = np.array(w_in_even, copy=True)
    wo = np.array(w_out_even, copy=True)
    for p, hq in enumerate(QB_PERM):
        wi[:, :, 2048 + p * 64:2048 + (p + 1) * 64] = w_in_even[:, :, 2048 + hq * 64:2048 + (hq + 1) * 64]
        wo[:, 512 + p * 64:512 + (p + 1) * 64, :] = w_out_even[:, 512 + hq * 64:512 + (hq + 1) * 64, :]
    return wi, wo


def host_rope_consts():
    pos = np.arange(S, dtype=np.float64)
    out = np.zeros((S, 384), np.float32)

    def tab(half):
        inv = 10000.0 ** (-np.arange(half, dtype=np.float32) / half)
        ang = pos.astype(np.float32)[:, None] * inv[None, :]
        return np.cos(ang).astype(np.float32), np.sin(ang).astype(np.float32)
    ca, sa = tab(64)
    cb, sb_ = tab(32)
    out[:, 0:64] = ca; out[:, 64:128] = sa
    out[:, 128:192] = ca * np.float32(128 ** -0.5); out[:, 192:256] = sa * np.float32(128 ** -0.5)
    out[:, 256:288] = cb * np.float32(0.125); out[:, 288:320] = sb_ * np.float32(0.125)
    out[:, 320:352] = cb; out[:, 352:384] = sb_
    return out.reshape(NT, 128, 384)


def rope_tm(cx, eng, src, skey, H, hd, cos, sin, ckey, dst, dkey, t1, t2, tkey):
    k = cx.k
    sv = src.rearrange("p (h t d) -> p h t d", h=H, t=2)
    dv = dst.rearrange("p (h t d) -> p h t d", h=H, t=2)
    x1, x2 = sv[:, :, 0, :], sv[:, :, 1, :]
    cb = cos.unsqueeze(1).broadcast_to([128, H, hd])
    sb_ = sin.unsqueeze(1).broadcast_to([128, H, hd])
    a = t1.rearrange("p (h d) -> p h d", h=H)
    b = t2.rearrange("p (h d) -> p h d", h=H)
    tt = lambda o, i0, i1, op, R, W: k.op(eng, lambda e: e.tensor_tensor(out=o, in0=i0, in1=i1, op=op), R=R, W=W)
    tt(a, x1, cb, ALU.mult, [skey, ckey], [tkey + 'a'])
    tt(b, x2, sb_, ALU.mult, [skey, ckey], [tkey + 'b'])
    tt(dv[:, :, 0, :], a, b, ALU.subtract, [tkey + 'a', tkey + 'b'], [dkey])
    tt(a, x1, sb_, ALU.mult, [skey, ckey], [tkey + 'a'])
    tt(b, x2, cb, ALU.mult, [skey, ckey], [tkey + 'b'])
    tt(dv[:, :, 1, :], a, b, ALU.add, [tkey + 'a', tkey + 'b'], [dkey])


def even_phase(cx, L, X, X1):
    nc, k, din = cx.nc, cx.k, cx.din
    ev = L // 2
    SGA, YA = cx.SGA, cx.YA
    NH = 16

    def inproj_pass(w, wkey, ncols, blocks, epilogue, epilogue2, es1, sbr):
        xt = [sbr(f"ext{j}", [128, D], F32) for j in range(2)]
        xb = [sbr(f"exb{j}", [128, D], BF16) for j in range(2)]
        xT = [sbr(f"exT{j}", [128, 8, 128], BF16) for j in range(2)]
        rpt = [sbr(f"erp{j}", [128, 384], F32) for j in range(2)]
        ptr = [es1.enter_context(cx.pst(f"eptr{j}", [128, 8, 128], BF16)) for j in range(2)]
        pp = [es1.enter_context(cx.pst(f"epp{j}", [128, 512], F32)) for j in range(len(blocks))]

        def stageA(i):
            b = i % 2
            k.dma('sp', xt[b][:], X[i * 128:(i + 1) * 128, :], W=[f'ext{b}'])
            k.dma('sp', rpt[b][:], cx.d_rope[i], W=[f'erp{b}'])
            tile_to_xT(cx, xt[b][:], f'ext{b}', xb[b], f'exb{b}', ptr[b], f'eptr{b}', xT[b], f'exT{b}', cast_eng='act', copy_eng='dve')
        stageA(0)
        for i in range(NT):
            b = i % 2
            if i + 1 < NT:
                stageA(i + 1)
            for blk, (c0, cn) in enumerate(blocks):
                P = pp[blk]
                for kc in range(8):
                    k.op('pe', lambda e: e.matmul(P[:, 0:cn], lhsT=xT[b][:, kc, :], rhs=w[:, kc, c0:c0 + cn],
                                                  start=(kc == 0), stop=(kc == 7)), R=[f'exT{b}', wkey], W=[f'epp{blk}'])
                epilogue(i, b, blk, P, rpt[b], f'erp{b}')
            if i > 0:
                epilogue2(i - 1)
        epilogue2(NT - 1)

    with ExitStack() as esA:
        sbl = lambda n, s, d: esA.enter_context(cx.sbt(n, s, d))
        qaT = sbl("qaT", [128, 4, S], BF16)
        kaT = sbl("kaT", [128, 4, S], BF16)
        va_all = sbl("va_all", [128, NT, 512], BF16)
        with ExitStack() as es1:
            sbr = lambda n, s, d: es1.enter_context(cx.sbt(n, s, d, side='right'))
            w = sbr("ewA", [128, 8, 2048], BF16)
            for cb in range(4):
                k.dma('pool', w[:, :, cb * 512:(cb + 1) * 512],
                      din['w_in_even'][ev][:, cb * 512:(cb + 1) * 512].rearrange("(kc p) n -> p kc n", p=128), W=['ewA'])
            hs = [sbr(f"ehs{j}", [128, 512], F32) for j in range(2)]
            qr = [[sbr(f"eqr{j}_{t}", [128, 512], BF16) for t in range(2)] for j in range(2)]
            tq = [sbr(f"etq{j}", [128, 256], F32) for j in range(4)]
            sga = [sbr(f"esga{j}", [128, 512], BF16) for j in range(2)]
            ptq = [es1.enter_context(cx.pst(f"eptq{j}", [128, 4, 128], BF16)) for j in range(2)]

            def epiA2(i):
                t = i % 2
                for blk in range(2):
                    for h in range(4):
                        k.op('pe', lambda e: e.transpose(out=ptq[blk][:, h, :], in_=qr[blk][t][:, h * 128:(h + 1) * 128], identity=cx.ident_b[:]),
                             R=[f'eqr{blk}_{t}', 'ident_b'], W=[f'eptq{blk}'])
                    if blk == 0:
                        k.op('act', lambda e: e.copy(out=qaT[:, :, i * 128:(i + 1) * 128], in_=ptq[blk][:]), R=[f'eptq{blk}'], W=['qaT'])
                    else:
                        k.op('dve', lambda e: e.tensor_copy(out=kaT[:, :, i * 128:(i + 1) * 128], in_=ptq[blk][:]), R=[f'eptq{blk}'], W=['kaT'])

            def epiA(i, b, blk, P, rp, rpkey):
                if blk < 2:
                    t = i % 2
                    k.op('act', lambda e: e.copy(out=hs[blk][:], in_=P[:]), R=[f'epp{blk}'], W=[f'ehs{blk}'])
                    eng = 'dve' if blk == 0 else 'pool'
                    co = 0 if blk == 0 else 128
                    rope_tm(cx, eng, hs[blk][:], f'ehs{blk}', 4, 64, rp[:, co:co + 64], rp[:, co + 64:co + 128],
                            rpkey, qr[blk][t][:], f'eqr{blk}_{t}', tq[2 * blk][:], tq[2 * blk + 1][:], f'etq{blk}')
                elif blk == 2:
                    k.op('dve', lambda e: e.tensor_copy(out=va_all[:, i, :], in_=P[:]), R=[f'epp{blk}'], W=['va_all'])
                else:
                    k.op('act', lambda e: e.activation(out=sga[b][:], in_=P[:], func=AF.Silu), R=[f'epp{blk}'], W=[f'esga{b}'])
                    k.dma('pool', SGA[i * 128:(i + 1) * 128, :], sga[b][:], R=[f'esga{b}'], W=['SGA'])
            inproj_pass(w, 'ewA', 2048, [(0, 512), (512, 512), (1024, 512), (1536, 512)], epiA, epiA2, es1, sbr)
        k.barrier()

        with ExitStack() as es1:
            sbr = lambda n, s, d: es1.enter_context(cx.sbt(n, s, d, side='right'))
            lgt = sbr("rlg", [128, 8], F32)
            gng = sbr("rgng", [128, 512], F32)
            k.dma('sp', lgt[:], din['ret_decay_logit'][ev].rearrange("a h -> (a h)").partition_broadcast(128), W=['rlg'])
            k.dma('sp', gng[:], din['ret_gn_gain'][ev].partition_broadcast(128), W=['rgng'])
            k.op('act', lambda e: e.activation(out=lgt[:], in_=lgt[:], func=AF.Exp, scale=-1.0), R=['rlg'], W=['rlg'])
            k.op('dve', lambda e: e.tensor_scalar(out=lgt[:], in0=lgt[:], scalar1=1.0, scalar2=None, op0=ALU.add), R=['rlg'], W=['rlg'])
            k.op('act', lambda e: e.activation(out=lgt[:], in_=lgt[:], func=AF.Ln), R=['rlg'], W=['rlg'])
            k.op('dve', lambda e: e.tensor_scalar(out=lgt[:], in0=lgt[:], scalar1=-1.0, scalar2=None, op0=ALU.mult), R=['rlg'], W=['rlg'])
            DT = sbr("rDT", [128, 128], F32)
            XI = [sbr("rXIF", [128, 128], BF16), sbr("rXIB", [128, 128], BF16)]
            zc = sbr("rzc", [128, 4], F32)
            arg = sbr("rarg", [128, 128], F32)
            qs = [sbr("rqf", [128, S], BF16), sbr("rqb", [128, S], BF16)]
            Vz = [sbr("rVzf", [128, NT, 128], BF16), sbr("rVzb", [128, NT, 128], BF16)]
            ktm = sbr("rktm", [128, NT, 128], BF16)
            Rb_all = sbr("rRb_all", [128, NT, 128], BF16)
            R32 = [sbr("rRf32", [128, 128], F32), sbr("rRb32", [128, 128], F32)]
            Rfb = [sbr(f"rRfb{j}", [128, 128], BF16) for j in range(2)]
            Pm = [sbr(f"rPm{j}", [128, 128], BF16) for j in range(2)]
            Yraw = [sbr(f"rYraw{j}", [128, NH, 128], F32) for j in range(2)]
            Ysq = sbr("rYsq", [128, NH, 128], F32)
            sgs = [sbr(f"rsgs{j}", [128, NH, 128], BF16) for j in range(2)]
            yout = [sbr(f"ryout{j}", [128, NH, 128], BF16) for j in range(2)]
            gv = sbr("rgv", [128, 8, NH], F32)
            pk = [es1.enter_context(cx.pst(f"rpk{j}", [128, 8, 128], BF16)) for j in range(2)]
            pkv = [es1.enter_context(cx.pst(f"rpkv{j}", [128, 128], F32)) for j in range(2)]
            pst = [es1.enter_context(cx.pst(f"rpst{j}", [128, 128], F32)) for j in range(2)]
            py = [es1.enter_context(cx.pst(f"rpy{j}", [128, 128], F32)) for j in range(2)]
            pcol = cslice(cx, 'pcol')
            nslab = 0
            for h in range(4):
                lgf, lgb = lgt[:, h:h + 1], lgt[:, 4 + h:5 + h]
                k.op('dve', lambda e: e.tensor_scalar(out=arg[:], in0=cslice(cx, 'rp'), scalar1=lgf, scalar2=None, op0=ALU.mult),
                     R=['cst', 'rlg'], W=['rarg'])
                k.op('dve', lambda e: e.scalar_tensor_tensor(out=arg[:], in0=cslice(cx, 'rn'), scalar=lgb, in1=arg[:], op0=ALU.mult, op1=ALU.add),
                     R=['cst', 'rlg', 'rarg'], W=['rarg'])
                k.op('act', lambda e: e.activation(out=DT[:], in_=arg[:], func=AF.Exp), R=['rarg'], W=['rDT'])
                k.op('act', lambda e: e.activation(out=XI[0][:], in_=cslice(cx, 'cp1'), func=AF.Exp, scale=lgf), R=['cst', 'rlg'], W=['rXI0'])
                k.op('act', lambda e: e.activation(out=XI[1][:], in_=cslice(cx, 'cmc'), func=AF.Exp, scale=lgb), R=['cst', 'rlg'], W=['rXI1'])
                k.op('act', lambda e: e.activation(out=zc[:, 0:1], in_=pcol[:, 0:1], func=AF.Exp, scale=lgf), R=['cst', 'rlg'], W=['rzc'])
                k.op('act', lambda e: e.activation(out=zc[:, 1:2], in_=pcol[:, 1:2], func=AF.Exp, scale=lgb), R=['cst', 'rlg'], W=['rzc'])
                k.op('act', lambda e: e.activation(out=zc[:, 2:3], in_=pcol[:, 2:3], func=AF.Exp, scale=lgf), R=['cst', 'rlg'], W=['rzc'])
                k.op('act', lambda e: e.activation(out=zc[:, 3:4], in_=pcol[:, 2:3], func=AF.Exp, scale=lgb), R=['cst', 'rlg'], W=['rzc'])
                for d_ in range(2):
                    k.op('dve', lambda e: e.tensor_tensor(out=qs[d_][:].rearrange("p (n c) -> p n c", c=128),
                                                          in0=qaT[:, h, :].rearrange("p (n c) -> p n c", c=128),
                                                          in1=XI[d_][:].unsqueeze(1).broadcast_to([128, NT, 128]), op=ALU.mult),
                         R=['qaT', f'rXI{d_}'], W=[f'rqs{d_}'])
                    k.op('act', lambda e: e.activation(out=Vz[d_][:], in_=va_all[:, :, h * 128:(h + 1) * 128], func=AF.Identity, scale=zc[:, d_:d_ + 1]),
                         R=['va_all', 'rzc'], W=[f'rVz{d_}'])
                for g8 in range(4):
                    P = pk[g8 % 2]
                    for j in range(8):
                        n = g8 * 8 + j
                        k.op('pe', lambda e: e.transpose(out=P[:, j, :], in_=kaT[:, h, n * 128:(n + 1) * 128], identity=cx.ident_b[:]),
                             R=['kaT', 'ident_b'], W=[f'rpk{g8 % 2}'])
                    k.op('dve', lambda e: e.tensor_copy(out=ktm[:, g8 * 8:(g8 + 1) * 8, :], in_=P[:]), R=[f'rpk{g8 % 2}'], W=['rktm'])
                k.op('dve', lambda e: e.memset(R32[1][:], 0.0), W=['rR32_1'])
                k.op('dve', lambda e: e.memset(R32[0][:], 0.0), W=['rR32_0'])
                nkv = 0
                for n in range(NT - 1, 0, -1):
                    P = pkv[nkv % 2]
                    k.op('pe', lambda e: e.matmul(P[:], lhsT=ktm[:, n, :], rhs=Vz[1][:, n, :], start=True, stop=True),
                         R=['rktm', 'rVz1'], W=[f'rpkv{nkv % 2}'])
                    k.op('dve', lambda e: e.scalar_tensor_tensor(out=R32[1][:], in0=R32[1][:], scalar=zc[:, 3:4], in1=P[:], op0=ALU.mult, op1=ALU.add),
                         R=['rR32_1', 'rzc', f'rpkv{nkv % 2}'], W=['rR32_1'])
                    k.op('act', lambda e: e.copy(out=Rb_all[:, n - 1, :], in_=R32[1][:]), R=['rR32_1'], W=['rRb_all'])
                    nkv += 1
                def scores(n):
                    b = n % 2
                    cs_ = slice(n * 128, (n + 1) * 128)
                    k.op('pe', lambda e: e.matmul(pst[b][:], lhsT=kaT[:, h, cs_], rhs=qaT[:, h, cs_], start=True, stop=True),
                         R=['kaT', 'qaT'], W=[f'rpst{b}'])
                    k.op('dve', lambda e: e.tensor_tensor(out=Pm[b][:], in0=pst[b][:], in1=DT[:], op=ALU.mult), R=[f'rpst{b}', 'rDT'], W=[f'rPm{b}'])
                scores(0)
                for n in range(NT):
                    b = n % 2
                    sl = nslab % 2
                    cs_ = slice(n * 128, (n + 1) * 128)
                    if n % NH == 0:
                        k.dma('sp', sgs[sl][:], SGA[n * 128:(n + NH) * 128, h * 128:(h + 1) * 128].rearrange("(j p) c -> p j c", p=128),
                              R=['SGA'], W=[f'rsgs{sl}'])
                    if n + 1 < NT:
                        scores(n + 1)
                    last = 'intra'
                    if n < NT - 1:
                        last = 'bwd'
                    elif n > 0:
                        last = 'fwd'
                    k.op('pe', lambda e: e.matmul(py[b][:], lhsT=Pm[b][:], rhs=va_all[:, n, h * 128:(h + 1) * 128], start=True, stop=(last == 'intra')),
                         R=[f'rPm{b}', 'va_all'], W=[f'rpy{b}'])
                    if n > 0:
                        k.op('pe', lambda e: e.matmul(py[b][:], lhsT=qs[0][:, cs_], rhs=Rfb[(n - 1) % 2][:], start=False, stop=(last == 'fwd')),
                             R=['rqs0', f'rRfb{(n - 1) % 2}'], W=[f'rpy{b}'])
                    if n < NT - 1:
                        k.op('pe', lambda e: e.matmul(py[b][:], lhsT=qs[1][:, cs_], rhs=Rb_all[:, n, :], start=False, stop=True),
                             R=['rqs1', 'rRb_all'], W=[f'rpy{b}'])
                    k.op('act', lambda e: e.copy(out=Yraw[sl][:, n % NH, :], in_=py[b][:]), R=[f'rpy{b}'], W=[f'rYraw{sl}'])
                    if n < NT - 1:
                        P = pkv[nkv % 2]
                        k.op('pe', lambda e: e.matmul(P[:], lhsT=ktm[:, n, :], rhs=Vz[0][:, n, :], start=True, stop=True),
                             R=['rktm', 'rVz0'], W=[f'rpkv{nkv % 2}'])
                        k.op('dve', lambda e: e.scalar_tensor_tensor(out=R32[0][:], in0=R32[0][:], scalar=zc[:, 2:3], in1=P[:], op0=ALU.mult, op1=ALU.add),
                             R=['rR32_0', 'rzc', f'rpkv{nkv % 2}'], W=['rR32_0'])
                        k.op('act', lambda e: e.copy(out=Rfb[n % 2][:], in_=R32[0][:]), R=['rR32_0'], W=[f'rRfb{n % 2}'])
                        nkv += 1
                    if n % NH == NH - 1:
                        Y = Yraw[sl]
                        G = lambda j: gv[:, j, :]
                        bc = lambda ap: ap.unsqueeze(2).broadcast_to([128, NH, 128])
                        yk = f'rYraw{sl}'
                        k.op('dve', lambda e: e.tensor_reduce(out=G(0), in_=Y[:], axis=AX.X, op=ALU.add), R=[yk], W=['rg0'])
                        k.op('act', lambda e: e.activation(out=Ysq[:], in_=Y[:], func=AF.Square), R=[yk], W=['rYsq'])
                        k.op('dve', lambda e: e.tensor_reduce(out=G(1), in_=Ysq[:], axis=AX.X, op=ALU.add), R=['rYsq'], W=['rg1'])
                        k.op('dve', lambda e: e.tensor_scalar(out=G(2), in0=G(0), scalar1=1.0 / 128, scalar2=None, op0=ALU.mult), R=['rg0'], W=['rg2'])
                        k.op('dve', lambda e: e.tensor_tensor(out=G(3), in0=G(2), in1=G(2), op=ALU.mult), R=['rg2'], W=['rg3'])
                        k.op('dve', lambda e: e.scalar_tensor_tensor(out=G(4), in0=G(1), scalar=1.0 / 128, in1=G(3), op0=ALU.mult, op1=ALU.subtract),
                             R=['rg1', 'rg3'], W=['rg4'])
                        k.op('act', lambda e: e.activation(out=G(5), in_=G(4), func=AF.Sqrt, bias=cx.eps_gn[:], scale=1.0), R=['rg4', 'eps_gn'], W=['rg5'])
                        k.op('dve', lambda e: e.reciprocal(out=G(6), in_=G(5)), R=['rg5'], W=['rg6'])
                        k.op('dve', lambda e: e.tensor_tensor(out=Y[:], in0=Y[:], in1=bc(G(2)), op=ALU.subtract), R=[yk, 'rg2'], W=[yk])
                        k.op('dve', lambda e: e.tensor_tensor(out=Y[:], in0=Y[:], in1=bc(G(6)), op=ALU.mult), R=[yk, 'rg6'], W=[yk])
                        k.op('dve', lambda e: e.tensor_tensor(out=Y[:], in0=Y[:], in1=gng[:, h * 128:(h + 1) * 128].unsqueeze(1).broadcast_to([128, NH, 128]),
                                                              op=ALU.mult), R=[yk, 'rgng'], W=[yk])
                        k.op('dve', lambda e: e.tensor_tensor(out=yout[sl][:], in0=Y[:], in1=sgs[sl][:], op=ALU.mult), R=[yk, f'rsgs{sl}'], W=[f'ryout{sl}'])
                        n0 = n - (NH - 1)
                        k.dma('sp', YA[n0 * 128:(n0 + NH) * 128, h * 128:(h + 1) * 128].rearrange("(j p) c -> p j c", p=128), yout[sl][:],
                              R=[f'ryout{sl}'], W=['YA'])
                        nslab += 1
        k.barrier()

    with ExitStack() as esB:
        sbl = lambda n, s, d: esB.enter_context(cx.sbt(n, s, d))
        yb_all = sbl("yb_all", [128, NT, 512], BF16)
        with ExitStack() as esB2:
            sbl2 = lambda n, s, d: esB2.enter_context(cx.sbt(n, s, d))
            qbT = sbl2("qbT", [128, 4, S], BF16)
            kbT = sbl2("kbT", [128, S], BF16)
            vb_all = sbl2("vb_all", [128, NT, 128], BF16)
            with ExitStack() as es1:
                sbr = lambda n, s, d: es1.enter_context(cx.sbt(n, s, d, side='right'))
                w = sbr("ewB", [128, 8, 768], BF16)
                for cb in range(2):
                    k.dma('pool', w[:, :, cb * 384:(cb + 1) * 384],
                          din['w_in_even'][ev][:, 2048 + cb * 384:2048 + (cb + 1) * 384].rearrange("(kc p) n -> p kc n", p=128), W=['ewB'])
                hs = [sbr(f"ehs{j}", [128, 512], F32) for j in range(2)]
                qr = [[sbr(f"eqr{j}_{t}", [128, 512], BF16) for t in range(2)] for j in range(2)]
                tq = [sbr(f"etq{j}", [128, 256], F32) for j in range(4)]
                ptq = [es1.enter_context(cx.pst(f"eptq{j}", [128, 4, 128], BF16)) for j in range(2)]

                def epiB2(i):
                    t = i % 2
                    for c in range(4):
                        k.op('pe', lambda e: e.transpose(out=ptq[0][:, c, :], in_=qr[0][t][:, c * 128:(c + 1) * 128], identity=cx.ident_b[:]),
                             R=[f'eqr0_{t}', 'ident_b'], W=['eptq0'])
                    k.op('act', lambda e: e.copy(out=qbT[:, :, i * 128:(i + 1) * 128], in_=ptq[0][:]), R=['eptq0'], W=['qbT'])
                    k.op('pe', lambda e: e.transpose(out=ptq[1][:, 0, :], in_=qr[1][t][:, 0:128], identity=cx.ident_b[:]),
                         R=[f'eqr1_{t}', 'ident_b'], W=['eptq1'])
                    k.op('dve', lambda e: e.tensor_copy(out=kbT[:, i * 128:(i + 1) * 128], in_=ptq[1][:, 0, :]), R=['eptq1'], W=['kbT'])

                def epiB(i, b, blk, P, rp, rpkey):
                    t = i % 2
                    cn = 512 if blk == 0 else 256
                    k.op('act', lambda e: e.copy(out=hs[blk][:, 0:cn], in_=P[:, 0:cn]), R=[f'epp{blk}'], W=[f'ehs{blk}'])
                    if blk == 0:
                        rope_tm(cx, 'dve', hs[0][:], 'ehs0', 8, 32, rp[:, 256:288], rp[:, 288:320], rpkey,
                                qr[0][t][:], f'eqr0_{t}', tq[0][:], tq[1][:], 'etq0')
                    else:
                        rope_tm(cx, 'pool', hs[1][:, 0:128], 'ehs1', 2, 32, rp[:, 320:352], rp[:, 352:384], rpkey,
                                qr[1][t][:, 0:128], f'eqr1_{t}', tq[2][:, 0:64], tq[3][:, 0:64], 'etq1')
                        k.op('dve', lambda e: e.tensor_copy(out=vb_all[:, i, :], in_=hs[1][:, 128:256]), R=['ehs1'], W=['vb_all'])
                inproj_pass(w, 'ewB', 768, [(0, 512), (512, 256)], epiB, epiB2, es1, sbr)
            k.barrier()

            with ExitStack() as es1:
                sbr = lambda n, s, d: es1.enter_context(cx.sbt(n, s, d, side='right'))
                sk = sbr("ask", [128, 8], F32)
                k.dma('sp', sk[:], din['sink_logit'][ev].partition_broadcast(128), W=['ask'])
                sm4 = [sbr(f"asm{j}", [128, 4, 384], F32) for j in range(2)]
                pb4 = [sbr(f"apb{j}", [128, 4, 384], BF16) for j in range(2)]
                pT4 = [sbr(f"apT{j}", [128, 12, 128], BF16) for j in range(2)]
                a8 = [sbr(f"aa8{j}", [128, 8, 4], F32) for j in range(2)]
                ps4 = es1.enter_context(cx.pst("aps4", [128, 4, 512], F32))
                ppT = es1.enter_context(cx.pst("appT", [128, 12, 128], BF16))
                po = es1.enter_context(cx.pst("apo", [128, 4, 64], F32))
                mk = cslice(cx, 'mk')
                steps = [(n, half) for n in range(NT) for half in range(2)]

                def geom(n):
                    j0 = max(n - 1, 0)
                    j1 = min(n + 1, NT - 1)
                    return j0, j1, j1 - j0 + 1, (j0 - (n - 1)) * 128

                def att_F1(it):
                    n, half = steps[it]
                    j0, j1, nk, mo = geom(n)
                    W_ = nk * 128
                    b = it % 2
                    pl = slice(64 * half, 64 * half + 64)
                    A = lambda j: a8[b][:, j, :]
                    skh = sk[:, half * 4:half * 4 + 4]
                    for c in range(4):
                        k.op('pe', lambda e: e.matmul(ps4[:, c, 0:W_], lhsT=qbT[pl, c, n * 128:(n + 1) * 128], rhs=kbT[pl, j0 * 128:(j1 + 1) * 128],
                                                      start=True, stop=True), R=['qbT', 'kbT'], W=['aps4'])
                    k.op('dve', lambda e: e.tensor_tensor(out=sm4[b][:, :, 0:W_], in0=ps4[:, :, 0:W_],
                                                          in1=mk[:, mo:mo + W_].unsqueeze(1).broadcast_to([128, 4, W_]), op=ALU.add),
                         R=['aps4', 'cst'], W=[f'asm{b}'])
                    k.op('dve', lambda e: e.tensor_reduce(out=A(0), in_=sm4[b][:, :, 0:W_], axis=AX.X, op=ALU.max), R=[f'asm{b}'], W=[f'aa{b}0'])
                    k.op('dve', lambda e: e.tensor_tensor(out=A(0), in0=A(0), in1=skh, op=ALU.max), R=[f'aa{b}0', 'ask'], W=[f'aa{b}0'])
                    k.op('dve', lambda e: e.tensor_scalar(out=A(1), in0=A(0), scalar1=-1.0, scalar2=None, op0=ALU.mult), R=[f'aa{b}0'], W=[f'aa{b}1'])
                    k.op('dve', lambda e: e.tensor_tensor(out=A(3), in0=skh, in1=A(0), op=ALU.subtract), R=['ask', f'aa{b}0'], W=[f'aa{b}3'])

                def att_F2(it):
                    n, half = steps[it]
                    j0, j1, nk, mo = geom(n)
                    W_ = nk * 128
                    b = it % 2
                    A = lambda j: a8[b][:, j, :]
                    for c in range(4):
                        k.op('act', lambda e: e.activation(out=pb4[b][:, c, 0:W_], in_=sm4[b][:, c, 0:W_], func=AF.Exp, bias=a8[b][:, 1, c:c + 1], scale=1.0,
                                                           accum_out=a8[b][:, 2, c:c + 1]), R=[f'asm{b}', f'aa{b}1'], W=[f'apb{b}', f'aa{b}2'])
                    k.op('act', lambda e: e.activation(out=A(3), in_=A(3), func=AF.Exp), R=[f'aa{b}3'], W=[f'aa{b}3'])

                def att_B1(it):
                    n, half = steps[it]
                    j0, j1, nk, mo = geom(n)
                    b = it % 2
                    for c in range(4):
                        for kb_ in range(nk):
                            k.op('pe', lambda e: e.transpose(out=ppT[:, c * 3 + kb_, :], in_=pb4[b][:, c, kb_ * 128:(kb_ + 1) * 128], identity=cx.ident_b[:]),
                                 R=[f'apb{b}', 'ident_b'], W=['appT'])
                    if nk == 3:
                        k.op('act', lambda e: e.copy(out=pT4[b][:, 0:6, :], in_=ppT[:, 0:6, :]), R=['appT'], W=[f'apT{b}'])
                        k.op('act', lambda e: e.copy(out=pT4[b][:, 6:12, :], in_=ppT[:, 6:12, :]), R=['appT'], W=[f'apT{b}'])
                    else:
                        for c in range(4):
                            k.op('act', lambda e: e.copy(out=pT4[b][:, c * 3:c * 3 + nk, :], in_=ppT[:, c * 3:c * 3 + nk, :]), R=['appT'], W=[f'apT{b}'])

                def att_B2(it):
                    n, half = steps[it]
                    j0, j1, nk, mo = geom(n)
                    b = it % 2
                    A = lambda j: a8[b][:, j, :]
                    for c in range(4):
                        for kb_ in range(nk):
                            k.op('pe', lambda e: e.matmul(po[:, c, :], lhsT=pT4[b][:, c * 3 + kb_, :], rhs=vb_all[:, j0 + kb_, half * 64:(half + 1) * 64],
                                                          start=(kb_ == 0), stop=(kb_ == nk - 1)), R=[f'apT{b}', 'vb_all'], W=['apo'])
                    k.op('dve', lambda e: e.tensor_tensor(out=A(4), in0=A(2), in1=A(3), op=ALU.add), R=[f'aa{b}2', f'aa{b}3'], W=[f'aa{b}4'])
                    k.op('dve', lambda e: e.reciprocal(out=A(5), in_=A(4)), R=[f'aa{b}4'], W=[f'aa{b}5'])
                    k.op('dve', lambda e: e.tensor_tensor(out=yb_all[:, n, :].rearrange("p (c t d) -> p c t d", c=4, t=2)[:, :, half, :], in0=po[:],
                                                          in1=A(5).unsqueeze(2).broadcast_to([128, 4, 64]), op=ALU.mult),
                         R=['apo', f'aa{b}5'], W=['yb_all'])
                att_F1(0)
                att_F2(0)
                for it in range(len(steps)):
                    if it + 1 < len(steps):
                        att_F1(it + 1)
                    att_B1(it)
                    if it + 1 < len(steps):
                        att_F2(it + 1)
                    att_B2(it)
            k.barrier()

        with ExitStack() as es1:
            sbr = lambda n, s, d: es1.enter_context(cx.sbt(n, s, d, side='right'))
            wo = sbr("ewo", [128, 8, D], BF16)
            for cb in range(2):
                k.dma('pool', wo[:, :, cb * 512:(cb + 1) * 512],
                      din['w_out_even'][ev][:, cb * 512:(cb + 1) * 512].rearrange("(kc p) n -> p kc n", p=128), W=['ewo'])
            NB = 3
            xt = [sbr(f"oxt{j}", [128, D], F32) for j in range(NB)]
            yat = [sbr(f"oya{j}", [128, 512], BF16) for j in range(NB)]
            hh_ = [sbr(f"oh{j}", [128, D], F32) for j in range(NB)]
            xo = [sbr(f"oxo{j}", [128, D], F32) for j in range(NB)]
            yT = [sbr(f"oyT{j}", [128, 8, 128], BF16) for j in range(2)]
            ptr = [es1.enter_context(cx.pst(f"optr{j}", [128, 8, 128], BF16)) for j in range(2)]
            pm = [es1.enter_context(cx.pst(f"opm{j}", [128, 512], F32)) for j in range(4)]
            wk = ln_work(cx, es1, "ln1e")
            gain, bias, gkeys = ln_params(cx, es1, 'ln1', cx.lnL if hasattr(cx, 'lnL') else L)

            def stageA(i):
                b = i % NB
                b2 = i % 2
                k.dma('sp', xt[b][:], X[i * 128:(i + 1) * 128, :], W=[f'oxt{b}'])
                k.dma('sp', yat[b][:], YA[i * 128:(i + 1) * 128, :], R=['YA'], W=[f'oya{b}'])
                for kc in range(8):
                    src = yat[b][:, kc * 128:(kc + 1) * 128] if kc < 4 else yb_all[:, i, (kc - 4) * 128:(kc - 3) * 128]
                    k.op('pe', lambda e: e.transpose(out=ptr[b2][:, kc, :], in_=src, identity=cx.ident_b[:]),
                         R=[f'oya{b}', 'yb_all', 'ident_b'], W=[f'optr{b2}'])
                k.op('act', lambda e: e.copy(out=yT[b2][:], in_=ptr[b2][:]), R=[f'optr{b2}'], W=[f'oyT{b2}'])
            nn = 0
            pend = [None]
            stageA(0)
            for i in range(NT):
                b = i % NB
                b2 = i % 2
                if i + 1 < NT:
                    stageA(i + 1)
                for hh in range(2):
                    P = pm[nn % 4]
                    for kc in range(8):
                        k.op('pe', lambda e: e.matmul(P[:], lhsT=yT[b2][:, kc, :], rhs=wo[:, kc, hh * 512:(hh + 1) * 512], start=(kc == 0), stop=(kc == 7)),
                             R=[f'oyT{b2}', 'ewo'], W=[f'opm{nn % 4}'])
                    k.op('dve', lambda e: e.scalar_tensor_tensor(out=hh_[b][:, hh * 512:(hh + 1) * 512], in0=xt[b][:, hh * 512:(hh + 1) * 512],
                                                                 scalar=ALPHA, in1=P[:], op0=ALU.mult, op1=ALU.add),
                         R=[f'oxt{b}', f'opm{nn % 4}'], W=[f'oh{b}'])
                    nn += 1
                tail = layernorm_tile(cx, hh_[b], f'oh{b}', xo[b][:], f'oxo{b}', gain, bias, gkeys, wk, sfx=str(b), mid=pend[0], defer=True)
                pend[0] = (lambda tail=tail, i=i, b=b: (tail(), k.dma('pool', X1[i * 128:(i + 1) * 128, :], xo[b][:], R=[f'oxo{b}'], W=[('X1', i)])))
            pend[0]()
    k.barrier()


W_SHAPES = {
    'w_in_even': (2, 1024, IN_EVEN), 'ret_decay_logit': (2, 2, 4), 'ret_gn_gain': (2, 512), 'sink_logit': (2, 8),
    'w_out_even': (2, 1024, 1024), 'w_out_fourier': (2, 1024, 1024),
    'ln1_gain': (4, 1024), 'ln1_bias': (4, 1024), 'ln2_gain': (4, 1024), 'ln2_bias': (4, 1024),
    'router_coarse_w': (4, 1024, 4), 'router_coarse_b': (4, 4), 'router_fine_w': (4, 1024, 32), 'router_fine_b': (4, 32),
    'expert_w_gate': (4, 32, 1024, 512), 'expert_w_up': (4, 32, 1024, 512), 'expert_w_down': (4, 32, 512, 1024),
}


def build_program():
    nc = bass.Bass("TRN2", target_bir_lowering=False)
    cx = Ctx()
    cx.nc = nc
    cx.k = KB(nc)

    def din(name, shape, dt=F32):
        return nc.dram_tensor(name, list(shape), dt, kind="ExternalInput").ap()
    cx.din = {nm: din(nm, shp) for nm, shp in W_SHAPES.items()}
    x_in = din('x', (S, D))
    cx.d_cst = din('cst', (128, CST_N))
    cx.d_rope = din('rope', (NT, 128, 384))
    cx.d_dftc = din('dftc', (128, 2, 2, 256))
    cx.d_dfts = din('dfts', (NT, 128, 2, NT // 2, 128), BF16)
    cx.d_cmid = din('cmid', (1, S), BF16)
    cx.d_ridx = nc.dram_tensor('ridx', [128, NT // 2 + 1], I32, kind='ExternalInput').ap()
    out = nc.dram_tensor("out", [S, D], F32, kind="ExternalOutput").ap()
    XA = nc.dram_tensor("XA", [S, D], F32, kind="Internal").ap()
    XB = nc.dram_tensor("XB", [S, D], F32, kind="Internal").ap()
    cx.XE = nc.dram_tensor("XE", [NE * CAP, D], BF16, kind="Internal").ap()
    cx.YE = nc.dram_tensor("YE", [NE * CAP, D], F32, kind="Internal").ap()
    cx.SGA = nc.dram_tensor("SGA", [S, 512], BF16, kind="Internal").ap()
    cx.YA = nc.dram_tensor("YA", [S, 512], BF16, kind="Internal").ap()
    load_common(cx)
    cur = x_in
    for L in range(DEPTH):
        if L % 2 == 0:
            even_phase(cx, L, cur, XA)
        else:
            fnet_phase(cx, L, cur, XA)
        dst = out if L == DEPTH - 1 else XB
        moe_phase(cx, L, XA, dst)
        cur = dst
    cx.k.finish_all()
    return nc


def kernel(**inputs):
    inp = {k_: np.ascontiguousarray(np.asarray(v, dtype=np.float32)) for k_, v in inputs.items()}
    B = inp['x'].shape[0]
    wi, wo = host_even_layout(inp['w_in_even'], inp['w_out_even'])
    dftc, dfts, cmid = host_dft_consts()
    shared = {nm: inp[nm] for nm in W_SHAPES}
    shared['w_in_even'] = wi
    shared['w_out_even'] = wo
    shared['cst'] = host_consts()
    shared['rope'] = host_rope_consts()
    shared['dftc'] = dftc
    shared['dfts'] = dfts
    shared['cmid'] = cmid
    shared['ridx'] = host_ridx()
    nc = build_program()
    in_maps = []
    for c in range(B):
        m = dict(shared)
        m['x'] = inp['x'][c]
        in_maps.append(m)
    res = run_bass_kernel_spmd(nc, in_maps, core_ids=list(range(B)))
    return np.stack([np.asarray(res.results[c]['out'], dtype=np.float32) for c in range(B)], 0)
```

```python
from contextlib import ExitStack
import numpy as np
import ml_dtypes
import concourse.bass as bass
import concourse.mybir as mybir
from concourse.bass_utils import run_bass_kernel_spmd

F32 = mybir.dt.float32; BF16 = mybir.dt.bfloat16; I32 = mybir.dt.int32
AF = mybir.ActivationFunctionType; ALU = mybir.AluOpType; AX = mybir.AxisListType

S = 4096; D = 1024; NT = S // 128; DEPTH = 4
NE = 32; HID = 512; CAP = 384
ALPHA = (2.0 * DEPTH) ** 0.25
LN_EPS = 1e-5; GN_EPS = 1e-6
IN_EVEN = 2816


class KB:
    def __init__(self, nc, n_dma_sems=36, same_engine_sync=True):
        self.nc = nc
        self.eng = {'pe': nc.tensor, 'act': nc.scalar, 'dve': nc.vector, 'pool': nc.gpsimd, 'sp': nc.sync}
        self.sem = {e: nc.alloc_semaphore(f"s_{e}") for e in ['pe', 'act', 'dve', 'pool']}
        self.seq = {e: 0 for e in self.sem}
        self.waited = {e: {} for e in self.eng}
        self.dma_sems = [nc.alloc_semaphore(f"d{i}") for i in range(n_dma_sems)]
        self.dma_cnt = [0] * n_dma_sems
        self.dma_next = 0
        self.lastw = {}
        self.readers = {}
        self.same = same_engine_sync
        self.nwaits = 0
        self.nops = 0

    def _wait(self, e, tok):
        semkey, sem, val = tok
        w = self.waited[e]
        if w.get(semkey, 0) >= val:
            return
        self.eng[e].wait_ge(sem, val)
        w[semkey] = val
        self.nwaits += 1

    def _deps(self, e, R, W):
        best = {}

        def add(t):
            if t[0] == e and (e == 'pe' or not self.same):
                return
            if t[0] not in best or best[t[0]][2] < t[2]:
                best[t[0]] = t
        for r in R:
            for t in self.lastw.get(r, {}).values():
                add(t)
        for w_ in W:
            for t in self.lastw.get(w_, {}).values():
                add(t)
            for t in self.readers.get(w_, {}).values():
                add(t)
        for t in best.values():
            self._wait(e, t)

    def _commit(self, tok, R, W):
        for r in R:
            d = self.readers.setdefault(r, {})
            if tok[0] not in d or d[tok[0]][2] < tok[2]:
                d[tok[0]] = tok
        for w_ in W:
            self.lastw.setdefault(w_, {})[tok[0]] = tok
            self.readers[w_] = {}

    def op(self, e, fn, R=(), W=()):
        self._deps(e, R, W)
        ins = fn(self.eng[e])
        self.seq[e] += 1
        ins.then_inc(self.sem[e], 1)
        self._commit((e, self.sem[e], self.seq[e]), R, W)
        self.nops += 1
        return ins

    def dma(self, q, out, in_, R=(), W=(), indirect=None, **kw):
        i = self.dma_next
        self.dma_next = (i + 1) % len(self.dma_sems)
        sem = self.dma_sems[i]
        if self.dma_cnt[i] > 0:
            self._wait(q, (('d', i), sem, 16 * self.dma_cnt[i]))
        self._deps(q, R, W)
        if indirect is None:
            ins = self.eng[q].dma_start(out=out, in_=in_, **kw)
        else:
            ins = self.eng[q].indirect_dma_start(out=out, in_=in_, **indirect, **kw)
        self.dma_cnt[i] += 1
        ins.then_inc(sem, 16)
        self._commit((('d', i), sem, 16 * self.dma_cnt[i]), R, W)
        self.nops += 1
        return ins

    def finish_all(self):
        toks = [(e, self.sem[e], self.seq[e]) for e in self.sem if self.seq[e] > 0]
        toks += [(('d', i), s, 16 * c) for i, (s, c) in enumerate(zip(self.dma_sems, self.dma_cnt)) if c > 0]
        for t in toks:
            self._wait('sp', t)

    def barrier(self):
        toks = [(e, self.sem[e], self.seq[e]) for e in self.sem if self.seq[e] > 0]
        toks += [(('d', i), s, 16 * c) for i, (s, c) in enumerate(zip(self.dma_sems, self.dma_cnt)) if c > 0]
        for e in self.eng:
            for t in toks:
                if t[0] != e or e != 'pe':
                    self._wait(e, t)
        self.lastw = {}
        self.readers = {}


CST_COLS = {}


def _cst_layout():
    off = 0
    for name, n in [('ident', 128), ('ltri', 128), ('ones', 128), ('ecap', 1024), ('rp', 128), ('rn', 128),
                    ('cp1', 128), ('cmc', 128), ('pcol', 4), ('mk', 384)]:
        CST_COLS[name] = (off, n)
        off += n
    return off


CST_N = _cst_layout()


def host_consts():
    c = np.zeros((128, CST_N), np.float32)
    p = np.arange(128)

    def put(name, arr):
        o, n = CST_COLS[name]
        c[:, o:o + n] = arr
    put('ident', np.eye(128))
    put('ltri', (p[:, None] < p[None, :]).astype(np.float32))
    put('ones', np.ones((128, 128)))
    put('ecap', np.tile((np.arange(NE) * CAP)[None, :], (128, NT)))
    dif = (p[None, :] - p[:, None]).astype(np.float32)
    put('rp', np.maximum(dif, 0.0))
    put('rn', np.maximum(-dif, 0.0))
    jj = np.arange(384)
    put('mk', np.where((jj[None, :] >= p[:, None]) & (jj[None, :] <= p[:, None] + 256), 0.0, -30000.0))
    put('cp1', np.tile((p + 1.0)[None, :], (128, 1)))
    put('cmc', np.tile((128.0 - p)[None, :], (128, 1)))
    pc = np.stack([127.0 - p, p.astype(np.float64), np.full(128, 128.0), np.zeros(128)], 1)
    put('pcol', pc)
    return c


class Ctx:
    uid = 0

    def sbt(self, name, shape, dt, **kw):
        Ctx.uid += 1
        return self.nc.sbuf_tensor(f"{name}_u{Ctx.uid}", shape, dt, **kw)

    def pst(self, name, shape, dt, **kw):
        Ctx.uid += 1
        return self.nc.psum_tensor(f"{name}_u{Ctx.uid}", shape, dt, **kw)


def cslice(cx, name):
    o, n = CST_COLS[name]
    return cx.cst[:, o:o + n]


def load_common(cx):
    nc, k = cx.nc, cx.k
    cx.cst = nc.alloc_sbuf_tensor("cst_sb", [128, CST_N], F32)
    k.dma('sp', cx.cst[:], cx.d_cst, W=['cst'])
    cx.ident_b = nc.alloc_sbuf_tensor("ident_b", [128, 128], BF16)
    k.op('dve', lambda e: e.tensor_copy(out=cx.ident_b[:], in_=cslice(cx, 'ident')), R=['cst'], W=['ident_b'])
    cx.bound_reg = nc.gpsimd.to_reg(NE * CAP - 1)
    cx.bound_reg_s = nc.gpsimd.to_reg(S - 1)
    cx.eps_ln = nc.alloc_sbuf_tensor("eps_ln", [128, 1], F32)
    k.op('dve', lambda e: e.memset(cx.eps_ln[:], LN_EPS), W=['eps_ln'])
    cx.eps_gn = nc.alloc_sbuf_tensor("eps_gn", [128, 1], F32)
    k.op('dve', lambda e: e.memset(cx.eps_gn[:], GN_EPS), W=['eps_gn'])


def layernorm_tile(cx, h, hkey, out, okey, gain, bias, gkeys, wk, sfx='', mid=None, defer=False, hout=None, houtkey=None):
    k = cx.k
    j = (int(sfx) % 4) if sfx else 0
    st, mv, sc = wk['st'][:, j, :], wk['mv'][:, j, :], wk['sc'][:, j, :]
    K_ = lambda n: n + str(j)
    if hout is None:
        hout, houtkey = h, hkey
    k.op('dve', lambda e: e.bn_stats(out=st[:, 0:6], in_=h[:, 0:512]), R=[hkey], W=[K_('ln_st')])
    k.op('dve', lambda e: e.bn_stats(out=st[:, 6:12], in_=h[:, 512:1024]), R=[hkey], W=[K_('ln_st')])
    k.op('dve', lambda e: e.bn_aggr(out=mv, in_=st), R=[K_('ln_st')], W=[K_('ln_mv')])
    k.op('act', lambda e: e.activation(out=sc[:, 0:1], in_=mv[:, 1:2], func=AF.Sqrt, bias=cx.eps_ln[:], scale=1.0),
         R=[K_('ln_mv'), 'eps_ln'], W=[K_('ln_sd')])
    if mid is not None:
        mid()
    k.op('dve', lambda e: e.reciprocal(out=sc[:, 1:2], in_=sc[:, 0:1]), R=[K_('ln_sd')], W=[K_('ln_rstd')])
    k.op('dve', lambda e: e.scalar_tensor_tensor(out=sc[:, 2:3], in0=mv[:, 0:1], scalar=-1.0, in1=sc[:, 1:2],
                                                 op0=ALU.mult, op1=ALU.mult), R=[K_('ln_mv'), K_('ln_rstd')], W=[K_('ln_nmr')])
    k.op('act', lambda e: e.activation(out=hout[:], in_=h[:], func=AF.Identity, bias=sc[:, 2:3], scale=sc[:, 1:2]),
         R=[hkey, K_('ln_rstd'), K_('ln_nmr')], W=[houtkey])

    def tail():
        k.op('dve', lambda e: e.tensor_tensor(out=hout[:], in0=hout[:], in1=gain, op=ALU.mult), R=[houtkey] + gkeys, W=[houtkey])
        k.op('pool', lambda e: e.tensor_tensor(out=out, in0=hout[:], in1=bias, op=ALU.add), R=[houtkey] + gkeys, W=[okey])
    if defer:
        return tail
    tail()
    return None


def layernorm_gen(cx, h, hkey, out, okey, gain, bias, gkeys, wk, slot):
    k = cx.k
    j = slot % 4
    st, mv, sc = wk['st'][:, j, :], wk['mv'][:, j, :], wk['sc'][:, j, :]
    K_ = lambda n: n + str(j)
    k.op('dve', lambda e: e.bn_stats(out=st[:, 0:6], in_=h[:, 0:512]), R=[hkey], W=[K_('ln_st')])
    yield
    k.op('dve', lambda e: e.bn_stats(out=st[:, 6:12], in_=h[:, 512:1024]), R=[hkey], W=[K_('ln_st')])
    yield
    k.op('dve', lambda e: e.bn_aggr(out=mv, in_=st), R=[K_('ln_st')], W=[K_('ln_mv')])
    yield
    k.op('act', lambda e: e.activation(out=sc[:, 0:1], in_=mv[:, 1:2], func=AF.Sqrt, bias=cx.eps_ln[:], scale=1.0),
         R=[K_('ln_mv'), 'eps_ln'], W=[K_('ln_sd')])
    yield
    k.op('dve', lambda e: e.reciprocal(out=sc[:, 1:2], in_=sc[:, 0:1]), R=[K_('ln_sd')], W=[K_('ln_rstd')])
    yield
    k.op('dve', lambda e: e.scalar_tensor_tensor(out=sc[:, 2:3], in0=mv[:, 0:1], scalar=-1.0, in1=sc[:, 1:2],
                                                 op0=ALU.mult, op1=ALU.mult), R=[K_('ln_mv'), K_('ln_rstd')], W=[K_('ln_nmr')])
    yield
    k.op('act', lambda e: e.activation(out=h[:], in_=h[:], func=AF.Identity, bias=sc[:, 2:3], scale=sc[:, 1:2]),
         R=[hkey, K_('ln_rstd'), K_('ln_nmr')], W=[hkey])
    yield
    k.op('dve', lambda e: e.tensor_tensor(out=h[:], in0=h[:], in1=gain, op=ALU.mult), R=[hkey] + gkeys, W=[hkey])
    yield
    k.op('pool', lambda e: e.tensor_tensor(out=out, in0=h[:], in1=bias, op=ALU.add), R=[hkey] + gkeys, W=[okey])
    yield


def interleave(gens):
    gens = list(gens)
    while gens:
        for g in list(gens):
            try:
                next(g)
            except StopIteration:
                gens.remove(g)


def ln_params(cx, es, which, L):
    nc, k = cx.nc, cx.k
    g = es.enter_context(cx.sbt(f"{which}_g_sb", [128, D], F32))
    b = es.enter_context(cx.sbt(f"{which}_b_sb", [128, D], F32))
    k.dma('sp', g[:], cx.din[which + '_gain'][L].partition_broadcast(128), W=[which + '_g'])
    k.dma('sp', b[:], cx.din[which + '_bias'][L].partition_broadcast(128), W=[which + '_b'])
    return g[:], b[:], [which + '_g', which + '_b']


def ln_work(cx, es, pfx):
    nc = cx.nc
    return {'st': es.enter_context(cx.sbt(pfx + "_st", [128, 4, 12], F32)),
            'mv': es.enter_context(cx.sbt(pfx + "_mv", [128, 4, 2], F32)),
            'sc': es.enter_context(cx.sbt(pfx + "_sc", [128, 4, 4], F32))}


def moe_phase(cx, L, X1, X2):
    nc, k = cx.nc, cx.k
    din = cx.din
    XE, YE = cx.XE, cx.YE
    NJ = CAP // 128
    NW = 4
    with ExitStack() as es:
        sb = lambda n, s, d: es.enter_context(cx.sbt(n, s, d))
        g_all = [sb("g1_all", [128, NT], F32), sb("g2_all", [128, NT], F32)]
        dsti = [sb("dsti0", [128, NT], I32), sb("dsti1", [128, NT], I32)]
        wg = [sb(f"wg{j}", [128, 8, HID], BF16) for j in range(NW - 1)]
        wu = [sb(f"wu{j}", [128, 8, HID], BF16) for j in range(NW - 1)]
        wd = [sb(f"wd{j}", [128, 4, D], BF16) for j in range(NW - 1)]

        def LW(ex):
            wb = ex % NW
            k.dma('pool', wg[wb][:], din['expert_w_gate'][L, ex].rearrange("(kc p) n -> p kc n", p=128), W=[f'wg{wb}'])
            k.dma('pool', wu[wb][:], din['expert_w_up'][L, ex].rearrange("(kc p) n -> p kc n", p=128), W=[f'wu{wb}'])
            k.dma('pool', wd[wb][:], din['expert_w_down'][L, ex].rearrange("(kc p) n -> p kc n", p=128), W=[f'wd{wb}'])
        LW(0)
        LW(1)
        LW(2)
        ident = cslice(cx, 'ident')

        with ExitStack() as esx:
            sb2 = lambda n, s, d: esx.enter_context(cx.sbt(n, s, d))
            xb_all = sb2("xb_all", [128, NT, D], BF16)
            lgc = sb2("lgc", [128, NT, 4], F32)
            lgf = sb2("lgf", [128, NT * NE], F32)
            wr = sb2("wr", [128, 8, 36], F32)
            rb = sb2("rb", [128, 36], F32)
            k.dma('sp', wr[:, :, 0:4], din['router_coarse_w'][L].rearrange("(kc p) n -> p kc n", p=128), W=['wr'])
            k.dma('sp', wr[:, :, 4:36], din['router_fine_w'][L].rearrange("(kc p) n -> p kc n", p=128), W=['wr'])
            k.dma('sp', rb[:, 0:4], din['router_coarse_b'][L].partition_broadcast(128), W=['rb'])
            k.dma('sp', rb[:, 4:36], din['router_fine_b'][L].partition_broadcast(128), W=['rb'])
            NB = 4
            xt = [sb2(f"mxt{j}", [128, D], F32) for j in range(NB)]
            xT32 = [sb2(f"mxT{j}", [128, 8, 128], F32) for j in range(2)]
            pst = [esx.enter_context(cx.pst(f"pst{j}", [128, 4, 128], F32)) for j in range(4)]
            psl = [esx.enter_context(cx.pst(f"psl{j}", [128, 36], F32)) for j in range(2)]
            GT = 8
            NG = NT // GT
            N1 = GT * NE
            oh1 = sb2("oh1", [128, N1], F32)
            oh2 = sb2("oh2", [128, N1], F32)
            sel = sb2("sel", [128, N1], F32)
            fm = sb2("fm", [128, N1], F32)
            fm2 = sb2("fm2", [128, N1], F32)
            posw = sb2("posw", [128, N1], F32)
            cum = [sb2("cumA", [128, N1], F32), sb2("cumB", [128, N1], F32)]
            tot = sb2("tot", [128, N1], F32)
            base = sb2("base", [128, NE], F32)
            sc4 = sb2("sc4", [128, GT, 4], F32)
            pen = sb2("pen", [128, GT, 4], F32)
            v = sb2("rv", [128, 8, GT], F32)
            dstf = sb2("dstf", [128, 2 * GT], F32)
            psw = esx.enter_context(cx.pst("psw", [128, N1], F32))
            pso = esx.enter_context(cx.pst("pso", [128, N1], F32))
            k.op('dve', lambda e: e.memset(base[:], 0.0), W=['base'])
            dv = lambda fn, R, W: k.op('dve', fn, R=R, W=W)

            def router_tile(i):
                b = i % NB
                b2 = i % 2
                X, XT = xt[b], xT32[b2]
                k.dma('sp', X[:], X1[i * 128:(i + 1) * 128, :], W=[f'mxt{b}'])
                k.op('act', lambda e: e.copy(out=xb_all[:, i, :], in_=X[:]), R=[f'mxt{b}'], W=[('xb', i)])
                for hh in range(2):
                    pi = (i % 2) * 2 + hh
                    P = pst[pi]
                    for j in range(4):
                        kc = hh * 4 + j
                        k.op('pe', lambda e: e.transpose(out=P[:, j, :], in_=X[:, kc * 128:(kc + 1) * 128], identity=ident),
                             R=[f'mxt{b}', 'cst'], W=[f'pst{pi}'])
                    if hh == 0:
                        k.op('act', lambda e: e.copy(out=XT[:, 0:4, :], in_=P[:]), R=[f'pst{pi}'], W=[f'mxT{b2}a'])
                    else:
                        k.op('dve', lambda e: e.tensor_copy(out=XT[:, 4:8, :], in_=P[:]), R=[f'pst{pi}'], W=[f'mxT{b2}b'])
                PL = psl[b2]
                for kc in range(8):
                    k.op('pe', lambda e: e.matmul(PL[:, :], lhsT=XT[:, kc, :], rhs=wr[:, kc, :], start=(kc == 0), stop=(kc == 7)),
                         R=[f'mxT{b2}a', f'mxT{b2}b', 'wr'], W=[f'psl{b2}'])
                k.op('dve', lambda e: e.tensor_tensor(out=lgc[:, i, :], in0=PL[:, 0:4], in1=rb[:, 0:4], op=ALU.add), R=[f'psl{b2}', 'rb'], W=['lgc'])
                k.op('dve', lambda e: e.tensor_tensor(out=lgf[:, i * NE:(i + 1) * NE], in0=PL[:, 4:36], in1=rb[:, 4:36], op=ALU.add),
                     R=[f'psl{b2}', 'rb'], W=['lgf'])

            def route_group(g):
                t0 = g * GT
                ts = slice(t0, t0 + GT)
                LC = lgc[:, ts, :]
                LF = lgf[:, t0 * NE:(t0 + GT) * NE]
                V = lambda j: v[:, j, :]
                b3 = lambda ap, n: ap.unsqueeze(2).broadcast_to([128, GT, n])
                dv(lambda e: e.tensor_reduce(out=V(0), in_=LC, axis=AX.X, op=ALU.max), ['lgc'], ['v0'])
                dv(lambda e: e.tensor_tensor(out=sc4[:], in0=LC, in1=b3(V(0), 4), op=ALU.subtract), ['lgc', 'v0'], ['sc4'])
                dv(lambda e: e.tensor_scalar(out=pen[:], in0=sc4[:], scalar1=0.0, scalar2=None, op0=ALU.is_equal), ['sc4'], ['pen'])
                dv(lambda e: e.tensor_scalar(out=pen[:], in0=pen[:], scalar1=1.0, scalar2=1e30, op0=ALU.subtract, op1=ALU.mult), ['pen'], ['pen'])
                k.op('act', lambda e: e.activation(out=sc4[:], in_=sc4[:], func=AF.Exp), R=['sc4'], W=['sc4'])
                dv(lambda e: e.tensor_reduce(out=V(1), in_=sc4[:], axis=AX.X, op=ALU.add), ['sc4'], ['v1'])
                dv(lambda e: e.reciprocal(out=V(2), in_=V(1)), ['v1'], ['v2'])
                dv(lambda e: e.tensor_tensor(out=fm[:].rearrange("p (a j) -> p a j", j=8), in0=LF.rearrange("p (a j) -> p a j", j=8),
                                             in1=pen[:].rearrange("p i g -> p (i g)").unsqueeze(2).broadcast_to([128, GT * 4, 8]), op=ALU.add),
                   ['lgf', 'pen'], ['fm'])
                fm3 = fm[:].rearrange("p (i e) -> p i e", e=NE)
                dv(lambda e: e.tensor_reduce(out=V(3), in_=fm3, axis=AX.X, op=ALU.max), ['fm'], ['v3'])
                dv(lambda e: e.tensor_tensor(out=oh1[:].rearrange("p (i e) -> p i e", e=NE), in0=fm3, in1=b3(V(3), NE), op=ALU.is_equal), ['fm', 'v3'], ['oh1'])
                dv(lambda e: e.scalar_tensor_tensor(out=fm2[:], in0=oh1[:], scalar=-1e30, in1=fm[:], op0=ALU.mult, op1=ALU.add), ['oh1', 'fm'], ['fm2'])
                fm23 = fm2[:].rearrange("p (i e) -> p i e", e=NE)
                dv(lambda e: e.tensor_reduce(out=V(4), in_=fm23, axis=AX.X, op=ALU.max), ['fm2'], ['v4'])
                dv(lambda e: e.tensor_tensor(out=oh2[:].rearrange("p (i e) -> p i e", e=NE), in0=fm23, in1=b3(V(4), NE), op=ALU.is_equal), ['fm2', 'v4'], ['oh2'])
                dv(lambda e: e.tensor_tensor(out=sel[:], in0=oh1[:], in1=oh2[:], op=ALU.add), ['oh1', 'oh2'], ['sel'])
                dv(lambda e: e.tensor_tensor(out=V(5), in0=V(4), in1=V(3), op=ALU.subtract), ['v3', 'v4'], ['v5'])
                k.op('act', lambda e: e.activation(out=V(6), in_=V(5), func=AF.Exp), R=['v5'], W=['v6'])
                dv(lambda e: e.tensor_scalar(out=V(7), in0=V(6), scalar1=1.0, scalar2=None, op0=ALU.add), ['v6'], ['v7'])
                dv(lambda e: e.reciprocal(out=V(7), in_=V(7)), ['v7'], ['v7'])
                dv(lambda e: e.tensor_tensor(out=g_all[0][:, ts], in0=V(2), in1=V(7), op=ALU.mult), ['v2', 'v7'], ['g1_all'])
                dv(lambda e: e.tensor_tensor(out=g_all[1][:, ts], in0=g_all[0][:, ts], in1=V(6), op=ALU.mult), ['g1_all', 'v6'], ['g2_all'])
                k.op('pe', lambda e: e.matmul(psw[:], lhsT=cslice(cx, 'ltri'), rhs=sel[:], start=True, stop=True), R=['sel', 'cst'], W=['psw'])
                k.op('pe', lambda e: e.matmul(pso[:], lhsT=cslice(cx, 'ones'), rhs=sel[:], start=True, stop=True), R=['sel', 'cst'], W=['pso'])
                k.op('act', lambda e: e.copy(out=cum[0][:], in_=pso[:]), R=['pso'], W=['cum0'])
                k.op('act', lambda e: e.copy(out=tot[:], in_=pso[:]), R=['pso'], W=['tot'])
                cur = 0
                sh = 1
                while sh < GT:
                    a_, b_ = cum[cur], cum[1 - cur]
                    dv(lambda e: e.tensor_copy(out=b_[:, 0:sh * NE], in_=a_[:, 0:sh * NE]), [f'cum{cur}'], [f'cum{1 - cur}'])
                    dv(lambda e: e.tensor_tensor(out=b_[:, sh * NE:], in0=a_[:, sh * NE:], in1=a_[:, 0:N1 - sh * NE], op=ALU.add),
                       [f'cum{cur}'], [f'cum{1 - cur}'])
                    cur = 1 - cur
                    sh *= 2
                inc = cum[cur]
                dv(lambda e: e.tensor_tensor(out=posw[:], in0=psw[:], in1=inc[:], op=ALU.add), ['psw', f'cum{cur}'], ['posw'])
                dv(lambda e: e.tensor_tensor(out=posw[:], in0=posw[:], in1=tot[:], op=ALU.subtract), ['posw', 'tot'], ['posw'])
                dv(lambda e: e.tensor_tensor(out=posw[:].rearrange("p (i e) -> p i e", e=NE), in0=posw[:].rearrange("p (i e) -> p i e", e=NE),
                                             in1=base[:].unsqueeze(1).broadcast_to([128, GT, NE]), op=ALU.add), ['posw', 'base'], ['posw'])
                dv(lambda e: e.tensor_tensor(out=base[:], in0=base[:], in1=inc[:, (GT - 1) * NE:GT * NE], op=ALU.add), ['base', f'cum{cur}'], ['base'])
                dv(lambda e: e.tensor_scalar(out=fm[:], in0=posw[:], scalar1=float(CAP), scalar2=1e6, op0=ALU.is_ge, op1=ALU.mult), ['posw', 'fm'], ['fm'])
                dv(lambda e: e.tensor_tensor(out=posw[:], in0=posw[:], in1=fm[:], op=ALU.add), ['posw', 'fm'], ['posw'])
                dv(lambda e: e.tensor_tensor(out=posw[:], in0=posw[:], in1=cslice(cx, 'ecap')[:, 0:N1], op=ALU.add), ['posw', 'cst'], ['posw'])
                for s_, oh in enumerate([oh1, oh2]):
                    dv(lambda e: e.tensor_tensor(out=fm2[:], in0=oh[:], in1=posw[:], op=ALU.mult), ['oh1', 'oh2', 'posw', 'fm2'], ['fm2'])
                    dv(lambda e: e.tensor_reduce(out=dstf[:, s_ * GT:(s_ + 1) * GT], in_=fm2[:].rearrange("p (i e) -> p i e", e=NE),
                                                 axis=AX.X, op=ALU.add), ['fm2'], ['dstf'])
                    dv(lambda e: e.tensor_copy(out=dsti[s_][:, ts], in_=dstf[:, s_ * GT:(s_ + 1) * GT]), ['dstf'], [f'dsti{s_}'])
                for i in range(t0, t0 + GT):
                    for s_ in range(2):
                        k.dma('pool', XE, xb_all[:, i, :], R=[('xb', i), f'dsti{s_}'], W=['XE'],
                              indirect=dict(out_offset=bass.IndirectOffsetOnAxis(ap=dsti[s_][:, i:i + 1], axis=0), in_offset=None,
                                            bounds_check=cx.bound_reg, oob_is_err=False))

            for g in range(NG):
                for i in range(g * GT, (g + 1) * GT):
                    router_tile(i)
                route_group(g)
        k.barrier()

        with ExitStack() as es2:
            sb2 = lambda n, s, d: es2.enter_context(cx.sbt(n, s, d))
            wg.append(sb2(f"wg{NW - 1}", [128, 8, HID], BF16))
            wu.append(sb2(f"wu{NW - 1}", [128, 8, HID], BF16))
            wd.append(sb2(f"wd{NW - 1}", [128, 4, D], BF16))
            xea = [sb2(f"xea{j}", [128, NJ, D], BF16) for j in range(2)]
            xeT = [sb2(f"xeT{j}", [128, 8, CAP], BF16) for j in range(2)]
            sg = [sb2(f"sg{j}", [128, CAP], F32) for j in range(2)]
            hid = [sb2(f"hid{j}", [128, 4, CAP], BF16) for j in range(2)]
            yo = [sb2(f"yo{j}", [128, NJ, D], F32) for j in range(2)]
            ptr = [es2.enter_context(cx.pst(f"ptr{j}", [128, 8, 128], BF16)) for j in range(2)]
            psg = [es2.enter_context(cx.pst(f"psg{j}", [128, CAP], F32)) for j in range(2)]
            psu = [es2.enter_context(cx.pst(f"psu{j}", [128, CAP], F32)) for j in range(2)]
            psy = [es2.enter_context(cx.pst(f"psy{j}", [128, 512], F32)) for j in range(2)]

            def LX(ex):
                k.dma('sp', xea[ex % 2][:], XE[ex * CAP:(ex + 1) * CAP, :].rearrange("(j p) d -> p j d", p=128), R=['XE'], W=[f'xea{ex % 2}'])

            def TGU(ex):
                wb = ex % NW
                XT = xeT[ex % 2]
                H = hid[ex % 2]
                XA = xea[ex % 2]
                for jt in range(NJ):
                    b2 = jt % 2
                    for kc in range(8):
                        k.op('pe', lambda e: e.transpose(out=ptr[b2][:, kc, :], in_=XA[:, jt, kc * 128:(kc + 1) * 128], identity=cx.ident_b[:]),
                             R=[f'xea{ex % 2}', 'ident_b'], W=[f'ptr{b2}'])
                    if b2 == 0:
                        k.op('act', lambda e: e.copy(out=XT[:, :, jt * 128:(jt + 1) * 128], in_=ptr[b2][:]), R=[f'ptr{b2}'], W=[f'xeT{ex % 2}'])
                    else:
                        k.op('dve', lambda e: e.tensor_copy(out=XT[:, :, jt * 128:(jt + 1) * 128], in_=ptr[b2][:]), R=[f'ptr{b2}'], W=[f'xeT{ex % 2}'])
                for hc in range(4):
                    b = hc % 2
                    for kc in range(8):
                        k.op('pe', lambda e: e.matmul(psg[b][:], lhsT=wg[wb][:, kc, hc * 128:(hc + 1) * 128], rhs=XT[:, kc, :],
                                                      start=(kc == 0), stop=(kc == 7)), R=[f'wg{wb}', f'xeT{ex % 2}'], W=[f'psg{b}'])
                    for kc in range(8):
                        k.op('pe', lambda e: e.matmul(psu[b][:], lhsT=wu[wb][:, kc, hc * 128:(hc + 1) * 128], rhs=XT[:, kc, :],
                                                      start=(kc == 0), stop=(kc == 7)), R=[f'wu{wb}', f'xeT{ex % 2}'], W=[f'psu{b}'])
                    k.op('act', lambda e: e.activation(out=sg[b][:], in_=psg[b][:], func=AF.Silu), R=[f'psg{b}'], W=[f'sg{b}'])
                    k.op('dve', lambda e: e.tensor_tensor(out=H[:, hc, :], in0=psu[b][:], in1=sg[b][:], op=ALU.mult),
                         R=[f'psu{b}', f'sg{b}'], W=[f'hid{ex % 2}'])

            def DN(ex):
                wb = ex % NW
                H = hid[ex % 2]
                YO = yo[ex % 2]
                for jt in range(NJ):
                    for hh in range(2):
                        for hc in range(4):
                            k.op('pe', lambda e: e.matmul(psy[hh][:], lhsT=H[:, hc, jt * 128:(jt + 1) * 128],
                                                          rhs=wd[wb][:, hc, hh * 512:(hh + 1) * 512], start=(hc == 0), stop=(hc == 3)),
                                 R=[f'hid{ex % 2}', f'wd{wb}'], W=[f'psy{hh}'])
                        if hh == 0:
                            k.op('act', lambda e: e.copy(out=YO[:, jt, 0:512], in_=psy[0][:]), R=['psy0'], W=[f'yo{ex % 2}'])
                        else:
                            k.op('dve', lambda e: e.tensor_copy(out=YO[:, jt, 512:1024], in_=psy[1][:]), R=['psy1'], W=[f'yo{ex % 2}'])
                k.dma('sp', YE[ex * CAP:(ex + 1) * CAP, :].rearrange("(j p) d -> p j d", p=128), YO[:], R=[f'yo{ex % 2}'], W=['YE'])

            LX(0)
            LX(1)
            TGU(0)
            for ex in range(NE):
                if ex + 3 < NE:
                    LW(ex + 3)
                if ex + 2 < NE and ex >= 0:
                    pass
                if ex + 1 < NE:
                    TGU(ex + 1)
                if ex + 2 < NE:
                    LX(ex + 2)
                DN(ex)
        k.barrier()

        with ExitStack() as es2:
            sb2 = lambda n, s, d: es2.enter_context(cx.sbt(n, s, d))
            NB = 5
            r1 = [sb2(f"r1_{j}", [128, D], F32) for j in range(NB)]
            r2 = [sb2(f"r2_{j}", [128, D], F32) for j in range(NB)]
            xt = [sb2(f"cxt{j}", [128, D], F32) for j in range(NB)]
            xo = [sb2(f"cxo{j}", [128, D], F32) for j in range(NB)]
            wk = ln_work(cx, es2, "ln2")
            gain, bias, gkeys = ln_params(cx, es2, 'ln2', cx.lnL if hasattr(cx, 'lnL') else L)
            def issue_loads(i):
                b = i % NB
                k.dma('sp', xt[b][:], X1[i * 128:(i + 1) * 128, :], W=[f'cxt{b}'])
                for s_, r in enumerate([r1[b], r2[b]]):
                    k.dma('pool', r[:], YE, R=['YE', f'dsti{s_}'], W=[f'r{s_}_{b}'],
                          indirect=dict(out_offset=None, in_offset=bass.IndirectOffsetOnAxis(ap=dsti[s_][:, i:i + 1], axis=0),
                                        bounds_check=cx.bound_reg, oob_is_err=False))
            for i in range(NB - 2):
                issue_loads(i)
            pend = [None]
            for i in range(NT):
                b = i % NB
                if i + NB - 2 < NT:
                    issue_loads(i + NB - 2)
                k.op('act', lambda e: e.activation(out=r2[b][:], in_=r2[b][:], func=AF.Identity, scale=g_all[1][:, i:i + 1]),
                     R=[f'r1_{b}', 'g2_all'], W=[f'r1_{b}'])
                k.op('dve', lambda e: e.scalar_tensor_tensor(out=r1[b][:], in0=r1[b][:], scalar=g_all[0][:, i:i + 1], in1=r2[b][:],
                                                             op0=ALU.mult, op1=ALU.add), R=[f'r0_{b}', f'r1_{b}', 'g1_all'], W=[f'r0_{b}'])
                k.op('dve', lambda e: e.scalar_tensor_tensor(out=xt[b][:], in0=xt[b][:], scalar=ALPHA, in1=r1[b][:],
                                                             op0=ALU.mult, op1=ALU.add), R=[f'cxt{b}', f'r0_{b}'], W=[f'cxt{b}'])
                tail = layernorm_tile(cx, xt[b], f'cxt{b}', xo[b][:], f'cxo{b}', gain, bias, gkeys, wk, sfx=str(b), mid=pend[0], defer=True)
                pend[0] = (lambda tail=tail, i=i, b=b: (tail(), k.dma('sp', X2[i * 128:(i + 1) * 128, :], xo[b][:], R=[f'cxo{b}'], W=[('X2', i)])))
            pend[0]()
    k.barrier()


def tile_to_xT(cx, X, xkey, xb, xbkey, ptr, ptrkey, xT, xTkey, cast_eng='pool', copy_eng='act'):
    k = cx.k
    if cast_eng == 'pool':
        k.op('pool', lambda e: e.tensor_copy(out=xb[:], in_=X), R=[xkey], W=[xbkey])
    else:
        k.op(cast_eng, lambda e: e.tensor_copy(out=xb[:], in_=X) if cast_eng == 'dve' else e.copy(out=xb[:], in_=X), R=[xkey], W=[xbkey])
    for kc in range(8):
        k.op('pe', lambda e: e.transpose(out=ptr[:, kc, :], in_=xb[:, kc * 128:(kc + 1) * 128], identity=cx.ident_b[:]),
             R=[xbkey, 'ident_b'], W=[ptrkey])
    if copy_eng == 'act':
        k.op('act', lambda e: e.copy(out=xT[:], in_=ptr[:]), R=[ptrkey], W=[xTkey])
    else:
        k.op('dve', lambda e: e.tensor_copy(out=xT[:], in_=ptr[:]), R=[ptrkey], W=[xTkey])


def host_dft_consts():
    a = np.arange(256)
    ang = 2.0 * np.pi * np.outer(a, a) / 256.0
    cc = (np.cos(ang) / 16.0).astype(np.float32)
    sc = (np.sin(ang) / 16.0).astype(np.float32)
    dftc = np.stack([cc.reshape(2, 128, 256), sc.reshape(2, 128, 256)], 2).transpose(1, 0, 2, 3).copy()
    j = np.arange(S // 2)
    kk = np.arange(S)
    jk = (np.outer(j, kk) % S).astype(np.float64)
    ang = 2.0 * np.pi * jk / S
    cs = (np.cos(ang) / 64.0)
    ss = (-np.sin(ang) / 64.0)
    NJ2 = NT // 2
    m = np.stack([cs, ss], 0).reshape(2, NJ2, 128, NT, 128)
    dfts = np.ascontiguousarray(m.transpose(3, 2, 0, 1, 4)).astype(ml_dtypes.bfloat16)
    cmid = (np.cos(np.pi * kk) / 64.0).reshape(1, S).astype(ml_dtypes.bfloat16)
    return dftc, dfts, cmid


def host_ridx():
    p = np.arange(128)[:, None]
    kt = np.arange(NT // 2 + 1)[None, :]
    return (S - kt * 128 - p).astype(np.int32)


def fnet_phase(cx, L, X, X1):
    nc, k = cx.nc, cx.k
    din = cx.din
    o = L // 2
    NH2 = NT // 2
    with ExitStack() as es:
        with ExitStack() as es1:
            sb1 = lambda n, s, d: es1.enter_context(cx.sbt(n, s, d, side='right'))
            wcs = [sb1("fWc", [128, 8, D], BF16), sb1("fWs", [128, 8, D], BF16)]
            with ExitStack() as es0:
                w32 = es0.enter_context(cx.sbt("fw32", [128, 8, D], F32))
                dc = es0.enter_context(cx.sbt("fdc", [128, 2, 2, 256], F32))
                pw = [es0.enter_context(cx.pst(f"fpw{j}", [128, 512], F32)) for j in range(2)]
                k.dma('sp', w32[:], din['w_out_fourier'][o].rearrange("(kc p) n -> p kc n", p=128), W=['fw32'])
                k.dma('sp', dc[:], cx.d_dftc, W=['fdc'])
                n = 0
                for t in range(2):
                    for fc in range(8):
                        g, ac = fc // 2, fc % 2
                        for hh in range(2):
                            P = pw[n % 2]
                            for a2 in range(2):
                                k.op('pe', lambda e: e.matmul(P[:], lhsT=dc[:, a2, t, ac * 128:(ac + 1) * 128],
                                                              rhs=w32[:, g * 2 + a2, hh * 512:(hh + 1) * 512], start=(a2 == 0), stop=(a2 == 1)),
                                     R=['fdc', 'fw32'], W=[f'fpw{n % 2}'])
                            if n % 2 == 0:
                                k.op('act', lambda e: e.copy(out=wcs[t][:, fc, hh * 512:(hh + 1) * 512], in_=P[:]), R=[f'fpw{n % 2}'], W=[f'fW{t}'])
                            else:
                                k.op('dve', lambda e: e.tensor_copy(out=wcs[t][:, fc, hh * 512:(hh + 1) * 512], in_=P[:]), R=[f'fpw{n % 2}'], W=[f'fW{t}'])
                            n += 1
                k.barrier()
            U_all = es.enter_context(cx.sbt("U_all", [128, NH2, D], BF16))
            V_all = es.enter_context(cx.sbt("V_all", [128, NH2, D], BF16))
            umid = es.enter_context(cx.sbt("umid", [1, D], BF16))
            xt = [[sb1(f"fxt{j}_{t}", [128, D], F32) for t in range(2)] for j in range(2)]
            xb = [[sb1(f"fxb{j}_{t}", [128, D], BF16) for t in range(2)] for j in range(2)]
            xTa = [sb1(f"fxTa{j}", [128, 8, 128], BF16) for j in range(2)]
            xTb = [sb1(f"fxTb{j}", [128, 8, 128], BF16) for j in range(2)]
            xTe = [sb1(f"fxTe{j}", [128, 8, 128], BF16) for j in range(2)]
            xTo = [sb1(f"fxTo{j}", [128, 8, 128], BF16) for j in range(2)]
            ptr = [[es1.enter_context(cx.pst(f"fptr{j}_{t}", [128, 8, 128], BF16)) for t in range(2)] for j in range(2)]
            pu = [es1.enter_context(cx.pst(f"fpu{j}", [128, 512], F32)) for j in range(4)]

            def stageA(i):
                b = i % 2
                for t, ti in enumerate([i, NT - 1 - i]):
                    k.dma('sp', xt[b][t][:], X[ti * 128:(ti + 1) * 128, :], W=[f'fxt{b}_{t}'])
                    tile_to_xT(cx, xt[b][t][:], f'fxt{b}_{t}', xb[b][t], f'fxb{b}_{t}', ptr[b][t], f'fptr{b}_{t}',
                               xTa[b] if t == 0 else xTb[b], f'fxT{"a" if t == 0 else "b"}{b}', cast_eng='act' if t == 0 else 'pool',
                               copy_eng='act' if t == 0 else 'dve')
                A_, B_ = xTa[b], xTb[b]
                E_, O_ = xTe[b], xTo[b]
                rkeys = [f'fxTa{b}', f'fxTb{b}']
                k.op('dve', lambda e: e.tensor_tensor(out=E_[:, :, 1:128], in0=A_[:, :, 1:128], in1=B_[:, :, 127:0:-1], op=ALU.add), R=rkeys, W=[f'fxTe{b}'])
                k.op('dve', lambda e: e.tensor_tensor(out=O_[:, :, 1:128], in0=A_[:, :, 1:128], in1=B_[:, :, 127:0:-1], op=ALU.subtract), R=rkeys, W=[f'fxTo{b}'])
                if i == 0:
                    k.op('dve', lambda e: e.tensor_copy(out=E_[:, :, 0:1], in_=A_[:, :, 0:1]), R=rkeys, W=[f'fxTe{b}'])
                    k.op('dve', lambda e: e.memset(O_[:, :, 0:1], 0.0), W=[f'fxTo{b}'])
                else:
                    Bp = xTb[1 - b]
                    k.op('dve', lambda e: e.tensor_tensor(out=E_[:, :, 0:1], in0=A_[:, :, 0:1], in1=Bp[:, :, 0:1], op=ALU.add), R=rkeys + [f'fxTb{1 - b}'], W=[f'fxTe{b}'])
                    k.op('dve', lambda e: e.tensor_tensor(out=O_[:, :, 0:1], in0=A_[:, :, 0:1], in1=Bp[:, :, 0:1], op=ALU.subtract), R=rkeys + [f'fxTb{1 - b}'], W=[f'fxTo{b}'])
            n = 0
            stageA(0)
            for i in range(NH2):
                b = i % 2
                if i + 1 < NH2:
                    stageA(i + 1)
                for t, (UV, XT) in enumerate([(U_all, xTe[b]), (V_all, xTo[b])]):
                    for hh in range(2):
                        P = pu[n % 4]
                        for kc in range(8):
                            k.op('pe', lambda e: e.matmul(P[:], lhsT=XT[:, kc, :], rhs=wcs[t][:, kc, hh * 512:(hh + 1) * 512],
                                                          start=(kc == 0), stop=(kc == 7)), R=[f'fxT{"e" if t == 0 else "o"}{b}', f'fW{t}'], W=[f'fpu{n % 4}'])
                        if n % 2 == 0:
                            k.op('act', lambda e: e.copy(out=UV[:, i, hh * 512:(hh + 1) * 512], in_=P[:]), R=[f'fpu{n % 4}'], W=[('UV', t, i)])
                        else:
                            k.op('dve', lambda e: e.tensor_copy(out=UV[:, i, hh * 512:(hh + 1) * 512], in_=P[:]), R=[f'fpu{n % 4}'], W=[('UV', t, i)])
                        n += 1
            bl = (NH2 - 1) % 2
            for hh in range(2):
                P = pu[n % 4]
                for kc in range(8):
                    k.op('pe', lambda e: e.matmul(P[0:1, :], lhsT=xTb[bl][:, kc, 0:1], rhs=wcs[0][:, kc, hh * 512:(hh + 1) * 512],
                                                  start=(kc == 0), stop=(kc == 7)), R=[f'fxTb{bl}', 'fW0'], W=[f'fpu{n % 4}'])
                k.op('act', lambda e: e.copy(out=umid[:, hh * 512:(hh + 1) * 512], in_=P[0:1, :]), R=[f'fpu{n % 4}'], W=['umid'])
                n += 1
        k.barrier()
        with ExitStack() as es2:
            sb2 = lambda n, s, d: es2.enter_context(cx.sbt(n, s, d, side='right'))
            cs = [sb2(f"fcs{j}", [128, 2, NH2, 128], BF16) for j in range(2)]
            cm = sb2("fcm", [1, S], BF16)
            k.dma('sp', cm[:], cx.d_cmid, W=['fcm'])
            xt = [sb2(f"gxt{j}", [128, D], F32) for j in range(2)]
            hh_ = [sb2(f"gh{j}", [128, D], F32) for j in range(2)]
            xo = [sb2(f"gxo{j}", [128, D], F32) for j in range(2)]
            pm = [es2.enter_context(cx.pst(f"fpm{j}", [128, 512], F32)) for j in range(4)]
            wk = ln_work(cx, es2, "ln1f")
            gain, bias, gkeys = ln_params(cx, es2, 'ln1', cx.lnL if hasattr(cx, 'lnL') else L)
            n = 0
            pend = [None]
            for kt in range(NT):
                b = kt % 2
                k.dma('sp', cs[b][:], cx.d_dfts[kt], W=[f'fcs{b}'])
                k.dma('sp', xt[b][:], X[kt * 128:(kt + 1) * 128, :], W=[f'gxt{b}'])
                for hh in range(2):
                    P = pm[n % 4]
                    for t, UV in enumerate([U_all, V_all]):
                        for jc in range(NH2):
                            k.op('pe', lambda e: e.matmul(P[:], lhsT=cs[b][:, t, jc, :], rhs=UV[:, jc, hh * 512:(hh + 1) * 512],
                                                          start=(t == 0 and jc == 0), stop=False),
                                 R=[f'fcs{b}', ('UV', t, jc)], W=[f'fpm{n % 4}'])
                    k.op('pe', lambda e: e.matmul(P[:], lhsT=cm[0:1, kt * 128:(kt + 1) * 128], rhs=umid[0:1, hh * 512:(hh + 1) * 512], start=False, stop=True),
                         R=['fcm', 'umid'], W=[f'fpm{n % 4}'])
                    k.op('dve', lambda e: e.scalar_tensor_tensor(out=hh_[b][:, hh * 512:(hh + 1) * 512], in0=xt[b][:, hh * 512:(hh + 1) * 512],
                                                                 scalar=ALPHA, in1=P[:], op0=ALU.mult, op1=ALU.add),
                         R=[f'gxt{b}', f'fpm{n % 4}'], W=[f'gh{b}'])
                    n += 1
                tail = layernorm_tile(cx, hh_[b], f'gh{b}', xo[b][:], f'gxo{b}', gain, bias, gkeys, wk, sfx=str(b), mid=pend[0], defer=True)
                pend[0] = (lambda tail=tail, kt=kt, b=b: (tail(), k.dma('pool', X1[kt * 128:(kt + 1) * 128, :], xo[b][:], R=[f'gxo{b}'], W=[('X1', kt)])))
            pend[0]()
    k.barrier()


QB_PERM = [half * 4 + c for c in range(4) for half in range(2)]


def host_even_layout(w_in_even, w_out_even):
    wi = np.array(w_in_even, copy=True)
    wo = np.array(w_out_even, copy=True)
    for p, hq in enumerate(QB_PERM):
        wi[:, :, 2048 + p * 64:2048 + (p + 1) * 64] = w_in_even[:, :, 2048 + hq * 64:2048 + (hq + 1) * 64]
        wo[:, 512 + p * 64:512 + (p + 1) * 64, :] = w_out_even[:, 512 + hq * 64:512 + (hq + 1) * 64, :]
    return wi, wo


def host_rope_consts():
    pos = np.arange(S, dtype=np.float64)
    out = np.zeros((S, 384), np.float32)

    def tab(half):
        inv = 10000.0 ** (-np.arange(half, dtype=np.float32) / half)
        ang = pos.astype(np.float32)[:, None] * inv[None, :]
        return np.cos(ang).astype(np.float32), np.sin(ang).astype(np.float32)
    ca, sa = tab(64)
    cb, sb_ = tab(32)
    out[:, 0:64] = ca; out[:, 64:128] = sa
    out[:, 128:192] = ca * np.float32(128 ** -0.5); out[:, 192:256] = sa * np.float32(128 ** -0.5)
    out[:, 256:288] = cb * np.float32(0.125); out[:, 288:320] = sb_ * np.float32(0.125)
    out[:, 320:352] = cb; out[:, 352:384] = sb_
    return out.reshape(NT, 128, 384)


def rope_tm(cx, eng, src, skey, H, hd, cos, sin, ckey, dst, dkey, t1, t2, tkey):
    k = cx.k
    sv = src.rearrange("p (h t d) -> p h t d", h=H, t=2)
    dv = dst.rearrange("p (h t d) -> p h t d", h=H, t=2)
    x1, x2 = sv[:, :, 0, :], sv[:, :, 1, :]
    cb = cos.unsqueeze(1).broadcast_to([128, H, hd])
    sb_ = sin.unsqueeze(1).broadcast_to([128, H, hd])
    a = t1.rearrange("p (h d) -> p h d", h=H)
    b = t2.rearrange("p (h d) -> p h d", h=H)
    tt = lambda o, i0, i1, op, R, W: k.op(eng, lambda e: e.tensor_tensor(out=o, in0=i0, in1=i1, op=op), R=R, W=W)
    tt(a, x1, cb, ALU.mult, [skey, ckey], [tkey + 'a'])
    tt(b, x2, sb_, ALU.mult, [skey, ckey], [tkey + 'b'])
    tt(dv[:, :, 0, :], a, b, ALU.subtract, [tkey + 'a', tkey + 'b'], [dkey])
    tt(a, x1, sb_, ALU.mult, [skey, ckey], [tkey + 'a'])
    tt(b, x2, cb, ALU.mult, [skey, ckey], [tkey + 'b'])
    tt(dv[:, :, 1, :], a, b, ALU.add, [tkey + 'a', tkey + 'b'], [dkey])


def even_phase(cx, L, X, X1):
    nc, k, din = cx.nc, cx.k, cx.din
    ev = L // 2
    SGA, YA = cx.SGA, cx.YA
    NH = 16

    def inproj_pass(w, wkey, ncols, blocks, epilogue, epilogue2, es1, sbr):
        xt = [sbr(f"ext{j}", [128, D], F32) for j in range(2)]
        xb = [sbr(f"exb{j}", [128, D], BF16) for j in range(2)]
        xT = [sbr(f"exT{j}", [128, 8, 128], BF16) for j in range(2)]
        rpt = [sbr(f"erp{j}", [128, 384], F32) for j in range(2)]
        ptr = [es1.enter_context(cx.pst(f"eptr{j}", [128, 8, 128], BF16)) for j in range(2)]
        pp = [es1.enter_context(cx.pst(f"epp{j}", [128, 512], F32)) for j in range(len(blocks))]

        def stageA(i):
            b = i % 2
            k.dma('sp', xt[b][:], X[i * 128:(i + 1) * 128, :], W=[f'ext{b}'])
            k.dma('sp', rpt[b][:], cx.d_rope[i], W=[f'erp{b}'])
            tile_to_xT(cx, xt[b][:], f'ext{b}', xb[b], f'exb{b}', ptr[b], f'eptr{b}', xT[b], f'exT{b}', cast_eng='act', copy_eng='dve')
        stageA(0)
        for i in range(NT):
            b = i % 2
            if i + 1 < NT:
                stageA(i + 1)
            for blk, (c0, cn) in enumerate(blocks):
                P = pp[blk]
                for kc in range(8):
                    k.op('pe', lambda e: e.matmul(P[:, 0:cn], lhsT=xT[b][:, kc, :], rhs=w[:, kc, c0:c0 + cn],
                                                  start=(kc == 0), stop=(kc == 7)), R=[f'exT{b}', wkey], W=[f'epp{blk}'])
                epilogue(i, b, blk, P, rpt[b], f'erp{b}')
            if i > 0:
                epilogue2(i - 1)
        epilogue2(NT - 1)

    with ExitStack() as esA:
        sbl = lambda n, s, d: esA.enter_context(cx.sbt(n, s, d))
        qaT = sbl("qaT", [128, 4, S], BF16)
        kaT = sbl("kaT", [128, 4, S], BF16)
        va_all = sbl("va_all", [128, NT, 512], BF16)
        with ExitStack() as es1:
            sbr = lambda n, s, d: es1.enter_context(cx.sbt(n, s, d, side='right'))
            w = sbr("ewA", [128, 8, 2048], BF16)
            for cb in range(4):
                k.dma('pool', w[:, :, cb * 512:(cb + 1) * 512],
                      din['w_in_even'][ev][:, cb * 512:(cb + 1) * 512].rearrange("(kc p) n -> p kc n", p=128), W=['ewA'])
            hs = [sbr(f"ehs{j}", [128, 512], F32) for j in range(2)]
            qr = [[sbr(f"eqr{j}_{t}", [128, 512], BF16) for t in range(2)] for j in range(2)]
            tq = [sbr(f"etq{j}", [128, 256], F32) for j in range(4)]
            sga = [sbr(f"esga{j}", [128, 512], BF16) for j in range(2)]
            ptq = [es1.enter_context(cx.pst(f"eptq{j}", [128, 4, 128], BF16)) for j in range(2)]

            def epiA2(i):
                t = i % 2
                for blk in range(2):
                    for h in range(4):
                        k.op('pe', lambda e: e.transpose(out=ptq[blk][:, h, :], in_=qr[blk][t][:, h * 128:(h + 1) * 128], identity=cx.ident_b[:]),
                             R=[f'eqr{blk}_{t}', 'ident_b'], W=[f'eptq{blk}'])
                    if blk == 0:
                        k.op('act', lambda e: e.copy(out=qaT[:, :, i * 128:(i + 1) * 128], in_=ptq[blk][:]), R=[f'eptq{blk}'], W=['qaT'])
                    else:
                        k.op('dve', lambda e: e.tensor_copy(out=kaT[:, :, i * 128:(i + 1) * 128], in_=ptq[blk][:]), R=[f'eptq{blk}'], W=['kaT'])

            def epiA(i, b, blk, P, rp, rpkey):
                if blk < 2:
                    t = i % 2
                    k.op('act', lambda e: e.copy(out=hs[blk][:], in_=P[:]), R=[f'epp{blk}'], W=[f'ehs{blk}'])
                    eng = 'dve' if blk == 0 else 'pool'
                    co = 0 if blk == 0 else 128
                    rope_tm(cx, eng, hs[blk][:], f'ehs{blk}', 4, 64, rp[:, co:co + 64], rp[:, co + 64:co + 128],
                            rpkey, qr[blk][t][:], f'eqr{blk}_{t}', tq[2 * blk][:], tq[2 * blk + 1][:], f'etq{blk}')
                elif blk == 2:
                    k.op('dve', lambda e: e.tensor_copy(out=va_all[:, i, :], in_=P[:]), R=[f'epp{blk}'], W=['va_all'])
                else:
                    k.op('act', lambda e: e.activation(out=sga[b][:], in_=P[:], func=AF.Silu), R=[f'epp{blk}'], W=[f'esga{b}'])
                    k.dma('pool', SGA[i * 128:(i + 1) * 128, :], sga[b][:], R=[f'esga{b}'], W=['SGA'])
            inproj_pass(w, 'ewA', 2048, [(0, 512), (512, 512), (1024, 512), (1536, 512)], epiA, epiA2, es1, sbr)
        k.barrier()

        with ExitStack() as es1:
            sbr = lambda n, s, d: es1.enter_context(cx.sbt(n, s, d, side='right'))
            lgt = sbr("rlg", [128, 8], F32)
            gng = sbr("rgng", [128, 512], F32)
            k.dma('sp', lgt[:], din['ret_decay_logit'][ev].rearrange("a h -> (a h)").partition_broadcast(128), W=['rlg'])
            k.dma('sp', gng[:], din['ret_gn_gain'][ev].partition_broadcast(128), W=['rgng'])
            k.op('act', lambda e: e.activation(out=lgt[:], in_=lgt[:], func=AF.Exp, scale=-1.0), R=['rlg'], W=['rlg'])
            k.op('dve', lambda e: e.tensor_scalar(out=lgt[:], in0=lgt[:], scalar1=1.0, scalar2=None, op0=ALU.add), R=['rlg'], W=['rlg'])
            k.op('act', lambda e: e.activation(out=lgt[:], in_=lgt[:], func=AF.Ln), R=['rlg'], W=['rlg'])
            k.op('dve', lambda e: e.tensor_scalar(out=lgt[:], in0=lgt[:], scalar1=-1.0, scalar2=None, op0=ALU.mult), R=['rlg'], W=['rlg'])
            DT = sbr("rDT", [128, 128], F32)
            XI = [sbr("rXIF", [128, 128], BF16), sbr("rXIB", [128, 128], BF16)]
            zc = sbr("rzc", [128, 4], F32)
            arg = sbr("rarg", [128, 128], F32)
            qs = [sbr("rqf", [128, S], BF16), sbr("rqb", [128, S], BF16)]
            Vz = [sbr("rVzf", [128, NT, 128], BF16), sbr("rVzb", [128, NT, 128], BF16)]
            ktm = sbr("rktm", [128, NT, 128], BF16)
            Rb_all = sbr("rRb_all", [128, NT, 128], BF16)
            R32 = [sbr("rRf32", [128, 128], F32), sbr("rRb32", [128, 128], F32)]
            Rfb = [sbr(f"rRfb{j}", [128, 128], BF16) for j in range(2)]
            Pm = [sbr(f"rPm{j}", [128, 128], BF16) for j in range(2)]
            Yraw = [sbr(f"rYraw{j}", [128, NH, 128], F32) for j in range(2)]
            Ysq = sbr("rYsq", [128, NH, 128], F32)
            sgs = [sbr(f"rsgs{j}", [128, NH, 128], BF16) for j in range(2)]
            yout = [sbr(f"ryout{j}", [128, NH, 128], BF16) for j in range(2)]
            gv = sbr("rgv", [128, 8, NH], F32)
            pk = [es1.enter_context(cx.pst(f"rpk{j}", [128, 8, 128], BF16)) for j in range(2)]
            pkv = [es1.enter_context(cx.pst(f"rpkv{j}", [128, 128], F32)) for j in range(2)]
            pst = [es1.enter_context(cx.pst(f"rpst{j}", [128, 128], F32)) for j in range(2)]
            py = [es1.enter_context(cx.pst(f"rpy{j}", [128, 128], F32)) for j in range(2)]
            pcol = cslice(cx, 'pcol')
            nslab = 0
            for h in range(4):
                lgf, lgb = lgt[:, h:h + 1], lgt[:, 4 + h:5 + h]
                k.op('dve', lambda e: e.tensor_scalar(out=arg[:], in0=cslice(cx, 'rp'), scalar1=lgf, scalar2=None, op0=ALU.mult),
                     R=['cst', 'rlg'], W=['rarg'])
                k.op('dve', lambda e: e.scalar_tensor_tensor(out=arg[:], in0=cslice(cx, 'rn'), scalar=lgb, in1=arg[:], op0=ALU.mult, op1=ALU.add),
                     R=['cst', 'rlg', 'rarg'], W=['rarg'])
                k.op('act', lambda e: e.activation(out=DT[:], in_=arg[:], func=AF.Exp), R=['rarg'], W=['rDT'])
                k.op('act', lambda e: e.activation(out=XI[0][:], in_=cslice(cx, 'cp1'), func=AF.Exp, scale=lgf), R=['cst', 'rlg'], W=['rXI0'])
                k.op('act', lambda e: e.activation(out=XI[1][:], in_=cslice(cx, 'cmc'), func=AF.Exp, scale=lgb), R=['cst', 'rlg'], W=['rXI1'])
                k.op('act', lambda e: e.activation(out=zc[:, 0:1], in_=pcol[:, 0:1], func=AF.Exp, scale=lgf), R=['cst', 'rlg'], W=['rzc'])
                k.op('act', lambda e: e.activation(out=zc[:, 1:2], in_=pcol[:, 1:2], func=AF.Exp, scale=lgb), R=['cst', 'rlg'], W=['rzc'])
                k.op('act', lambda e: e.activation(out=zc[:, 2:3], in_=pcol[:, 2:3], func=AF.Exp, scale=lgf), R=['cst', 'rlg'], W=['rzc'])
                k.op('act', lambda e: e.activation(out=zc[:, 3:4], in_=pcol[:, 2:3], func=AF.Exp, scale=lgb), R=['cst', 'rlg'], W=['rzc'])
                for d_ in range(2):
                    k.op('dve', lambda e: e.tensor_tensor(out=qs[d_][:].rearrange("p (n c) -> p n c", c=128),
                                                          in0=qaT[:, h, :].rearrange("p (n c) -> p n c", c=128),
                                                          in1=XI[d_][:].unsqueeze(1).broadcast_to([128, NT, 128]), op=ALU.mult),
                         R=['qaT', f'rXI{d_}'], W=[f'rqs{d_}'])
                    k.op('act', lambda e: e.activation(out=Vz[d_][:], in_=va_all[:, :, h * 128:(h + 1) * 128], func=AF.Identity, scale=zc[:, d_:d_ + 1]),
                         R=['va_all', 'rzc'], W=[f'rVz{d_}'])
                for g8 in range(4):
                    P = pk[g8 % 2]
                    for j in range(8):
                        n = g8 * 8 + j
                        k.op('pe', lambda e: e.transpose(out=P[:, j, :], in_=kaT[:, h, n * 128:(n + 1) * 128], identity=cx.ident_b[:]),
                             R=['kaT', 'ident_b'], W=[f'rpk{g8 % 2}'])
                    k.op('dve', lambda e: e.tensor_copy(out=ktm[:, g8 * 8:(g8 + 1) * 8, :], in_=P[:]), R=[f'rpk{g8 % 2}'], W=['rktm'])
                k.op('dve', lambda e: e.memset(R32[1][:], 0.0), W=['rR32_1'])
                k.op('dve', lambda e: e.memset(R32[0][:], 0.0), W=['rR32_0'])
                nkv = 0
                for n in range(NT - 1, 0, -1):
                    P = pkv[nkv % 2]
                    k.op('pe', lambda e: e.matmul(P[:], lhsT=ktm[:, n, :], rhs=Vz[1][:, n, :], start=True, stop=True),
                         R=['rktm', 'rVz1'], W=[f'rpkv{nkv % 2}'])
                    k.op('dve', lambda e: e.scalar_tensor_tensor(out=R32[1][:], in0=R32[1][:], scalar=zc[:, 3:4], in1=P[:], op0=ALU.mult, op1=ALU.add),
                         R=['rR32_1', 'rzc', f'rpkv{nkv % 2}'], W=['rR32_1'])
                    k.op('act', lambda e: e.copy(out=Rb_all[:, n - 1, :], in_=R32[1][:]), R=['rR32_1'], W=['rRb_all'])
                    nkv += 1
                def scores(n):
                    b = n % 2
                    cs_ = slice(n * 128, (n + 1) * 128)
                    k.op('pe', lambda e: e.matmul(pst[b][:], lhsT=kaT[:, h, cs_], rhs=qaT[:, h, cs_], start=True, stop=True),
                         R=['kaT', 'qaT'], W=[f'rpst{b}'])
                    k.op('dve', lambda e: e.tensor_tensor(out=Pm[b][:], in0=pst[b][:], in1=DT[:], op=ALU.mult), R=[f'rpst{b}', 'rDT'], W=[f'rPm{b}'])
                scores(0)
                for n in range(NT):
                    b = n % 2
                    sl = nslab % 2
                    cs_ = slice(n * 128, (n + 1) * 128)
                    if n % NH == 0:
                        k.dma('sp', sgs[sl][:], SGA[n * 128:(n + NH) * 128, h * 128:(h + 1) * 128].rearrange("(j p) c -> p j c", p=128),
                              R=['SGA'], W=[f'rsgs{sl}'])
                    if n + 1 < NT:
                        scores(n + 1)
                    last = 'intra'
                    if n < NT - 1:
                        last = 'bwd'
                    elif n > 0:
                        last = 'fwd'
                    k.op('pe', lambda e: e.matmul(py[b][:], lhsT=Pm[b][:], rhs=va_all[:, n, h * 128:(h + 1) * 128], start=True, stop=(last == 'intra')),
                         R=[f'rPm{b}', 'va_all'], W=[f'rpy{b}'])
                    if n > 0:
                        k.op('pe', lambda e: e.matmul(py[b][:], lhsT=qs[0][:, cs_], rhs=Rfb[(n - 1) % 2][:], start=False, stop=(last == 'fwd')),
                             R=['rqs0', f'rRfb{(n - 1) % 2}'], W=[f'rpy{b}'])
                    if n < NT - 1:
                        k.op('pe', lambda e: e.matmul(py[b][:], lhsT=qs[1][:, cs_], rhs=Rb_all[:, n, :], start=False, stop=True),
                             R=['rqs1', 'rRb_all'], W=[f'rpy{b}'])
                    k.op('act', lambda e: e.copy(out=Yraw[sl][:, n % NH, :], in_=py[b][:]), R=[f'rpy{b}'], W=[f'rYraw{sl}'])
                    if n < NT - 1:
                        P = pkv[nkv % 2]
                        k.op('pe', lambda e: e.matmul(P[:], lhsT=ktm[:, n, :], rhs=Vz[0][:, n, :], start=True, stop=True),
                             R=['rktm', 'rVz0'], W=[f'rpkv{nkv % 2}'])
                        k.op('dve', lambda e: e.scalar_tensor_tensor(out=R32[0][:], in0=R32[0][:], scalar=zc[:, 2:3], in1=P[:], op0=ALU.mult, op1=ALU.add),
                             R=['rR32_0', 'rzc', f'rpkv{nkv % 2}'], W=['rR32_0'])
                        k.op('act', lambda e: e.copy(out=Rfb[n % 2][:], in_=R32[0][:]), R=['rR32_0'], W=[f'rRfb{n % 2}'])
                        nkv += 1
                    if n % NH == NH - 1:
                        Y = Yraw[sl]
                        G = lambda j: gv[:, j, :]
                        bc = lambda ap: ap.unsqueeze(2).broadcast_to([128, NH, 128])
                        yk = f'rYraw{sl}'
                        k.op('dve', lambda e: e.tensor_reduce(out=G(0), in_=Y[:], axis=AX.X, op=ALU.add), R=[yk], W=['rg0'])
                        k.op('act', lambda e: e.activation(out=Ysq[:], in_=Y[:], func=AF.Square), R=[yk], W=['rYsq'])
                        k.op('dve', lambda e: e.tensor_reduce(out=G(1), in_=Ysq[:], axis=AX.X, op=ALU.add), R=['rYsq'], W=['rg1'])
                        k.op('dve', lambda e: e.tensor_scalar(out=G(2), in0=G(0), scalar1=1.0 / 128, scalar2=None, op0=ALU.mult), R=['rg0'], W=['rg2'])
                        k.op('dve', lambda e: e.tensor_tensor(out=G(3), in0=G(2), in1=G(2), op=ALU.mult), R=['rg2'], W=['rg3'])
                        k.op('dve', lambda e: e.scalar_tensor_tensor(out=G(4), in0=G(1), scalar=1.0 / 128, in1=G(3), op0=ALU.mult, op1=ALU.subtract),
                             R=['rg1', 'rg3'], W=['rg4'])
                        k.op('act', lambda e: e.activation(out=G(5), in_=G(4), func=AF.Sqrt, bias=cx.eps_gn[:], scale=1.0), R=['rg4', 'eps_gn'], W=['rg5'])
                        k.op('dve', lambda e: e.reciprocal(out=G(6), in_=G(5)), R=['rg5'], W=['rg6'])
                        k.op('dve', lambda e: e.tensor_tensor(out=Y[:], in0=Y[:], in1=bc(G(2)), op=ALU.subtract), R=[yk, 'rg2'], W=[yk])
                        k.op('dve', lambda e: e.tensor_tensor(out=Y[:], in0=Y[:], in1=bc(G(6)), op=ALU.mult), R=[yk, 'rg6'], W=[yk])
                        k.op('dve', lambda e: e.tensor_tensor(out=Y[:], in0=Y[:], in1=gng[:, h * 128:(h + 1) * 128].unsqueeze(1).broadcast_to([128, NH, 128]),
                                                              op=ALU.mult), R=[yk, 'rgng'], W=[yk])
                        k.op('dve', lambda e: e.tensor_tensor(out=yout[sl][:], in0=Y[:], in1=sgs[sl][:], op=ALU.mult), R=[yk, f'rsgs{sl}'], W=[f'ryout{sl}'])
                        n0 = n - (NH - 1)
                        k.dma('sp', YA[n0 * 128:(n0 + NH) * 128, h * 128:(h + 1) * 128].rearrange("(j p) c -> p j c", p=128), yout[sl][:],
                              R=[f'ryout{sl}'], W=['YA'])
                        nslab += 1
        k.barrier()

    with ExitStack() as esB:
        sbl = lambda n, s, d: esB.enter_context(cx.sbt(n, s, d))
        yb_all = sbl("yb_all", [128, NT, 512], BF16)
        with ExitStack() as esB2:
            sbl2 = lambda n, s, d: esB2.enter_context(cx.sbt(n, s, d))
            qbT = sbl2("qbT", [128, 4, S], BF16)
            kbT = sbl2("kbT", [128, S], BF16)
            vb_all = sbl2("vb_all", [128, NT, 128], BF16)
            with ExitStack() as es1:
                sbr = lambda n, s, d: es1.enter_context(cx.sbt(n, s, d, side='right'))
                w = sbr("ewB", [128, 8, 768], BF16)
                for cb in range(2):
                    k.dma('pool', w[:, :, cb * 384:(cb + 1) * 384],
                          din['w_in_even'][ev][:, 2048 + cb * 384:2048 + (cb + 1) * 384].rearrange("(kc p) n -> p kc n", p=128), W=['ewB'])
                hs = [sbr(f"ehs{j}", [128, 512], F32) for j in range(2)]
                qr = [[sbr(f"eqr{j}_{t}", [128, 512], BF16) for t in range(2)] for j in range(2)]
                tq = [sbr(f"etq{j}", [128, 256], F32) for j in range(4)]
                ptq = [es1.enter_context(cx.pst(f"eptq{j}", [128, 4, 128], BF16)) for j in range(2)]

                def epiB2(i):
                    t = i % 2
                    for c in range(4):
                        k.op('pe', lambda e: e.transpose(out=ptq[0][:, c, :], in_=qr[0][t][:, c * 128:(c + 1) * 128], identity=cx.ident_b[:]),
                             R=[f'eqr0_{t}', 'ident_b'], W=['eptq0'])
                    k.op('act', lambda e: e.copy(out=qbT[:, :, i * 128:(i + 1) * 128], in_=ptq[0][:]), R=['eptq0'], W=['qbT'])
                    k.op('pe', lambda e: e.transpose(out=ptq[1][:, 0, :], in_=qr[1][t][:, 0:128], identity=cx.ident_b[:]),
                         R=[f'eqr1_{t}', 'ident_b'], W=['eptq1'])
                    k.op('dve', lambda e: e.tensor_copy(out=kbT[:, i * 128:(i + 1) * 128], in_=ptq[1][:, 0, :]), R=['eptq1'], W=['kbT'])

                def epiB(i, b, blk, P, rp, rpkey):
                    t = i % 2
                    cn = 512 if blk == 0 else 256
                    k.op('act', lambda e: e.copy(out=hs[blk][:, 0:cn], in_=P[:, 0:cn]), R=[f'epp{blk}'], W=[f'ehs{blk}'])
                    if blk == 0:
                        rope_tm(cx, 'dve', hs[0][:], 'ehs0', 8, 32, rp[:, 256:288], rp[:, 288:320], rpkey,
                                qr[0][t][:], f'eqr0_{t}', tq[0][:], tq[1][:], 'etq0')
                    else:
                        rope_tm(cx, 'pool', hs[1][:, 0:128], 'ehs1', 2, 32, rp[:, 320:352], rp[:, 352:384], rpkey,
                                qr[1][t][:, 0:128], f'eqr1_{t}', tq[2][:, 0:64], tq[3][:, 0:64], 'etq1')
                        k.op('dve', lambda e: e.tensor_copy(out=vb_all[:, i, :], in_=hs[1][:, 128:256]), R=['ehs1'], W=['vb_all'])
                inproj_pass(w, 'ewB', 768, [(0, 512), (512, 256)], epiB, epiB2, es1, sbr)
            k.barrier()

            with ExitStack() as es1:
                sbr = lambda n, s, d: es1.enter_context(cx.sbt(n, s, d, side='right'))
                sk = sbr("ask", [128, 8], F32)
                k.dma('sp', sk[:], din['sink_logit'][ev].partition_broadcast(128), W=['ask'])
                sm4 = [sbr(f"asm{j}", [128, 4, 384], F32) for j in range(2)]
                pb4 = [sbr(f"apb{j}", [128, 4, 384], BF16) for j in range(2)]
                pT4 = [sbr(f"apT{j}", [128, 12, 128], BF16) for j in range(2)]
                a8 = [sbr(f"aa8{j}", [128, 8, 4], F32) for j in range(2)]
                ps4 = es1.enter_context(cx.pst("aps4", [128, 4, 512], F32))
                ppT = es1.enter_context(cx.pst("appT", [128, 12, 128], BF16))
                po = es1.enter_context(cx.pst("apo", [128, 4, 64], F32))
                mk = cslice(cx, 'mk')
                steps = [(n, half) for n in range(NT) for half in range(2)]

                def geom(n):
                    j0 = max(n - 1, 0)
                    j1 = min(n + 1, NT - 1)
                    return j0, j1, j1 - j0 + 1, (j0 - (n - 1)) * 128

                def att_F1(it):
                    n, half = steps[it]
                    j0, j1, nk, mo = geom(n)
                    W_ = nk * 128
                    b = it % 2
                    pl = slice(64 * half, 64 * half + 64)
                    A = lambda j: a8[b][:, j, :]
                    skh = sk[:, half * 4:half * 4 + 4]
                    for c in range(4):
                        k.op('pe', lambda e: e.matmul(ps4[:, c, 0:W_], lhsT=qbT[pl, c, n * 128:(n + 1) * 128], rhs=kbT[pl, j0 * 128:(j1 + 1) * 128],
                                                      start=True, stop=True), R=['qbT', 'kbT'], W=['aps4'])
                    k.op('dve', lambda e: e.tensor_tensor(out=sm4[b][:, :, 0:W_], in0=ps4[:, :, 0:W_],
                                                          in1=mk[:, mo:mo + W_].unsqueeze(1).broadcast_to([128, 4, W_]), op=ALU.add),
                         R=['aps4', 'cst'], W=[f'asm{b}'])
                    k.op('dve', lambda e: e.tensor_reduce(out=A(0), in_=sm4[b][:, :, 0:W_], axis=AX.X, op=ALU.max), R=[f'asm{b}'], W=[f'aa{b}0'])
                    k.op('dve', lambda e: e.tensor_tensor(out=A(0), in0=A(0), in1=skh, op=ALU.max), R=[f'aa{b}0', 'ask'], W=[f'aa{b}0'])
                    k.op('dve', lambda e: e.tensor_scalar(out=A(1), in0=A(0), scalar1=-1.0, scalar2=None, op0=ALU.mult), R=[f'aa{b}0'], W=[f'aa{b}1'])
                    k.op('dve', lambda e: e.tensor_tensor(out=A(3), in0=skh, in1=A(0), op=ALU.subtract), R=['ask', f'aa{b}0'], W=[f'aa{b}3'])

                def att_F2(it):
                    n, half = steps[it]
                    j0, j1, nk, mo = geom(n)
                    W_ = nk * 128
                    b = it % 2
                    A = lambda j: a8[b][:, j, :]
                    for c in range(4):
                        k.op('act', lambda e: e.activation(out=pb4[b][:, c, 0:W_], in_=sm4[b][:, c, 0:W_], func=AF.Exp, bias=a8[b][:, 1, c:c + 1], scale=1.0,
                                                           accum_out=a8[b][:, 2, c:c + 1]), R=[f'asm{b}', f'aa{b}1'], W=[f'apb{b}', f'aa{b}2'])
                    k.op('act', lambda e: e.activation(out=A(3), in_=A(3), func=AF.Exp), R=[f'aa{b}3'], W=[f'aa{b}3'])

                def att_B1(it):
                    n, half = steps[it]
                    j0, j1, nk, mo = geom(n)
                    b = it % 2
                    for c in range(4):
                        for kb_ in range(nk):
                            k.op('pe', lambda e: e.transpose(out=ppT[:, c * 3 + kb_, :], in_=pb4[b][:, c, kb_ * 128:(kb_ + 1) * 128], identity=cx.ident_b[:]),
                                 R=[f'apb{b}', 'ident_b'], W=['appT'])
                    if nk == 3:
                        k.op('act', lambda e: e.copy(out=pT4[b][:, 0:6, :], in_=ppT[:, 0:6, :]), R=['appT'], W=[f'apT{b}'])
                        k.op('act', lambda e: e.copy(out=pT4[b][:, 6:12, :], in_=ppT[:, 6:12, :]), R=['appT'], W=[f'apT{b}'])
                    else:
                        for c in range(4):
                            k.op('act', lambda e: e.copy(out=pT4[b][:, c * 3:c * 3 + nk, :], in_=ppT[:, c * 3:c * 3 + nk, :]), R=['appT'], W=[f'apT{b}'])

                def att_B2(it):
                    n, half = steps[it]
                    j0, j1, nk, mo = geom(n)
                    b = it % 2
                    A = lambda j: a8[b][:, j, :]
                    for c in range(4):
                        for kb_ in range(nk):
                            k.op('pe', lambda e: e.matmul(po[:, c, :], lhsT=pT4[b][:, c * 3 + kb_, :], rhs=vb_all[:, j0 + kb_, half * 64:(half + 1) * 64],
                                                          start=(kb_ == 0), stop=(kb_ == nk - 1)), R=[f'apT{b}', 'vb_all'], W=['apo'])
                    k.op('dve', lambda e: e.tensor_tensor(out=A(4), in0=A(2), in1=A(3), op=ALU.add), R=[f'aa{b}2', f'aa{b}3'], W=[f'aa{b}4'])
                    k.op('dve', lambda e: e.reciprocal(out=A(5), in_=A(4)), R=[f'aa{b}4'], W=[f'aa{b}5'])
                    k.op('dve', lambda e: e.tensor_tensor(out=yb_all[:, n, :].rearrange("p (c t d) -> p c t d", c=4, t=2)[:, :, half, :], in0=po[:],
                                                          in1=A(5).unsqueeze(2).broadcast_to([128, 4, 64]), op=ALU.mult),
                         R=['apo', f'aa{b}5'], W=['yb_all'])
                att_F1(0)
                att_F2(0)
                for it in range(len(steps)):
                    if it + 1 < len(steps):
                        att_F1(it + 1)
                    att_B1(it)
                    if it + 1 < len(steps):
                        att_F2(it + 1)
                    att_B2(it)
            k.barrier()

        with ExitStack() as es1:
            sbr = lambda n, s, d: es1.enter_context(cx.sbt(n, s, d, side='right'))
            wo = sbr("ewo", [128, 8, D], BF16)
            for cb in range(2):
                k.dma('pool', wo[:, :, cb * 512:(cb + 1) * 512],
                      din['w_out_even'][ev][:, cb * 512:(cb + 1) * 512].rearrange("(kc p) n -> p kc n", p=128), W=['ewo'])
            NB = 3
            xt = [sbr(f"oxt{j}", [128, D], F32) for j in range(NB)]
            yat = [sbr(f"oya{j}", [128, 512], BF16) for j in range(NB)]
            hh_ = [sbr(f"oh{j}", [128, D], F32) for j in range(NB)]
            xo = [sbr(f"oxo{j}", [128, D], F32) for j in range(NB)]
            yT = [sbr(f"oyT{j}", [128, 8, 128], BF16) for j in range(2)]
            ptr = [es1.enter_context(cx.pst(f"optr{j}", [128, 8, 128], BF16)) for j in range(2)]
            pm = [es1.enter_context(cx.pst(f"opm{j}", [128, 512], F32)) for j in range(4)]
            wk = ln_work(cx, es1, "ln1e")
            gain, bias, gkeys = ln_params(cx, es1, 'ln1', cx.lnL if hasattr(cx, 'lnL') else L)

            def stageA(i):
                b = i % NB
                b2 = i % 2
                k.dma('sp', xt[b][:], X[i * 128:(i + 1) * 128, :], W=[f'oxt{b}'])
                k.dma('sp', yat[b][:], YA[i * 128:(i + 1) * 128, :], R=['YA'], W=[f'oya{b}'])
                for kc in range(8):
                    src = yat[b][:, kc * 128:(kc + 1) * 128] if kc < 4 else yb_all[:, i, (kc - 4) * 128:(kc - 3) * 128]
                    k.op('pe', lambda e: e.transpose(out=ptr[b2][:, kc, :], in_=src, identity=cx.ident_b[:]),
                         R=[f'oya{b}', 'yb_all', 'ident_b'], W=[f'optr{b2}'])
                k.op('act', lambda e: e.copy(out=yT[b2][:], in_=ptr[b2][:]), R=[f'optr{b2}'], W=[f'oyT{b2}'])
            nn = 0
            pend = [None]
            stageA(0)
            for i in range(NT):
                b = i % NB
                b2 = i % 2
                if i + 1 < NT:
                    stageA(i + 1)
                for hh in range(2):
                    P = pm[nn % 4]
                    for kc in range(8):
                        k.op('pe', lambda e: e.matmul(P[:], lhsT=yT[b2][:, kc, :], rhs=wo[:, kc, hh * 512:(hh + 1) * 512], start=(kc == 0), stop=(kc == 7)),
                             R=[f'oyT{b2}', 'ewo'], W=[f'opm{nn % 4}'])
                    k.op('dve', lambda e: e.scalar_tensor_tensor(out=hh_[b][:, hh * 512:(hh + 1) * 512], in0=xt[b][:, hh * 512:(hh + 1) * 512],
                                                                 scalar=ALPHA, in1=P[:], op0=ALU.mult, op1=ALU.add),
                         R=[f'oxt{b}', f'opm{nn % 4}'], W=[f'oh{b}'])
                    nn += 1
                tail = layernorm_tile(cx, hh_[b], f'oh{b}', xo[b][:], f'oxo{b}', gain, bias, gkeys, wk, sfx=str(b), mid=pend[0], defer=True)
                pend[0] = (lambda tail=tail, i=i, b=b: (tail(), k.dma('pool', X1[i * 128:(i + 1) * 128, :], xo[b][:], R=[f'oxo{b}'], W=[('X1', i)])))
            pend[0]()
    k.barrier()


W_SHAPES = {
    'w_in_even': (2, 1024, IN_EVEN), 'ret_decay_logit': (2, 2, 4), 'ret_gn_gain': (2, 512), 'sink_logit': (2, 8),
    'w_out_even': (2, 1024, 1024), 'w_out_fourier': (2, 1024, 1024),
    'ln1_gain': (4, 1024), 'ln1_bias': (4, 1024), 'ln2_gain': (4, 1024), 'ln2_bias': (4, 1024),
    'router_coarse_w': (4, 1024, 4), 'router_coarse_b': (4, 4), 'router_fine_w': (4, 1024, 32), 'router_fine_b': (4, 32),
    'expert_w_gate': (4, 32, 1024, 512), 'expert_w_up': (4, 32, 1024, 512), 'expert_w_down': (4, 32, 512, 1024),
}


def build_program():
    nc = bass.Bass("TRN2", target_bir_lowering=False)
    cx = Ctx()
    cx.nc = nc
    cx.k = KB(nc)

    def din(name, shape, dt=F32):
        return nc.dram_tensor(name, list(shape), dt, kind="ExternalInput").ap()
    cx.din = {nm: din(nm, shp) for nm, shp in W_SHAPES.items()}
    x_in = din('x', (S, D))
    cx.d_cst = din('cst', (128, CST_N))
    cx.d_rope = din('rope', (NT, 128, 384))
    cx.d_dftc = din('dftc', (128, 2, 2, 256))
    cx.d_dfts = din('dfts', (NT, 128, 2, NT // 2, 128), BF16)
    cx.d_cmid = din('cmid', (1, S), BF16)
    cx.d_ridx = nc.dram_tensor('ridx', [128, NT // 2 + 1], I32, kind='ExternalInput').ap()
    out = nc.dram_tensor("out", [S, D], F32, kind="ExternalOutput").ap()
    XA = nc.dram_tensor("XA", [S, D], F32, kind="Internal").ap()
    XB = nc.dram_tensor("XB", [S, D], F32, kind="Internal").ap()
    cx.XE = nc.dram_tensor("XE", [NE * CAP, D], BF16, kind="Internal").ap()
    cx.YE = nc.dram_tensor("YE", [NE * CAP, D], F32, kind="Internal").ap()
    cx.SGA = nc.dram_tensor("SGA", [S, 512], BF16, kind="Internal").ap()
    cx.YA = nc.dram_tensor("YA", [S, 512], BF16, kind="Internal").ap()
    load_common(cx)
    cur = x_in
    for L in range(DEPTH):
        if L % 2 == 0:
            even_phase(cx, L, cur, XA)
        else:
            fnet_phase(cx, L, cur, XA)
        dst = out if L == DEPTH - 1 else XB
        moe_phase(cx, L, XA, dst)
        cur = dst
    cx.k.finish_all()
    return nc


def kernel(**inputs):
    inp = {k_: np.ascontiguousarray(np.asarray(v, dtype=np.float32)) for k_, v in inputs.items()}
    B = inp['x'].shape[0]
    wi, wo = host_even_layout(inp['w_in_even'], inp['w_out_even'])
    dftc, dfts, cmid = host_dft_consts()
    shared = {nm: inp[nm] for nm in W_SHAPES}
    shared['w_in_even'] = wi
    shared['w_out_even'] = wo
    shared['cst'] = host_consts()
    shared['rope'] = host_rope_consts()
    shared['dftc'] = dftc
    shared['dfts'] = dfts
    shared['cmid'] = cmid
    shared['ridx'] = host_ridx()
    nc = build_program()
    in_maps = []
    for c in range(B):
        m = dict(shared)
        m['x'] = inp['x'][c]
        in_maps.append(m)
    res = run_bass_kernel_spmd(nc, in_maps, core_ids=list(range(B)))
    return np.stack([np.asarray(res.results[c]['out'], dtype=np.float32) for c in range(B)], 0)
```
